# Optimizing a Trainium2 kernel written in Bass

```python
import math
import jax, jax.numpy as jnp
from jax import lax
import numpy as np

D_MODEL = 1024
BATCH = 8
SEQ = 4096
DEPTH = 4

HGRN_HEADS = 4
HGRN_DK = 128
HGRN_DV = 128
HGRN_KW = HGRN_HEADS * HGRN_DK
HGRN_VW = HGRN_HEADS * HGRN_DV
HGRN_CHUNK = 32
MLA_HEADS = 4
MLA_NOPE = 64
MLA_ROPE = 32
MLA_V = 64
MLA_Q_LORA = 192
MLA_KV_LORA = 128
ROPE_THETA = 10000.0
DSA_HEADS = 4
DSA_HEAD_DIM = 64
DSA_WIDTH = DSA_HEADS * DSA_HEAD_DIM
IDX_HEADS = 8
IDX_DIM = 64
TOPK_MAX = 256
NUM_BUCKETS = 32
MAX_DISTANCE = 128
MAX_POS_OFFSET = 1024
D_FF = 4 * D_MODEL
QB = 128
EPS = 1e-6

D_MIX = HGRN_VW + MLA_HEADS * MLA_V + DSA_WIDTH
IN_SIZES = (HGRN_KW, HGRN_KW, HGRN_VW, HGRN_VW,
            MLA_Q_LORA, MLA_KV_LORA + MLA_ROPE,
            DSA_WIDTH, DSA_WIDTH, DSA_WIDTH,
            IDX_HEADS * IDX_DIM, IDX_DIM, IDX_HEADS)
N_IN = sum(IN_SIZES)
SPLIT_POINTS = tuple(int(v) for v in np.cumsum(IN_SIZES)[:-1])

kernel_name = 'hybrid_hgrn2_mla_dsa'


def rms_norm(x, w):
    xf = x.astype(jnp.float32)
    y = xf * lax.rsqrt(jnp.mean(xf * xf, axis=-1, keepdims=True) + EPS)
    return (y * w.astype(jnp.float32)).astype(x.dtype)


def layer_norm(x, w, b):
    xf = x.astype(jnp.float32)
    mu = jnp.mean(xf, axis=-1, keepdims=True)
    var = jnp.mean(jnp.square(xf - mu), axis=-1, keepdims=True)
    y = (xf - mu) * lax.rsqrt(var + EPS)
    return (y * w.astype(jnp.float32) + b.astype(jnp.float32)).astype(x.dtype)


def apply_rope(x, cos, sin):
    half = x.shape[-1] // 2
    x1 = x[..., :half].astype(jnp.float32)
    x2 = x[..., half:].astype(jnp.float32)
    return jnp.concatenate([x1 * cos - x2 * sin, x2 * cos + x1 * sin], axis=-1).astype(x.dtype)


def t5_bucket(dist):
    n = jnp.maximum(dist, 0)
    max_exact = NUM_BUCKETS // 2
    nf = jnp.maximum(n, 1).astype(jnp.float32)
    large = max_exact + (jnp.log(nf / max_exact) / math.log(MAX_DISTANCE / max_exact)
                         * (NUM_BUCKETS - max_exact)).astype(jnp.int32)
    large = jnp.minimum(large, NUM_BUCKETS - 1)
    return jnp.where(n < max_exact, n, large)


def hgrn2_mixer(q, f_pre, i, g, lb, norm_w):
    B, S, _ = q.shape
    N = S // HGRN_CHUNK
    C = HGRN_CHUNK
    fp = f_pre.astype(jnp.float32)
    lb = lb.astype(jnp.float32)
    log_f = jnp.logaddexp(jnp.log(lb), jnp.log1p(-lb) + jax.nn.log_sigmoid(fp))
    k = (1.0 - lb) * jax.nn.sigmoid(-fp)

    def chunked(a, d):
        return a.astype(jnp.float32).reshape(B, N, C, HGRN_HEADS, d).transpose(1, 0, 3, 2, 4)

    qc = chunked(q, HGRN_DK) * (HGRN_DK ** -0.5)
    kc = chunked(k, HGRN_DK)
    vc = chunked(i, HGRN_DV)
    G = jnp.cumsum(chunked(log_f, HGRN_DK), axis=3)
    q_dec = qc * jnp.exp(G)
    k_inv = kc * jnp.exp(-G)
    causal = jnp.tril(jnp.ones((C, C), dtype=bool))
    A = jnp.where(causal, jnp.einsum('nbhtk,nbhsk->nbhts', q_dec, k_inv), 0.0)
    o_intra = jnp.einsum('nbhts,nbhsv->nbhtv', A, vc)
    G_last = G[:, :, :, -1:, :]
    k_state = kc * jnp.exp(G_last - G)
    chunk_decay = jnp.exp(G_last[:, :, :, 0, :])

    def step(state, xs):
        q_n, k_n, v_n, d_n = xs
        o_n = jnp.einsum('bhtk,bhkv->bhtv', q_n, state)
        state = d_n[..., None] * state + jnp.einsum('bhtk,bhtv->bhkv', k_n, v_n)
        return state, o_n

    s0 = jnp.zeros((B, HGRN_HEADS, HGRN_DK, HGRN_DV), jnp.float32)
    _, o_inter = lax.scan(step, s0, (q_dec, k_state, vc, chunk_decay))
    o = (o_intra + o_inter).transpose(1, 0, 3, 2, 4).reshape(B, S, HGRN_HEADS, HGRN_DV)
    gate = g.astype(jnp.float32).reshape(B, S, HGRN_HEADS, HGRN_DV)
    o = rms_norm(o, norm_w) * jax.nn.silu(gate)
    return o.reshape(B, S, HGRN_VW).astype(q.dtype)


def dense_causal_attention(q, k, v, scale):
    B, S, H, Dk = q.shape
    Dv = v.shape[-1]
    nb = S // QB
    qb = q.reshape(B, nb, QB, H, Dk).swapaxes(0, 1)
    key_pos = jnp.arange(S)

    def one_block(args):
        q_b, start = args
        t = start + jnp.arange(QB)
        logits = jnp.einsum('bqhd,bshd->bhqs', q_b, k).astype(jnp.float32) * scale
        mask = key_pos[None, :] <= t[:, None]
        logits = jnp.where(mask, logits, -jnp.inf)
        p = jax.nn.softmax(logits, axis=-1)
        return jnp.einsum('bhqs,bshd->bqhd', p.astype(v.dtype), v)

    out = lax.map(one_block, (qb, jnp.arange(nb) * QB))
    return out.swapaxes(0, 1).reshape(B, S, H * Dv)


def mla_mixer(q_a, kv_a, cos, sin, q_norm_w, w_qb, kv_norm_w, w_kvb):
    B, S, _ = q_a.shape
    q = (rms_norm(q_a, q_norm_w) @ w_qb).reshape(B, S, MLA_HEADS, MLA_NOPE + MLA_ROPE)
    q_nope, q_pe = q[..., :MLA_NOPE], q[..., MLA_NOPE:]
    c_kv, k_pe = kv_a[..., :MLA_KV_LORA], kv_a[..., MLA_KV_LORA:]
    kv = (rms_norm(c_kv, kv_norm_w) @ w_kvb).reshape(B, S, MLA_HEADS, MLA_NOPE + MLA_V)
    k_nope, v = kv[..., :MLA_NOPE], kv[..., MLA_NOPE:]
    q_pe = apply_rope(q_pe, cos[:, :, None, :], sin[:, :, None, :])
    k_pe = apply_rope(k_pe, cos, sin)[:, :, None, :]
    qf = jnp.concatenate([q_nope, q_pe], axis=-1)
    kf = jnp.concatenate([k_nope, jnp.broadcast_to(k_pe, (B, S, MLA_HEADS, MLA_ROPE))], axis=-1)
    return dense_causal_attention(qf, kf, v, (MLA_NOPE + MLA_ROPE) ** -0.5)


def dsa_mixer(q, k, v, qi, ki, wi, positions, rel_table):
    B, S, H, D = q.shape
    nb = S // QB
    n_sel = min(TOPK_MAX, S // 4)
    key_pos = jnp.arange(S)
    gather = jax.vmap(lambda a, idx: a[idx])
    kif = ki.astype(jnp.float32)

    def blockify(a):
        return a.reshape((B, nb, QB) + a.shape[2:]).swapaxes(0, 1)

    def one_block(args):
        q_b, qi_b, w_b, pos_b, start = args
        t = start + jnp.arange(QB)
        idx_logits = jnp.einsum('bqhd,bsd->bqhs', qi_b.astype(jnp.float32), kif) * (IDX_DIM ** -0.5)
        score = jnp.einsum('bqhs,bqh->bqs', jax.nn.relu(idx_logits), w_b.astype(jnp.float32))
        causal = key_pos[None, :] <= t[:, None]
        score = jnp.where(causal[None], score, -jnp.inf)
        top_val, top_idx = lax.top_k(score, n_sel)
        valid = jnp.isfinite(top_val)
        k_sel = gather(k, top_idx)
        v_sel = gather(v, top_idx)
        pos_sel = gather(positions, top_idx)
        bias = rel_table.astype(jnp.float32)[t5_bucket(pos_b[:, :, None] - pos_sel)]
        logits = (jnp.einsum('bqhd,bqkhd->bqhk', q_b, k_sel).astype(jnp.float32) * (D ** -0.5)
                  + bias.transpose(0, 1, 3, 2))
        logits = jnp.where(valid[:, :, None, :], logits, -jnp.inf)
        p = jax.nn.softmax(logits, axis=-1)
        return jnp.einsum('bqhk,bqkhd->bqhd', p.astype(v.dtype), v_sel)

    out = lax.map(one_block, (blockify(q), blockify(qi), blockify(wi), blockify(positions),
                              jnp.arange(nb) * QB))
    return out.swapaxes(0, 1).reshape(B, S, H * D)


def setup_inputs(seed: int = 0) -> dict:
    key = jax.random.key(seed)
    ks = jax.random.split(key, 20)
    f32 = jnp.float32

    def nrm(k, shape, scale):
        return jax.random.normal(k, shape, f32) * scale

    def gain(k, shape):
        return 1.0 + 0.02 * jax.random.normal(k, shape, f32)

    x = jax.random.normal(ks[0], (BATCH, SEQ, D_MODEL), f32)
    offsets = jax.random.randint(ks[1], (BATCH, 1), 0, MAX_POS_OFFSET, dtype=jnp.int32)
    positions = (offsets + jnp.arange(SEQ, dtype=jnp.int32)[None, :]).astype(jnp.int32)
    return {
        'x': x,
        'positions': positions,
        'attn_norm_w': gain(ks[2], (DEPTH, D_MODEL)),
        'w_in': nrm(ks[3], (DEPTH, D_MODEL, N_IN), D_MODEL ** -0.5),
        'hgrn_lb_logits': nrm(ks[4], (DEPTH, HGRN_KW), 0.1),
        'hgrn_norm_w': gain(ks[5], (DEPTH, HGRN_DV)),
        'mla_q_norm_w': gain(ks[6], (DEPTH, MLA_Q_LORA)),
        'mla_w_qb': nrm(ks[7], (DEPTH, MLA_Q_LORA, MLA_HEADS * (MLA_NOPE + MLA_ROPE)), MLA_Q_LORA ** -0.5),
        'mla_kv_norm_w': gain(ks[8], (DEPTH, MLA_KV_LORA)),
        'mla_w_kvb': nrm(ks[9], (DEPTH, MLA_KV_LORA, MLA_HEADS * (MLA_NOPE + MLA_V)), MLA_KV_LORA ** -0.5),
        'idx_k_norm_w': gain(ks[10], (DEPTH, IDX_DIM)),
        'idx_k_norm_b': nrm(ks[11], (DEPTH, IDX_DIM), 0.02),
        'rel_bias_table': nrm(ks[12], (NUM_BUCKETS, DSA_HEADS), 0.3),
        'w_out': nrm(ks[13], (DEPTH, D_MIX, D_MODEL), D_MIX ** -0.5),
        'mlp_norm_w': gain(ks[14], (DEPTH, D_MODEL)),
        'w_mlp_in': nrm(ks[15], (DEPTH, D_MODEL, D_FF), D_MODEL ** -0.5),
        'w_mlp_out': nrm(ks[16], (DEPTH, D_FF, D_MODEL), D_FF ** -0.5),
        'final_norm_w': gain(ks[17], (D_MODEL,)),
    }


def reference(x, positions, attn_norm_w, w_in, hgrn_lb_logits, hgrn_norm_w, mla_q_norm_w, mla_w_qb,
              mla_kv_norm_w, mla_w_kvb, idx_k_norm_w, idx_k_norm_b, rel_bias_table, w_out,
              mlp_norm_w, w_mlp_in, w_mlp_out, final_norm_w):
    B, S, _ = x.shape
    inv_freq = 1.0 / (ROPE_THETA ** (jnp.arange(0, MLA_ROPE, 2, dtype=jnp.float32) / MLA_ROPE))
    ang = positions.astype(jnp.float32)[..., None] * inv_freq
    cos, sin = jnp.cos(ang), jnp.sin(ang)
    lb_all = jnp.cumsum(jax.nn.softmax(hgrn_lb_logits.astype(jnp.float32), axis=0), axis=0)
    lb_all = lb_all - lb_all[0:1]
    h = x
    for l in range(DEPTH):
        u = rms_norm(h, attn_norm_w[l])
        proj = u @ w_in[l]
        (hq, hf, hi, hg, mqa, mkva, dq, dk, dv, iq, ik, iw) = jnp.split(proj, SPLIT_POINTS, axis=-1)
        y_hgrn = hgrn2_mixer(hq, hf, hi, hg, lb_all[l], hgrn_norm_w[l])
        y_mla = mla_mixer(mqa, mkva, cos, sin, mla_q_norm_w[l], mla_w_qb[l], mla_kv_norm_w[l], mla_w_kvb[l])
        y_dsa = dsa_mixer(dq.reshape(B, S, DSA_HEADS, DSA_HEAD_DIM),
                          dk.reshape(B, S, DSA_HEADS, DSA_HEAD_DIM),
                          dv.reshape(B, S, DSA_HEADS, DSA_HEAD_DIM),
                          iq.reshape(B, S, IDX_HEADS, IDX_DIM),
                          layer_norm(ik, idx_k_norm_w[l], idx_k_norm_b[l]),
                          iw * (IDX_HEADS ** -0.5),
                          positions, rel_bias_table)
        mixed = jnp.concatenate([y_hgrn, y_mla.astype(h.dtype), y_dsa.astype(h.dtype)], axis=-1)
        h = h + mixed @ w_out[l]
        u = rms_norm(h, mlp_norm_w[l])
        h = h + jnp.square(jax.nn.relu(u @ w_mlp_in[l])) @ w_mlp_out[l]
    return rms_norm(h, final_norm_w)
```

```python
import math
import numpy as np
import concourse.bass as bass
import concourse.mybir as mybir
from concourse.bass_utils import run_bass_kernel_spmd
from contextlib import ExitStack

F32 = mybir.dt.float32
BF16 = mybir.dt.bfloat16
I32 = mybir.dt.int32
AF = mybir.ActivationFunctionType
ALU = mybir.AluOpType
AX = mybir.AxisListType

ENGS = ("pe", "act", "dve", "pool", "sp")
SEM_LIMIT = 30000

S = 4096
D = 1024
NT = S // 128
NIN = 3752
DFF = 4096
EPS = 1e-6
NEG = -1.0e30
KBIS = 13
NSEL = 256


class LT:
    __slots__ = ("name", "last_w", "readers", "sem", "cnt", "dram", "burst", "wtoks")

    def __init__(self, name, dram=False):
        self.name = name
        self.last_w = None
        self.readers = []
        self.sem = None
        self.cnt = 0
        self.dram = dram
        self.burst = []
        self.wtoks = {}


class Op:
    __slots__ = ("eng", "fn", "raw", "oth", "sig", "tok", "isdma", "n", "toks")


class Prog:
    def __init__(self, nc, es):
        self.nc = nc
        self.es = es
        self.ops = []
        self.streams = {e: [] for e in ENGS}
        self.nsem = 0
        self.sem_pool = {}

    def new_sem(self, name):
        self.nsem += 1
        return self.es.enter_context(self.nc.semaphore(f"s{self.nsem}_{name}"))

    def _rec(self, eng, fn, r, w, isdma, n):
        o = Op()
        o.eng = eng; o.fn = fn; o.isdma = isdma; o.n = n; o.sig = False
        o.raw = []; o.oth = []; o.toks = []; o.tok = None
        oi = len(self.ops)
        for t in r:
            if t.dram:
                o.toks.extend(t.wtoks.values())
                t.readers.append(oi)
            else:
                if t.last_w is not None:
                    o.raw.append(t.last_w)
                t.readers.append(oi)
        for t in w:
            if t.dram:
                if t.readers:
                    t.burst = [x for x in t.readers if x != oi]
                    t.readers = []
                o.oth.extend(t.burst)
            else:
                if t.last_w is not None:
                    o.oth.append(t.last_w)
                o.oth.extend(x for x in t.readers if x != oi)
                t.last_w = oi
                t.readers = []
        if isdma:
            dst = w[0]
            key = dst.name
            if dst.dram:
                key = "src_" + [t for t in r if not t.dram][0].name
            ent = self.sem_pool.get(key)
            if ent is None:
                ent = [self.new_sem(key), 0]
                self.sem_pool[key] = ent
            ent[1] += 16 * n
            o.tok = (ent[0], ent[1])
            if dst.dram:
                dst.wtoks[id(ent[0])] = (ent[0], ent[1])
            else:
                dst.sem = ent[0]
                dst.cnt = ent[1]
        self.ops.append(o)
        self.streams[eng].append(oi)
        return oi

    def op(self, eng, fn, r=(), w=()):
        return self._rec(eng, fn, list(r), list(w), False, 0)

    def dma(self, q, fn, r=(), w=(), n=1):
        return self._rec(q, fn, list(r), list(w), True, n)

    def emit(self, final_waits=()):
        nc = self.nc
        ops = self.ops
        for oi, o in enumerate(ops):
            keep = []
            for d in o.raw:
                do = ops[d]
                if (not do.isdma) and (not o.isdma) and do.eng == o.eng and o.eng == "pe":
                    continue
                keep.append(d)
            for d in o.oth:
                do = ops[d]
                if (not do.isdma) and (not o.isdma) and do.eng == o.eng and o.eng == "pe":
                    continue
                keep.append(d)
            o.raw = sorted(set(keep))
            for d in o.raw:
                ops[d].sig = True
        for e in ENGS:
            sem = None
            cnt = 0
            for oi in self.streams[e]:
                o = ops[oi]
                if o.isdma or not o.sig:
                    continue
                if sem is None or cnt >= SEM_LIMIT:
                    sem = self.new_sem("eng_" + e)
                    cnt = 0
                cnt += 1
                o.tok = (sem, cnt)
        stats = {e: [0, 0] for e in ENGS}
        with nc.Block() as block:
            def body(ename):
                def run(eng):
                    known = {}
                    for oi in self.streams[ename]:
                        o = ops[oi]
                        need = {}
                        for d in o.raw:
                            s, v = ops[d].tok
                            if need.get(id(s), (None, 0))[1] < v:
                                need[id(s)] = (s, v)
                        for s, v in o.toks:
                            if need.get(id(s), (None, 0))[1] < v:
                                need[id(s)] = (s, v)
                        for k, (s, v) in need.items():
                            if known.get(k, 0) < v:
                                eng.wait_ge(s, v)
                                known[k] = v
                                stats[ename][1] += 1
                        ins = o.fn(eng)
                        stats[ename][0] += 1
                        if o.isdma:
                            if not isinstance(ins, (list, tuple)):
                                ins = [ins]
                            assert len(ins) == o.n, (len(ins), o.n)
                            for i_ in ins:
                                i_.then_inc(o.tok[0], 16)
                        elif o.sig:
                            ins.then_inc(o.tok[0], 1)
                    if ename == "sp":
                        for t in final_waits:
                            for (s_, v_) in t.wtoks.values():
                                eng.wait_ge(s_, v_)
                return run
            block.tensor(body("pe"))
            block.scalar(body("act"))
            block.vector(body("dve"))
            block.gpsimd(body("pool"))
            block.sync(body("sp"))
        return stats


class Arena:
    def __init__(self, tensor, elem_bytes):
        self.t = tensor
        self.eb = elem_bytes
        self.live = []
        self.bump = 0

    def carve(self, off_elems, shape, dtype, name):
        n = int(np.prod(shape[1:]))
        db = 2 if dtype == BF16 else 4
        start = off_elems * self.eb
        end = start + n * db
        assert end <= self.t.shape[1] * self.eb, (name, end)
        lt = LT(name)
        keep = []
        for (s0, e0, l0) in self.live:
            if s0 < end and start < e0:
                lt.readers.extend(l0.readers)
                if l0.last_w is not None:
                    lt.readers.append(l0.last_w)
                if s0 >= start and e0 <= end:
                    continue
            keep.append((s0, e0, l0))
        keep.append((start, end, lt))
        self.live = keep
        ap = self.t[:, off_elems:off_elems + (end - start) // self.eb]
        if dtype != ap.dtype:
            ap = ap.bitcast(dtype)
        if len(shape) == 3:
            ap = ap.rearrange("p (a b) -> p a b", a=shape[1])
        elif len(shape) == 4:
            ap = ap.rearrange("p (a b c) -> p a b c", a=shape[1], b=shape[2])
        if shape[0] < 128:
            ap = ap[0:shape[0]]
        return ap, lt

    def reset(self):
        self.bump = 0

    def alloc(self, shape, dtype, name):
        db = 2 if dtype == BF16 else 4
        n = int(np.prod(shape[1:])) * db
        n = (n + 3) // 4 * 4
        off = self.bump
        self.bump += n // self.eb
        return self.carve(off, shape, dtype, name)


def t5_thresholds():
    def bucket(n):
        if n < 16:
            return n
        nf = np.float32(max(n, 1))
        v = np.log(nf / np.float32(16)) / np.float32(math.log(128 / 16)) * np.float32(16)
        return min(16 + int(np.float32(v)), 31)
    thr = []
    for j in range(1, 32):
        n = 0
        while bucket(n) < j:
            n += 1
        thr.append(n)
    return thr


def build_nc(layers=(0, 1, 2, 3), final_norm=True, debug=False):
    nc = bass.Bass("TRN2", target_bir_lowering=False)
    dt_in = lambda n, s, d=F32: nc.dram_tensor(n, s, d, kind="ExternalInput").ap()
    x_d = dt_in("x", [S, D])
    pos_d = dt_in("pos", [S], I32)
    NLW = len(layers)
    anw_d = dt_in("attn_norm_w", [NLW, D])
    win_d = dt_in("w_in", [NLW, D, NIN])
    lbl_d = dt_in("hgrn_lb_logits", [4, 512])
    hnw_d = dt_in("hgrn_norm_w", [NLW, 128])
    qnw_d = dt_in("mla_q_norm_w", [NLW, 192])
    wqb_d = dt_in("mla_w_qb", [NLW, 192, 384])
    kvnw_d = dt_in("mla_kv_norm_w", [NLW, 128])
    wkvb_d = dt_in("mla_w_kvb", [NLW, 128, 512])
    iknw_d = dt_in("idx_k_norm_w", [NLW, 64])
    iknb_d = dt_in("idx_k_norm_b", [NLW, 64])
    tab_d = dt_in("rel_bias_table", [32, 4])
    wout_d = dt_in("w_out", [NLW, D, D])
    mnw_d = dt_in("mlp_norm_w", [NLW, D])
    w1_d = dt_in("w_mlp_in", [NLW, D, DFF])
    w2_d = dt_in("w_mlp_out", [NLW, DFF, D])
    fnw_d = dt_in("final_norm_w", [D])
    invf_d = dt_in("c_invf", [128, 16])
    pw2_d = dt_in("c_pw2", [128, KBIS])
    out_d = nc.dram_tensor("out", [S, D], F32, kind="ExternalOutput").ap()
    skind = "ExternalOutput" if debug else "Internal"
    sc = lambda n, s, d: nc.dram_tensor(n, s, d, kind=skind).ap()
    hb0_d = sc("hb0", [S, D], F32)
    hb1_d = sc("hb1", [S, D], F32)
    yh_d = sc("yh", [S, 512], BF16)
    mqT_d = sc("mqT", [96, 4, S], BF16)
    dqT_d = sc("dqT", [128, 2, S], BF16)
    iqT_d = sc("iqT", [128, 4, S], BF16)
    iws_d = sc("iws", [S, 8], F32)
    lbs_d = sc("lbs", [4, 128, 512], F32)
    mix_d = sc("mixdbg", [S, 512], BF16)

    with ExitStack() as es:
        P = Prog(nc, es)
        sbt = lambda n, s, d: es.enter_context(nc.sbuf_tensor(n, s, d))
        AR = Arena(sbt("arena", [128, 75328], BF16), 2)
        TM = Arena(sbt("tmp", [128, 10240], F32), 4)
        pbt = [es.enter_context(nc.psum_tensor(f"pb{k}", [128, 512], F32)) for k in range(8)]
        pb = [t[:] for t in pbt]
        Lpb = [LT(f"pb{k}") for k in range(8)]

        def cst(name, shape, dtype):
            return sbt(name, shape, dtype)[:], LT(name)

        def MM(out, lhsT, rhs, start, stop, r, w):
            P.op("pe", lambda e: e.matmul(out, lhsT=lhsT, rhs=rhs, start=start, stop=stop, skip_group_check=True), r, w)

        def TR(out, in_, ident, r, w):
            P.op("pe", lambda e: e.transpose(out=out, in_=in_, identity=ident), r, w)

        def ACT(out, in_, func, r, w, scale=None, bias=None):
            kw = {}
            if scale is not None:
                kw["scale"] = scale
            if bias is not None:
                kw["bias"] = bias
            P.op("act", lambda e: e.activation(out=out, in_=in_, func=func, **kw), r, w)

        def TS(eng, out, in0, s1, s2, op0, op1, r, w, accum=None):
            kw = {}
            if op1 is not None:
                kw["op1"] = op1
            if accum is not None:
                kw["accum_out"] = accum
            P.op(eng, lambda e: e.tensor_scalar(out=out, in0=in0, scalar1=s1, scalar2=s2, op0=op0, **kw), r, w)

        def TT(eng, out, in0, in1, op, r, w):
            P.op(eng, lambda e: e.tensor_tensor(out=out, in0=in0, in1=in1, op=op), r, w)

        def STT(out, in0, scalar, in1, op0, op1, r, w):
            P.op("dve", lambda e: e.scalar_tensor_tensor(out=out, in0=in0, scalar=scalar, in1=in1, op0=op0, op1=op1), r, w)

        def TTR(out, in0, in1, accum, r, w):
            P.op("dve", lambda e: e.scalar_tensor_tensor(out=out, in0=in0, scalar=1.0, in1=in1, op0=ALU.mult,
                                                         op1=ALU.mult, accum_out=accum), r, w)

        def RSTD(out, ss, tmp, inv_w, r, w):
            ACT(tmp, ss, AF.Ln, r + [Lcpi], w, scale=inv_w, bias=cpi[:, 1:2])
            ACT(out, tmp, AF.Exp, w, w, scale=-0.5)

        def RED(out, in_, op, r, w):
            P.op("dve", lambda e: e.tensor_reduce(out=out, in_=in_, axis=AX.X, op=op), r, w)

        def RCP(out, in_, r, w):
            P.op("dve", lambda e: e.reciprocal(out=out, in_=in_), r, w)

        def CP(eng, out, in_, r, w):
            P.op(eng, lambda e: e.tensor_copy(out=out, in_=in_), r, w)

        def MS(eng, ap, val, w):
            P.op(eng, lambda e: e.memset(ap, val), (), w)

        def ASEL(out, in_, pattern, cmp, fill, base, cm, r, w):
            P.op("pool", lambda e: e.affine_select(out=out, in_=in_, pattern=pattern, compare_op=cmp, fill=fill,
                                                   base=base, channel_multiplier=cm), r, w)

        def DMA(q, out, in_, r, w):
            P.dma(q, lambda e: e.dma_start(out=out, in_=in_), r, w)

        def DMAS(q, out, in_, r, w):
            P.dma(q, lambda e: e.dma_start(out=out, in_=in_, allow_slow_non_contiguous=True), r, w)

        Lhb0 = LT("hb0", True); Lhb1 = LT("hb1", True); Lyh = LT("yh", True)
        LmqT = LT("mqT", True); LdqT = LT("dqT", True); LiqT = LT("iqT", True)
        Liws = LT("iws", True); Llbs = LT("lbs", True); Lout = LT("out", True); Lmixd = LT("mixdbg", True)

        idf, Lidf = cst("idf", [128, 128], F32)
        idb, Lidb = cst("idb", [128, 128], BF16)
        Bd, LBd = cst("Bd", [128, 128], F32)
        Lc, LLc = cst("Lc", [128, 128], F32)
        Uc, LUc = cst("Uc", [128, 128], F32)
        Ind, LInd = cst("Ind", [128, 4], F32)
        cosT, Lcos = cst("cosT", [128, NT, 16], F32)
        sinT, Lsin = cst("sinT", [128, NT, 16], F32)
        invf, Linvf = cst("invf", [128, 16], F32)
        pw2, Lpw2 = cst("pw2", [128, KBIS], F32)
        tabb, Ltabb = cst("tabb", [128, 128], F32)
        dlt, Ldlt = cst("dlt", [128, 124], F32)
        biasN, LbiasN = cst("biasN", [128, 4, 256], F32)
        biasN8, LbiasN8 = cst("biasN8", [128, 4, 256], BF16)
        lbB, LlbB = cst("lbB", [128, 512], F32)
        omlbB, LomlbB = cst("omlbB", [128, 512], F32)
        ncol, Lncol = cst("ncol", [128, 24], F32)
        cpi, Lcpi = cst("cpi", [128, 2], F32)
        vst, Lvst = cst("vst", [24, 128], F32)

        MS("pool", idf, 0.0, [Lidf])
        ASEL(idf, idf, [[-1, 128]], ALU.not_equal, 1.0, 0, 1, [Lidf], [Lidf])
        CP("dve", idb, idf, [Lidf], [Lidb])
        MS("pool", Bd, 1.0, [LBd])
        for c in range(4):
            v = Bd[:, 32 * c:32 * c + 32]
            ASEL(v, v, [[0, 32]], ALU.is_ge, 0.0, -32 * c, 1, [LBd], [LBd])
            ASEL(v, v, [[0, 32]], ALU.is_ge, 0.0, 32 * c + 31, -1, [LBd], [LBd])
        ASEL(Lc, Bd, [[1, 128]], ALU.is_ge, 0.0, 0, -1, [LBd], [LLc])
        TT("dve", Uc, Bd, Lc, ALU.subtract, [LBd, LLc], [LUc])
        for c in range(4):
            CP("dve", Ind[:, c:c + 1], Bd[:, 32 * c:32 * c + 1], [LBd], [LInd])
        cm01f, Lcm01f = cst("cm01f", [128, 128], F32)
        cm01, Lcm01 = cst("cm01", [128, 128], BF16)
        cmNEG, LcmNEG = cst("cmNEG", [128, 128], F32)
        MS("pool", cm01f, 1.0, [Lcm01f])
        ASEL(cm01f, cm01f, [[-1, 128]], ALU.is_ge, 0.0, 0, 1, [Lcm01f], [Lcm01f])
        CP("dve", cm01, cm01f, [Lcm01f], [Lcm01])
        cm01Tf, Lcm01Tf = cst("cm01Tf", [128, 128], F32)
        cm01T, Lcm01T = cst("cm01T", [128, 128], BF16)
        MS("pool", cm01Tf, 1.0, [Lcm01Tf])
        ASEL(cm01Tf, cm01Tf, [[1, 128]], ALU.is_ge, 0.0, 0, -1, [Lcm01Tf], [Lcm01Tf])
        CP("dve", cm01T, cm01Tf, [Lcm01Tf], [Lcm01T])
        MS("pool", cmNEG, 0.0, [LcmNEG])
        ASEL(cmNEG, cmNEG, [[-1, 128]], ALU.is_ge, NEG, 0, 1, [LcmNEG], [LcmNEG])
        MS("pool", cpi[:, 0:1], math.pi, [Lcpi])
        MS("pool", cpi[:, 1:2], EPS, [Lcpi])
        DMA("sp", invf, invf_d, [], [Linvf])
        DMA("sp", pw2, pw2_d, [], [Lpw2])
        DMA("sp", tabb, tab_d.rearrange("a b -> (a b)").partition_broadcast(128), [], [Ltabb])
        TT("dve", dlt, tabb[:, 4:128], tabb[:, 0:124], ALU.subtract, [Ltabb], [Ldlt])

        TM.reset()
        posi, Lposi = TM.alloc([128, NT], I32, "posi")
        posf, Lposf = TM.alloc([128, NT], F32, "posf")
        ang, Lang = TM.alloc([128, NT, 16], F32, "ang")
        ang2, Lang2 = TM.alloc([128, NT, 16], F32, "ang2")
        posr, Lposr = TM.alloc([NT, 128], I32, "posr")
        posrf, Lposrf = TM.alloc([NT, 128], F32, "posrf")
        DMA("sp", posr, pos_d.rearrange("(n p) -> n p", p=128), [], [Lposr])
        CP("dve", posrf, posr, [Lposr], [Lposrf])
        TR(pb[0][:, 0:NT], posrf, idf[0:NT, 0:NT], [Lposrf, Lidf], [Lpb[0]])
        CP("dve", posf, pb[0][:, 0:NT], [Lpb[0]], [Lposf])
        TT("dve", ang, posf.unsqueeze(2).to_broadcast([128, NT, 16]),
           invf.unsqueeze(1).to_broadcast([128, NT, 16]), ALU.mult, [Lposf, Linvf], [Lang])
        TWO_PI = 2.0 * math.pi
        angi, Langi = TM.alloc([128, NT, 16], I32, "angi")
        angk, Langk = TM.alloc([128, NT, 16], F32, "angk")
        TS("dve", ang2, ang, math.pi / 2, None, ALU.add, None, [Lang], [Lang2])

        def reduce_pi(a, La):
            TS("dve", angk, a, 1.0 / TWO_PI, None, ALU.mult, None, [La], [Langk])
            CP("dve", angi, angk, [Langk], [Langi])
            CP("dve", angk, angi, [Langi], [Langk])
            STT(a, angk, -TWO_PI, a, ALU.mult, ALU.add, [Langk, La], [La])
            TS("dve", angk, a, math.pi, -TWO_PI, ALU.is_gt, ALU.mult, [La], [Langk])
            TT("dve", a, a, angk, ALU.add, [La, Langk], [La])
            TS("dve", angk, a, -math.pi, TWO_PI, ALU.is_lt, ALU.mult, [La], [Langk])
            TT("dve", a, a, angk, ALU.add, [La, Langk], [La])
        reduce_pi(ang, Lang)
        reduce_pi(ang2, Lang2)
        ACT(sinT, ang, AF.Sin, [Lang], [Lsin])
        ACT(cosT, ang2, AF.Sin, [Lang2], [Lcos])

        dti, Ldti = TM.alloc([128, 256], I32, "dti")
        dtf, Ldtf = TM.alloc([128, 256], F32, "dtf")
        stp, Lstp = TM.alloc([128, 256], F32, "stp")
        P.op("pool", lambda e: e.iota(dti[:, 0:128], pattern=[[-1, 128]], base=128, channel_multiplier=1), [], [Ldti])
        P.op("pool", lambda e: e.iota(dti[:, 128:256], pattern=[[-1, 128]], base=0, channel_multiplier=1), [Ldti], [Ldti])
        CP("dve", dtf, dti, [Ldti], [Ldtf])
        for h in range(4):
            CP("dve", biasN[:, h, :], tabb[:, h:h + 1].to_broadcast([128, 256]), [Ltabb], [LbiasN])
        thr = t5_thresholds()
        for j in range(1, 32):
            TS("dve", stp, dtf, float(thr[j - 1]) - 0.5, None, ALU.is_ge, None, [Ldtf], [Lstp])
            for h in range(4):
                STT(biasN[:, h, :], stp, dlt[:, (j - 1) * 4 + h:(j - 1) * 4 + h + 1], biasN[:, h, :],
                    ALU.mult, ALU.add, [Lstp, Ldlt, LbiasN], [LbiasN])

        TS("dve", biasN8, biasN, 8.0, None, ALU.mult, None, [LbiasN], [LbiasN8])
        lg, Llg = TM.alloc([128, 4, 512], F32, "lg")
        lsum, Llsum = TM.alloc([128, 512], F32, "lsum")
        lacc, Llacc = TM.alloc([128, 512], F32, "lacc")
        DMA("sp", lg, lbl_d.rearrange("a b -> (a b)").partition_broadcast(128).rearrange("p (a b) -> p a b", a=4),
            [], [Llg])
        ACT(lg, lg, AF.Exp, [Llg], [Llg])
        TT("dve", lsum, lg[:, 0, :], lg[:, 1, :], ALU.add, [Llg], [Llsum])
        TT("dve", lsum, lsum, lg[:, 2, :], ALU.add, [Llg, Llsum], [Llsum])
        TT("dve", lsum, lsum, lg[:, 3, :], ALU.add, [Llg, Llsum], [Llsum])
        RCP(lsum, lsum, [Llsum], [Llsum])
        MS("dve", lacc, 0.0, [Llacc])
        for l in range(4):
            if l > 0:
                TT("dve", lg[:, l, :], lg[:, l, :], lsum, ALU.mult, [Llg, Llsum], [Llg])
                TT("dve", lacc, lacc, lg[:, l, :], ALU.add, [Llacc, Llg], [Llacc])
            DMA("sp", lbs_d[l], lacc, [Llacc], [Llbs])

        ISQ = 128.0 ** -0.5

        def rms_to_uT(xt, Lxt, width_chunks, ub, Lub, uT, LuT, bank, Lbank, stat, Lstat, junk, Ljunk):
            W = width_chunks * 128
            TTR(junk[:, 0:W], xt, xt, stat[:, 0:1], [Lxt], [Ljunk, Lstat])
            RSTD(stat[:, 2:3], stat[:, 0:1], stat[:, 1:2], 1.0 / W, [Lstat], [Lstat])
            ACT(ub, xt, AF.Copy, [Lxt, Lstat], [Lub], scale=stat[:, 2:3])
            bb = bank.bitcast(BF16)
            for kc in range(width_chunks):
                TR(bb[:, kc * 128:(kc + 1) * 128], ub[:, kc * 128:(kc + 1) * 128], idb, [Lub, Lidb], [Lbank])
            CP("dve", uT, bb[:, 0:W].rearrange("p (a b) -> p a b", a=width_chunks), [Lbank], [LuT])

        def load_w_rows(dst, Ldst, src2d, nrows_chunks, col0, ncols, scale_col=None, Lsc=None, q="pool"):
            P.dma(q, lambda e: [e.dma_start(out=dst[:, kc, :], in_=src2d[kc * 128:(kc + 1) * 128, col0:col0 + ncols])
                                for kc in range(nrows_chunks)], [], [Ldst], n=nrows_chunks)
            if scale_col is not None:
                for kc in range(nrows_chunks):
                    TS("dve", dst[:, kc, :], dst[:, kc, :], scale_col[:, kc:kc + 1], None, ALU.mult, None,
                       [Ldst, Lsc], [Ldst])

        for li, l in enumerate(layers):
            first = (li == 0)
            last = (li == len(layers) - 1)
            src_d = x_d if first else hb0_d
            Lsrc = None if first else Lhb0
            rsrc = [] if first else [Lhb0]

            MS("pool", vst, 0.0, [Lvst])
            DMA("sp", vst[0:8, :], anw_d[li].rearrange("(k p) -> k p", p=128), [], [Lvst])
            DMA("sp", vst[8:16, :], mnw_d[li].rearrange("(k p) -> k p", p=128), [], [Lvst])
            DMA("sp", vst[16:17, :], qnw_d[li, 0:128].rearrange("(k p) -> k p", p=128), [], [Lvst])
            DMA("sp", vst[17:18, 0:64], qnw_d[li, 128:192].rearrange("(k p) -> k p", p=64), [], [Lvst])
            DMA("sp", vst[18:19, :], kvnw_d[li].rearrange("(k p) -> k p", p=128), [], [Lvst])
            DMA("sp", vst[19:20, :], hnw_d[li].rearrange("(k p) -> k p", p=128), [], [Lvst])
            DMA("sp", vst[20:21, 0:64], iknw_d[li].rearrange("(k p) -> k p", p=64), [], [Lvst])
            DMA("sp", vst[20:21, 64:128], iknw_d[li].rearrange("(k p) -> k p", p=64), [], [Lvst])
            DMA("sp", vst[21:22, 0:64], iknb_d[li].rearrange("(k p) -> k p", p=64), [], [Lvst])
            DMA("sp", vst[21:22, 64:128], iknb_d[li].rearrange("(k p) -> k p", p=64), [], [Lvst])
            TR(pb[0][:, 0:24], vst, idf[0:24, 0:24], [Lvst, Lidf], [Lpb[0]])
            CP("dve", ncol, pb[0][:, 0:24], [Lpb[0]], [Lncol])
            DMA("sp", lbB, lbs_d[l], [Llbs], [LlbB])
            TS("dve", omlbB, lbB, -1.0, 1.0, ALU.mult, ALU.add, [LlbB], [LomlbB])

            winH, LwinH = AR.carve(45312, [128, 8, 2048], BF16, "winH")
            load_w_rows(winH, LwinH, win_d[li], 8, 0, 2048, ncol[:, 0:8], Lncol)
            TM.reset()
            xts = [TM.alloc([128, D], F32, f"xt{k}") for k in range(3)]
            ub, Lub = TM.alloc([128, D], BF16, "ub")
            uTs = [TM.alloc([128, 8, 128], BF16, f"uT{k}") for k in range(3)]
            stats_ = [TM.alloc([128, 16], F32, f"stat{k}") for k in range(2)]
            statF = [TM.alloc([128, 4], F32, f"statF{k}") for k in range(3)]
            AR.bump = 0
            P3 = [AR.alloc([128, 2048], F32, f"Hprf{k}") for k in range(3)]
            HS = []
            for k in range(2):
                d_ = {}
                for nm, shp, dt_ in [("tA", [128, 512], F32),
                                     ("tB", [128, 512], F32), ("tC", [128, 512], F32), ("logf", [128, 512], F32),
                                     ("kk", [128, 512], F32), ("sil", [128, 512], F32), ("dcy", [128, 16], F32),
                                     ("qd", [128, 512], BF16), ("ki", [128, 512], BF16), ("ks", [128, 4, 512], BF16),
                                     ("vb", [128, 512], BF16), ("kiT", [128, 4, 128], BF16), ("qdT", [128, 4, 128], BF16),
                                     ("ATb", [128, 4, 128], BF16), ("yht", [128, 512], BF16),
                                     ("qdTc0", [128, 4, 128], BF16), ("qdTc1", [128, 4, 128], BF16),
                                     ("qdTc2", [128, 4, 128], BF16), ("qdTc3", [128, 4, 128], BF16)]:
                    d_[nm] = AR.alloc(shp, dt_, f"H{nm}{k}")
                HS.append(d_)
                for c in range(4):
                    MS("pool", d_[f"qdTc{c}"][0], 0.0, [d_[f"qdTc{c}"][1]])
            Sf3, LSf3 = AR.alloc([128, 4, 128], F32, "Sf3")
            Sb3, LSb3 = AR.alloc([128, 4, 128], BF16, "Sb3")
            MS("pool", Sf3, 0.0, [LSf3])
            MS("pool", Sb3, 0.0, [LSb3])

            def H_front(i):
                xt, Lxt = xts[i % 3]
                uT, LuT = uTs[i % 3]
                st_, Lst_ = statF[i % 3]
                prf, Lprf = P3[i % 3]
                DMA("sp", xt, src_d[i * 128:(i + 1) * 128, :], rsrc, [Lxt])
                rms_to_uT(xt, Lxt, 8, ub, Lub, uT, LuT, pb[0], Lpb[0], st_, Lst_, prf, Lprf)
                for n in range(4):
                    bk = 1 + (n % 2)
                    for kc in range(8):
                        MM(pb[bk], uT[:, kc, :], winH[:, kc, n * 512:(n + 1) * 512], kc == 0, kc == 7,
                           [LuT, LwinH], [Lpb[bk]])
                    ACT(prf[:, n * 512:(n + 1) * 512], pb[bk], AF.Copy, [Lpb[bk]], [Lprf])

            def H_get(i):
                hs = HS[i % 2]
                return hs

            def H_mid(i, part):
                hs = HS[i % 2]
                prf, Lprf = P3[i % 3]; tA, LtA = hs["tA"]; tB, LtB = hs["tB"]; tC, LtC = hs["tC"]
                logf, Llogf = hs["logf"]; kk, Lkk = hs["kk"]; sil, Lsil = hs["sil"]; dcy, Ldcy = hs["dcy"]
                qd, Lqd = hs["qd"]; ki, Lki = hs["ki"]; ks, Lks = hs["ks"]; vb, Lvb = hs["vb"]
                kiT, LkiT = hs["kiT"]; qdT, LqdT = hs["qdT"]; ATb, LATb = hs["ATb"]
                qdTc = [hs[f"qdTc{c}"] for c in range(4)]
                hq = prf[:, 0:512]; hf = prf[:, 512:1024]; hi = prf[:, 1024:1536]; hg = prf[:, 1536:2048]
                if part == 0:
                    ACT(tA, hf, AF.Sigmoid, [Lprf], [LtA])
                    ACT(sil, hg, AF.Silu, [Lprf], [Lsil])
                    TT("dve", tA, tA, omlbB, ALU.mult, [LtA, LomlbB], [LtA])
                    TT("dve", tA, tA, lbB, ALU.add, [LtA, LlbB], [LtA])
                    TS("dve", kk, tA, -1.0, 1.0, ALU.mult, ALU.add, [LtA], [Lkk])
                    ACT(logf, tA, AF.Ln, [LtA], [Llogf])
                    MM(pb[3], Lc, logf, True, True, [LLc, Llogf], [Lpb[3]])
                    MM(pb[4], Uc, logf, True, True, [LUc, Llogf], [Lpb[4]])
                    for h in range(4):
                        MM(pb[5][:, h * 4:(h + 1) * 4], logf[:, h * 128:(h + 1) * 128], Ind, True, True,
                           [Llogf, LInd], [Lpb[5]])
                elif part == 1:
                    ACT(dcy, pb[5][:, 0:16], AF.Exp, [Lpb[5]], [Ldcy])
                    ACT(tB, pb[3], AF.Exp, [Lpb[3]], [LtB])
                    STT(qd, hq, ISQ, tB, ALU.mult, ALU.mult, [Lprf, LtB], [Lqd])
                    ACT(tC, pb[3], AF.Exp, [Lpb[3]], [LtC], scale=-1.0)
                    TT("dve", ki, kk, tC, ALU.mult, [Lkk, LtC], [Lki])
                    ACT(tB, pb[4], AF.Exp, [Lpb[4]], [LtB])
                    TT("dve", tC, kk, tB, ALU.mult, [Lkk, LtB], [LtC])
                    for c in range(4):
                        ACT(ks[:, c, :], tC, AF.Copy, [LtC, LInd], [Lks], scale=Ind[:, c:c + 1])
                    ACT(vb, hi, AF.Copy, [Lprf], [Lvb])
                elif part == 2:
                    b6_ = pb[6].bitcast(BF16)
                    for h in range(4):
                        TR(b6_[:, h * 128:(h + 1) * 128], qd[:, h * 128:(h + 1) * 128], idb, [Lqd, Lidb], [Lpb[6]])
                        TR(b6_[:, 512 + h * 128:512 + (h + 1) * 128], ki[:, h * 128:(h + 1) * 128], idb, [Lki, Lidb], [Lpb[6]])
                    b6q = b6_[:, 0:512].rearrange("p (a b) -> p a b", a=4)
                    CP("dve", qdT, b6q, [Lpb[6]], [LqdT])
                    CP("dve", kiT, b6_[:, 512:1024].rearrange("p (a b) -> p a b", a=4), [Lpb[6]], [LkiT])
                    for c in range(4):
                        ACT(qdTc[c][0][:, :, 32 * c:32 * c + 32], qdT[:, :, 32 * c:32 * c + 32], AF.Copy, [LqdT], [qdTc[c][1]])
                else:
                    for h in range(4):
                        MM(pb[3][:, h * 128:(h + 1) * 128], kiT[:, h, :], qdT[:, h, :], True, True, [LkiT, LqdT], [Lpb[3]])
                    TT("dve", ATb, pb[3].rearrange("p (a b) -> p a b", a=4), Lc.unsqueeze(1).to_broadcast([128, 4, 128]),
                       ALU.mult, [Lpb[3], LLc], [LATb])

            def H_tail(i, c):
                hs = HS[i % 2]
                dcy, Ldcy = hs["dcy"]; ks, Lks = hs["ks"]; vb, Lvb = hs["vb"]; ATb, LATb = hs["ATb"]
                qdTc = [hs[f"qdTc{cc}"] for cc in range(4)]
                dcy3 = dcy.rearrange("p (a b) -> p a b", a=4)
                if c == 0:
                    for h in range(4):
                        MM(pb[7][:, h * 128:(h + 1) * 128], ATb[:, h, :], vb[:, h * 128:(h + 1) * 128], h == 0, False,
                           [LATb, Lvb], [Lpb[7]])
                for h in range(4):
                    MM(pb[7][:, h * 128:(h + 1) * 128], qdTc[c][0][:, h, :], Sb3[:, h, :], False, c == 3,
                       [qdTc[c][1], LSb3], [Lpb[7]])
                for h in range(4):
                    MM(pb[1][:, h * 128:(h + 1) * 128], ks[:, c, h * 128:(h + 1) * 128], vb[:, h * 128:(h + 1) * 128],
                       True, True, [Lks, Lvb], [Lpb[1]])
                TT("dve", Sf3, Sf3, dcy3[:, :, c:c + 1].to_broadcast([128, 4, 128]), ALU.mult, [LSf3, Ldcy], [LSf3])
                TT("dve", Sf3, Sf3, pb[1].rearrange("p (a b) -> p a b", a=4), ALU.add, [LSf3, Lpb[1]], [LSf3])
                CP("dve", Sb3, Sf3, [LSf3], [LSb3])

            def H_out(i):
                st_, Lst_ = stats_[i % 2]
                hs = HS[i % 2]
                tC, LtC = hs["tC"]; tB, LtB = hs["tB"]; sil, Lsil = hs["sil"]; yht, Lyht = hs["yht"]
                ACT(tB, pb[7], AF.Copy, [Lpb[7]], [LtB])
                for h in range(4):
                    TTR(tC[:, h * 128:(h + 1) * 128], tB[:, h * 128:(h + 1) * 128], tB[:, h * 128:(h + 1) * 128],
                        st_[:, 4 + h:5 + h], [LtB], [LtC, Lst_])
                RSTD(st_[:, 12:16], st_[:, 4:8], st_[:, 8:12], 1.0 / 128, [Lst_], [Lst_])
                for h in range(4):
                    STT(yht[:, h * 128:(h + 1) * 128], tB[:, h * 128:(h + 1) * 128], st_[:, 12 + h:13 + h],
                        sil[:, h * 128:(h + 1) * 128], ALU.mult, ALU.mult, [LtB, Lst_, Lsil], [Lyht])
                DMA("sp", yh_d[i * 128:(i + 1) * 128, :], yht, [Lyht], [Lyh])

            H_front(0)
            if NT > 1:
                H_front(1)
            for p_ in range(4):
                H_mid(0, p_)
            for i in range(NT):
                if i + 2 < NT:
                    H_front(i + 2)
                for c in range(4):
                    if i + 1 < NT:
                        H_mid(i + 1, c)
                    H_tail(i, c)
                H_out(i)

            if debug == "H":
                break
            mkT, LmkT = AR.carve(0, [128, 4, S], BF16, "mkT")
            mvc, Lmvc = AR.carve(16384, [128, NT, 4, 65], BF16, "mvc")
            dkT, LdkT = AR.carve(24704, [128, 2, S], BF16, "dkT")
            dvc, Ldvc = AR.carve(32896, [128, NT, 4, 65], BF16, "dvc")
            kiT2, LkiT2 = AR.carve(41216, [128, S], BF16, "kiT2")
            wout, Lwout = AR.carve(45312, [128, 8, D], BF16, "wout")
            winK, LwinK = AR.carve(53504, [128, 8, 1704], BF16, "winK")
            wqb, Lwqb = AR.carve(67136, [128, 2, 384], BF16, "wqb")
            wkvb, Lwkvb = AR.carve(67904, [128, 512], BF16, "wkvb")
            load_w_rows(winK, LwinK, win_d[li], 8, 2048, 1704, ncol[:, 0:8], Lncol)
            DMA("pool", wqb[:, 0, :], wqb_d[li, 0:128, :], [], [Lwqb])
            DMA("pool", wqb[0:64, 1, :], wqb_d[li, 128:192, :], [], [Lwqb])
            TS("dve", wqb[:, 0, :], wqb[:, 0, :], ncol[:, 16:17], None, ALU.mult, None, [Lwqb, Lncol], [Lwqb])
            TS("dve", wqb[0:64, 1, :], wqb[0:64, 1, :], ncol[0:64, 17:18], None, ALU.mult, None, [Lwqb, Lncol], [Lwqb])
            DMA("pool", wkvb, wkvb_d[li], [], [Lwkvb])
            TS("dve", wkvb, wkvb, ncol[:, 18:19], None, ALU.mult, None, [Lwkvb, Lncol], [Lwkvb])
            load_w_rows(wout, Lwout, wout_d[li], 8, 0, D)
            for kc in range(4):
                TS("dve", wout[:, kc, :], wout[:, kc, :], ncol[:, 19:20], None, ALU.mult, None, [Lwout, Lncol], [Lwout])
            MS("pool", mvc[:, :, :, 64:65], 1.0, [Lmvc])
            MS("pool", dvc[:, :, :, 64:65], 1.0, [Ldvc])

            TM.reset()
            xts = [TM.alloc([128, D], F32, f"xt{k}") for k in range(2)]
            ub, Lub = TM.alloc([128, D], BF16, "ub")
            uTs = [TM.alloc([128, 8, 128], BF16, f"uT{k}") for k in range(2)]
            KS = []
            for k in range(2):
                d_ = {}
                for nm, shp, dt_ in [("stat", [128, 16], F32), ("junk", [128, D], F32), ("dqs", [128, 2, 128], BF16),
                                     ("iqs", [128, 4, 128], BF16), ("mqs", [128, 4, 128], BF16), ("iwt", [128, 8], F32),
                                     ("ikx", [128, 64], F32), ("cen", [128, 64], F32), ("kin2", [128, 128], BF16),
                                     ("qn", [128, 192], BF16), ("qnT", [128, 2, 128], BF16), ("kvn", [128, 128], BF16),
                                     ("kvnT", [128, 128], BF16), ("qfull", [128, 4, 96], BF16), ("kpe", [128, 96], BF16),
                                     ("r1", [128, 4, 16], F32), ("r2", [128, 4, 16], F32)]:
                    d_[nm] = TM.alloc(shp, dt_, f"K{nm}{k}")
                KS.append(d_)
                MS("pool", d_["kpe"][0], 0.0, [d_["kpe"][1]])

            def rope(ks_, dst1, dst2, x1, x2, i, nh, rr, ww):
                r1, Lr1 = ks_["r1"]; r2, Lr2 = ks_["r2"]
                cs = cosT[:, i, :].unsqueeze(1).to_broadcast([128, nh, 16])
                sn = sinT[:, i, :].unsqueeze(1).to_broadcast([128, nh, 16])
                a1 = r1[:, 0:nh, :]; a2 = r2[:, 0:nh, :]
                TT("dve", a1, x1, cs, ALU.mult, rr + [Lcos], [Lr1])
                TT("dve", a2, x2, sn, ALU.mult, rr + [Lsin], [Lr2])
                TT("dve", dst1, a1, a2, ALU.subtract, [Lr1, Lr2], ww)
                TT("dve", a1, x2, cs, ALU.mult, rr + [Lcos], [Lr1])
                TT("dve", a2, x1, sn, ALU.mult, rr + [Lsin], [Lr2])
                TT("dve", dst2, a1, a2, ALU.add, [Lr1, Lr2], ww)

            b7 = pb[7].bitcast(BF16)
            b5 = pb[5].bitcast(BF16)

            def K_front(i):
                ks_ = KS[i % 2]
                xt, Lxt = xts[i % 2]
                uT, LuT = uTs[i % 2]
                stat, Lstat = ks_["stat"]; junk, Ljunk = ks_["junk"]
                dqs, Ldqs = ks_["dqs"]; iqs, Liqs = ks_["iqs"]; iwt, Liwt = ks_["iwt"]; ikx, Likx = ks_["ikx"]
                tc = slice(i * 128, (i + 1) * 128)
                DMA("sp", xt, src_d[tc, :], rsrc, [Lxt])
                rms_to_uT(xt, Lxt, 8, ub, Lub, uT, LuT, pb[0], Lpb[0], stat, Lstat, junk, Ljunk)
                for kc in range(8):
                    MM(pb[1][:, 0:352], uT[:, kc, :], winK[:, kc, 0:352], kc == 0, kc == 7, [LuT, LwinK], [Lpb[1]])
                for kc in range(8):
                    MM(pb[2][:, 0:256], uT[:, kc, :], winK[:, kc, 864:1120], kc == 0, kc == 7, [LuT, LwinK], [Lpb[2]])
                for kc in range(8):
                    MM(pb[2][:, 256:328], uT[:, kc, :], winK[:, kc, 1632:1704], kc == 0, kc == 7, [LuT, LwinK], [Lpb[2]])
                for c in range(4):
                    for kc in range(8):
                        MM(pb[3][:, c * 128:(c + 1) * 128], winK[:, kc, 352 + c * 128:352 + (c + 1) * 128], uT[:, kc, :],
                           kc == 0, kc == 7, [LuT, LwinK], [Lpb[3]])
                for c in range(4):
                    for kc in range(8):
                        MM(pb[4][:, c * 128:(c + 1) * 128], winK[:, kc, 1120 + c * 128:1120 + (c + 1) * 128], uT[:, kc, :],
                           kc == 0, kc == 7, [LuT, LwinK], [Lpb[4]])
                ACT(junk[:, 0:352], pb[1][:, 0:352], AF.Copy, [Lpb[1]], [Ljunk])
                CP("dve", dqs, pb[3][:, 0:256].rearrange("p (a b) -> p a b", a=2), [Lpb[3]], [Ldqs])
                DMA("sp", dqT_d[:, :, tc], dqs, [Ldqs], [LdqT])
                CP("dve", dkT[:, :, tc], pb[3][:, 256:512].rearrange("p (a b) -> p a b", a=2), [Lpb[3]], [LdkT])
                ACT(iqs, pb[4].rearrange("p (a b) -> p a b", a=4), AF.Copy, [Lpb[4]], [Liqs])
                DMA("sp", iqT_d[:, :, tc], iqs, [Liqs], [LiqT])
                CP("dve", dvc[:, i, :, 0:64], pb[2][:, 0:256].rearrange("p (a b) -> p a b", a=4), [Lpb[2]], [Ldvc])
                CP("dve", iwt, pb[2][:, 320:328], [Lpb[2]], [Liwt])
                DMA("sp", iws_d[tc, :], iwt, [Liwt], [Liws])
                CP("dve", ikx, pb[2][:, 256:320], [Lpb[2]], [Likx])

            def K_rest(i):
                ks_ = KS[i % 2]
                stat, Lstat = ks_["stat"]; junk, Ljunk = ks_["junk"]
                mqs, Lmqs = ks_["mqs"]; ikx, Likx = ks_["ikx"]; cen, Lcen = ks_["cen"]; kin2, Lkin2 = ks_["kin2"]
                qn, Lqn = ks_["qn"]; qnT, LqnT = ks_["qnT"]; kvn, Lkvn = ks_["kvn"]; kvnT, LkvnT = ks_["kvnT"]
                qfull, Lqfull = ks_["qfull"]; kpe, Lkpe = ks_["kpe"]
                tc = slice(i * 128, (i + 1) * 128)
                RED(stat[:, 4:5], ikx, ALU.add, [Likx], [Lstat])
                TS("dve", stat[:, 5:6], stat[:, 4:5], -1.0 / 64, None, ALU.mult, None, [Lstat], [Lstat])
                TS("dve", cen, ikx, stat[:, 5:6], None, ALU.add, None, [Likx, Lstat], [Lcen])
                TTR(junk[:, 704:768], cen, cen, stat[:, 6:7], [Lcen], [Ljunk, Lstat])
                RSTD(stat[:, 8:9], stat[:, 6:7], stat[:, 7:8], 1.0 / 64, [Lstat], [Lstat])
                TS("dve", kin2[:, 0:64], cen, stat[:, 8:9], None, ALU.mult, None, [Lcen, Lstat], [Lkin2])
                TS("dve", kin2[:, 64:128], cen, stat[:, 8:9], None, ALU.mult, None, [Lcen, Lstat], [Lkin2])
                TR(b7[:, 0:128], kin2, idb, [Lkin2, Lidb], [Lpb[7]])
                ACT(kiT2[:, tc], b7[:, 0:128], AF.Identity, [Lpb[7], Lncol], [LkiT2], scale=ncol[:, 20:21], bias=ncol[:, 21:22])
                TTR(junk[:, 512:704], junk[:, 0:192], junk[:, 0:192], stat[:, 9:10], [Ljunk], [Ljunk, Lstat])
                RSTD(stat[:, 11:12], stat[:, 9:10], stat[:, 10:11], 1.0 / 192, [Lstat], [Lstat])
                ACT(qn, junk[:, 0:192], AF.Copy, [Ljunk, Lstat], [Lqn], scale=stat[:, 11:12])
                TR(b7[:, 128:256], qn[:, 0:128], idb, [Lqn, Lidb], [Lpb[7]])
                TR(b7[0:64, 256:384], qn[:, 128:192], idb, [Lqn, Lidb], [Lpb[7]])
                CP("dve", qnT[:, 0, :], b7[:, 128:256], [Lpb[7]], [LqnT])
                CP("dve", qnT[0:64, 1, :], b7[0:64, 256:384], [Lpb[7]], [LqnT])
                MM(pb[5][:, 0:384], qnT[:, 0, :], wqb[:, 0, :], True, False, [LqnT, Lwqb], [Lpb[5]])
                MM(pb[5][:, 0:384], qnT[0:64, 1, :], wqb[0:64, 1, :], False, True, [LqnT, Lwqb], [Lpb[5]])
                q3 = pb[5][:, 0:384].rearrange("p (a b) -> p a b", a=4)
                CP("dve", qfull[:, :, 0:64], q3[:, :, 0:64], [Lpb[5]], [Lqfull])
                rope(ks_, qfull[:, :, 64:80], qfull[:, :, 80:96], q3[:, :, 64:80], q3[:, :, 80:96], i, 4, [Lpb[5]], [Lqfull])
                for h in range(4):
                    TR(b7[0:96, 384 + h * 128:384 + (h + 1) * 128], qfull[:, h, :], idb, [Lqfull, Lidb], [Lpb[7]])
                CP("dve", mqs[0:96], b7[0:96, 384:896].rearrange("p (a b) -> p a b", a=4), [Lpb[7]], [Lmqs])
                DMA("sp", mqT_d[:, :, tc], mqs[0:96], [Lmqs], [LmqT])
                TTR(junk[:, 512:640], junk[:, 192:320], junk[:, 192:320], stat[:, 12:13], [Ljunk], [Ljunk, Lstat])
                RSTD(stat[:, 14:15], stat[:, 12:13], stat[:, 13:14], 1.0 / 128, [Lstat], [Lstat])
                ACT(kvn, junk[:, 192:320], AF.Copy, [Ljunk, Lstat], [Lkvn], scale=stat[:, 14:15])
                TR(b7[:, 896:1024], kvn, idb, [Lkvn, Lidb], [Lpb[7]])
                CP("dve", kvnT, b7[:, 896:1024], [Lpb[7]], [LkvnT])
                MM(pb[6], kvnT, wkvb, True, True, [LkvnT, Lwkvb], [Lpb[6]])
                CP("dve", mvc[:, i, :, 0:64], pb[6].rearrange("p (a b) -> p a b", a=4)[:, :, 64:128], [Lpb[6]], [Lmvc])
                for h in range(4):
                    MM(pb[6][0:64, h * 128:(h + 1) * 128], wkvb[:, h * 128:h * 128 + 64], kvnT, True, True,
                       [LkvnT, Lwkvb], [Lpb[6]])
                CP("dve", mkT[0:64, :, tc], pb[6][0:64, :].rearrange("p (a b) -> p a b", a=4), [Lpb[6]], [LmkT])
                rope(ks_, kpe[:, 64:80].unsqueeze(1), kpe[:, 80:96].unsqueeze(1), junk[:, 320:336].unsqueeze(1),
                     junk[:, 336:352].unsqueeze(1), i, 1, [Ljunk], [Lkpe])
                TR(b5[0:96, 768:896], kpe, idb, [Lkpe, Lidb], [Lpb[5]])
                CP("dve", mkT[64:96, :, tc], b5[64:96, 768:896].unsqueeze(1).to_broadcast([32, 4, 128]), [Lpb[5]], [LmkT])

            K_front(0)
            for i in range(NT):
                if i + 1 < NT:
                    K_front(i + 1)
                K_rest(i)

            if debug == "K":
                break
            SCs = [AR.carve(53504 + 8192 * k, [128, S], F32, f"SC{k}") for k in range(2)]
            bjunk, Lbjunk = AR.carve(69888, [128, S], BF16, "bjunk")
            TM.reset()
            xt, Lxt = TM.alloc([128, D], F32, "xtA")
            hm, Lhm = TM.alloc([128, D], F32, "hm")
            mqs2 = [TM.alloc([128, 4, 128], BF16, f"mq{k}") for k in range(2)]
            dqs2 = [TM.alloc([128, 4, 128], BF16, f"dq{k}") for k in range(2)]
            iqs2 = [TM.alloc([128, 8, 128], BF16, f"iq{k}") for k in range(2)]
            for k in range(2):
                MS("pool", dqs2[k][0], 0.0, [dqs2[k][1]])
                MS("pool", iqs2[k][0], 0.0, [iqs2[k][1]])
            iws2 = [TM.alloc([128, 8], F32, f"iw{k}") for k in range(2)]
            aws2 = [TM.alloc([128, 8], F32, f"aw{k}") for k in range(2)]
            sgs2 = [TM.alloc([128, 8], F32, f"sg{k}") for k in range(2)]
            Dh1 = TM.alloc([128, 8, 128], BF16, "Dh0")
            Dhs2 = [Dh1, Dh1]
            Rbs = [TM.alloc([128, 512], BF16, f"Rb{k}") for k in range(3)]
            mbuf, Lmbuf = TM.alloc([128, S], BF16, "mbuf")
            Ebs = [TM.alloc([128, 512], BF16, f"Eb{k}") for k in range(4)]
            thrs = [TM.alloc([128, 1], F32, f"thr{k}") for k in range(2)]
            bs, Lbs = TM.alloc([128, 8], F32, "bs")
            wk, Lwk = TM.alloc([128, KBIS], F32, "wk")
            cand, Lcand = TM.alloc([128, 1], F32, "cand")
            cnt, Lcnt = TM.alloc([128, 1], F32, "cnt")
            dl, Ldl = TM.alloc([128, 1], F32, "dl")
            tt_, Ltt = TM.alloc([128, 1], F32, "tt")
            yht2, Lyht2 = TM.alloc([128, 512], BF16, "yht2")
            mixed, Lmixed = TM.alloc([128, 512], BF16, "mixed")
            mixT, LmixT = TM.alloc([128, 8, 128], BF16, "mixT")
            rec, Lrec = TM.alloc([128, 8], F32, "rec")
            pbL = [0, 1]
            b6 = pb[6].bitcast(BF16)
            b2 = pb[2].bitcast(BF16)
            pTh = [b6[:, 0:512], b2[:, 0:512]]
            LpTh = [Lpb[6], Lpb[2]]
            LO = [Lpb[7]] * 4
            IDX_SCALE = (8.0 ** -0.5) * (64.0 ** -0.5)

            def load_q(i):
                tc = slice(i * 128, (i + 1) * 128)
                k = i % 2
                DMA("sp", mqs2[k][0][0:96], mqT_d[:, :, tc], [LmqT], [mqs2[k][1]])
                for e_ in range(2):
                    DMA("sp", dqs2[k][0][64 * e_:64 * e_ + 64, e_:4:2, :], dqT_d[64 * e_:64 * e_ + 64, :, tc], [LdqT], [dqs2[k][1]])
                    DMA("sp", iqs2[k][0][64 * e_:64 * e_ + 64, e_:8:2, :], iqT_d[64 * e_:64 * e_ + 64, :, tc], [LiqT], [iqs2[k][1]])
                DMA("sp", iws2[k][0], iws_d[tc, :], [Liws], [iws2[k][1]])

            def prep(i):
                k = i % 2
                iw, Liw = iws2[k]; aw, Law = aws2[k]; sg, Lsg = sgs2[k]; Dh, LDh = Dhs2[k]
                STT(aw, iw, -1.0, iw, ALU.mult, ALU.max, [Liw], [Law])
                TS("dve", aw, aw, IDX_SCALE, None, ALU.mult, None, [Law], [Law])
                TS("dve", sg, iw, 0.0, 2.0, ALU.is_ge, ALU.mult, [Liw], [Lsg])
                TS("dve", sg, sg, -1.0, None, ALU.add, None, [Lsg], [Lsg])
                for h in range(8):
                    TS("dve", Dh[:, h, :], idb, sg[:, h:h + 1], None, ALU.mult, None, [Lidb, Lsg], [LDh])

            def indexer(i):
                k = i % 2
                iq, Liq = iqs2[k]; iw, Liw = iws2[k]; aw, Law = aws2[k]; sg, Lsg = sgs2[k]; Dh, LDh = Dhs2[k]
                SC, LSC = SCs[k]
                n = 128 * (i + 1)
                nblk = (n + 511) // 512
                items = [(c, h) for c in range(nblk) for h in range(8)]

                def st0(j):
                    c, h = items[j]
                    w = min(512, n - c * 512)
                    pl = pbL[j % 2]
                    MM(pb[pl][:, 0:w], iq[:, h, :], kiT2[:, c * 512:c * 512 + w], True, True,
                       [Liq, LkiT2], [Lpb[pl]])
                    Rb, LRb = Rbs[j % 3]
                    ACT(Rb[:, 0:w], pb[pl][:, 0:w], AF.Relu, [Lpb[pl], Law], [LRb], scale=aw[:, h:h + 1])

                def st1(j):
                    c, h = items[j]
                    w = min(512, n - c * 512)
                    Rb, LRb = Rbs[j % 3]
                    MM(pb[3][:, 0:w], Dh[:, h, :], Rb[:, 0:w], h == 0, h == 7, [LDh, LRb], [Lpb[3]])
                    if h == 7:
                        ACT(SC[:, c * 512:c * 512 + w], pb[3][:, 0:w], AF.Copy, [Lpb[3]], [LSC])
                for s_ in range(len(items) + 2):
                    if s_ < len(items):
                        st0(s_)
                    if 0 <= s_ - 2 < len(items):
                        st1(s_ - 2)
                TT("pool", SC[:, n - 128:n], SC[:, n - 128:n], cmNEG, ALU.add, [LSC, LcmNEG], [LSC])

            def bisect(i):
                k = i % 2
                SC, LSC = SCs[k]
                thr_, Lthr = thrs[k]
                n = 128 * (i + 1)
                if n <= NSEL:
                    MS("dve", thr_, -1.0e29, [Lthr])
                    return
                RED(bs[:, 0:1], SC[:, 0:128 * i], ALU.min, [LSC], [Lbs])
                RED(bs[:, 1:2], SC[:, 0:n], ALU.max, [LSC], [Lbs])
                TT("dve", bs[:, 2:3], bs[:, 1:2], bs[:, 0:1], ALU.subtract, [Lbs], [Lbs])
                TS("dve", wk, pw2, bs[:, 2:3], None, ALU.mult, None, [Lpw2, Lbs], [Lwk])
                CP("dve", tt_, bs[:, 0:1], [Lbs], [Ltt])
                for kk_ in range(KBIS):
                    TT("dve", cand, tt_, wk[:, kk_:kk_ + 1], ALU.add, [Ltt, Lwk], [Lcand])
                    TS("dve", bjunk[:, 0:n], SC[:, 0:n], cand, 0.0, ALU.is_ge, ALU.add, [LSC, Lcand], [Lbjunk, Lcnt], accum=cnt)
                    TS("dve", dl, cnt, NSEL - 0.5, wk[:, kk_:kk_ + 1], ALU.is_ge, ALU.mult, [Lcnt, Lwk], [Ldl])
                    TT("dve", tt_, tt_, dl, ALU.add, [Ltt, Ldl], [Ltt])
                CP("dve", thr_, tt_, [Ltt], [Lthr])

            def maskbias(i):
                SC, LSC = SCs[i % 2]
                thr_, Lthr = thrs[i % 2]
                n = 128 * (i + 1)
                TS("dve", mbuf[:, 0:n], SC[:, 0:n], thr_[:, 0:1], -30000.0, ALU.is_lt, ALU.mult, [LSC, Lthr], [Lmbuf])

            SB_ = [2, 4, 5, 6]

            def attention(i, kind):
                k = i % 2
                if kind == "mla":
                    q, Lq = mqs2[k]; kc_, Lkc = mkT, LmkT; vc, Lvc = mvc, Lmvc
                    nblk = (i + 4) // 4
                    blocks = [list(range(4 * c, min(4 * c + 4, i + 1))) for c in range(nblk)]
                    scale = 96.0 ** -0.5
                else:
                    q, Lq = dqs2[k]; kc_, Lkc = dkT, LdkT; vc, Lvc = dvc, Ldvc
                    blocks = [list(range(4 * c, min(4 * c + 4, i + 1))) for c in range((i + 4) // 4)]
                    scale = 0.125
                items = [(h, b) for h in range(4) for b in range(len(blocks))]

                def st0(j):
                    h, b = items[j]
                    tiles = blocks[b]
                    w = 128 * len(tiles)
                    pS = SB_[j % 4]
                    Eb, LEb = Ebs[j % 4]
                    for jj, tj in enumerate(tiles):
                        o_ = pb[pS][:, jj * 128:(jj + 1) * 128]
                        ks_ = slice(tj * 128, (tj + 1) * 128)
                        if kind == "mla":
                            MM(o_, kc_[0:96, h, ks_], q[0:96, h, :], True, True, [Lq, Lkc], [Lpb[pS]])
                        else:
                            near = tj >= i - 1
                            MM(o_, kc_[:, h // 2, ks_], q[:, h, :], True, False, [Lq, Lkc], [Lpb[pS]])
                            MM(o_, mbuf[:, ks_], idb, False, not near, [Lidb, Lmbuf], [Lpb[pS]])
                            if near:
                                bo = 128 if tj == i else 0
                                MM(o_, biasN8[:, h, bo:bo + 128], idb, False, True, [Lidb, LbiasN8], [Lpb[pS]])
                    if kind == "mla":
                        ACT(Eb[:, 0:w], pb[pS][:, 0:w], AF.Exp, [Lpb[pS]], [LEb], scale=scale)
                        if tiles[-1] == i:
                            TT("pool", Eb[:, w - 128:w], Eb[:, w - 128:w], cm01T, ALU.mult, [LEb, Lcm01T], [LEb])
                    else:
                        nfar = sum(1 for tj in tiles if tj < i - 1)
                        if nfar > 0:
                            ACT(Eb[:, 0:128 * nfar], pb[pS][:, 0:128 * nfar], AF.Exp, [Lpb[pS], Ltabb], [LEb], scale=scale,
                                bias=tabb[:, 124 + h:125 + h])
                        if nfar < len(tiles):
                            ACT(Eb[:, 128 * nfar:w], pb[pS][:, 128 * nfar:w], AF.Exp, [Lpb[pS]], [LEb], scale=scale)

                def st2(j):
                    h, b = items[j]
                    tiles = blocks[b]
                    Eb, LEb = Ebs[j % 4]
                    for jj, tj in enumerate(tiles):
                        MM(pb[7][:, h * 65:(h + 1) * 65], Eb[:, jj * 128:(jj + 1) * 128], vc[:, tj, h, :],
                           (b == 0 and jj == 0), (b == len(blocks) - 1 and jj == len(tiles) - 1), [LEb, Lvc], [LO[h]])
                for s_ in range(len(items) + 2):
                    if s_ < len(items):
                        st0(s_)
                    if 0 <= s_ - 2 < len(items):
                        st2(s_ - 2)
                off = 0 if kind == "mla" else 256
                ro = 0 if kind == "mla" else 4
                o3 = pb[7][:, 0:260].rearrange("p (a b) -> p a b", a=4)
                ACT(rec[:, ro:ro + 4], o3[:, :, 64], AF.Ln, [LO[0]], [Lrec])
                ACT(rec[:, ro:ro + 4], rec[:, ro:ro + 4], AF.Exp, [Lrec], [Lrec], scale=-1.0)
                for h in range(4):
                    ACT(mixed[:, off + h * 64:off + (h + 1) * 64], pb[7][:, h * 65:h * 65 + 64], AF.Copy,
                        [LO[h], Lrec], [Lmixed], scale=rec[:, ro + h:ro + h + 1])

            def epilogue(i):
                tc = slice(i * 128, (i + 1) * 128)
                DMA("sp", xt, src_d[tc, :], rsrc, [Lxt])
                DMA("sp", yht2, yh_d[tc, :], [Lyh], [Lyht2])
                if debug:
                    DMA("sp", mix_d[tc, :], mixed, [Lmixed], [Lmixd])
                for kc in range(4):
                    TR(b6[:, kc * 128:(kc + 1) * 128], yht2[:, kc * 128:(kc + 1) * 128], idb, [Lyht2, Lidb], [LpTh[0]])
                for kc in range(4):
                    TR(b2[:, kc * 128:(kc + 1) * 128], mixed[:, kc * 128:(kc + 1) * 128], idb, [Lmixed, Lidb], [LpTh[1]])
                ACT(mixT[:, 0:4, :], b6[:, 0:512].rearrange("p (a b) -> p a b", a=4), AF.Copy, [LpTh[0]], [LmixT])
                ACT(mixT[:, 4:8, :], b2[:, 0:512].rearrange("p (a b) -> p a b", a=4), AF.Copy, [LpTh[1]], [LmixT])
                for n_ in range(2):
                    for kc in range(8):
                        MM(pb[4 + n_], mixT[:, kc, :], wout[:, kc, n_ * 512:(n_ + 1) * 512], kc == 0, kc == 7,
                           [LmixT, Lwout], [Lpb[4 + n_]])
                    ACT(hm[:, n_ * 512:(n_ + 1) * 512], pb[4 + n_], AF.Copy, [Lpb[4 + n_]], [Lhm])
                    TT("pool", hm[:, n_ * 512:(n_ + 1) * 512], hm[:, n_ * 512:(n_ + 1) * 512], xt[:, n_ * 512:(n_ + 1) * 512],
                       ALU.add, [Lhm, Lxt], [Lhm])
                DMA("pool", hb1_d[tc, :], hm, [Lhm], [Lhb1])

            load_q(0)
            prep(0)
            indexer(0)
            if NT > 1:
                load_q(1)
            for i in range(NT):
                if i + 1 < NT:
                    prep(i + 1)
                bisect(i)
                if i + 1 < NT:
                    indexer(i + 1)
                attention(i, "mla")
                maskbias(i)
                attention(i, "dsa")
                if i + 2 < NT:
                    load_q(i + 2)
                epilogue(i)
            if debug == "A":
                break
            w1, Lw1 = AR.carve(0, [128, 8, DFF], BF16, "w1")
            w2, Lw2 = AR.carve(32768, [128, 32, D], BF16, "w2")
            hT, LhT = AR.carve(65536, [128, 32, 256], BF16, "hT")
            load_w_rows(w1, Lw1, w1_d[li], 8, 0, DFF, ncol[:, 8:16], Lncol)
            load_w_rows(w2, Lw2, w2_d[li], 32, 0, D)
            TM.reset()
            hts = [TM.alloc([128, D], F32, f"ht{k}") for k in range(4)]
            ub, Lub = TM.alloc([128, D], BF16, "ubM")
            uT2, LuT2 = TM.alloc([128, 8, 256], BF16, "uT2")
            hos = [TM.alloc([128, D], F32, f"ho{k}") for k in range(2)]
            stat, Lstat = TM.alloc([128, 16], F32, "statM")
            junk, Ljunk = TM.alloc([128, D], F32, "junkM")
            sq, Lsq = TM.alloc([128, 256], BF16, "sq")
            sq2, Lsq2 = TM.alloc([128, 256], BF16, "sq2")
            sqs = [(sq, Lsq), (sq2, Lsq2)]
            do_fin = last and final_norm
            if do_fin:
                fnb, Lfnb = TM.alloc([128, D], F32, "fnb")
                DMA("sp", fnb, fnw_d.partition_broadcast(128), [], [Lfnb])
            b0 = pb[0].bitcast(BF16)
            def M_load(st):
                for sub in range(2):
                    i = 2 * st + sub
                    ht, Lht = hts[(st % 2) * 2 + sub]
                    DMA("sp", ht, hb1_d[i * 128:(i + 1) * 128, :], [Lhb1], [Lht])

            M_load(0)
            for st in range(NT // 2):
                if st + 1 < NT // 2:
                    M_load(st + 1)
                for sub in range(2):
                    i = 2 * st + sub
                    ht, Lht = hts[(st % 2) * 2 + sub]
                    TTR(junk, ht, ht, stat[:, 0:1], [Lht], [Ljunk, Lstat])
                    RSTD(stat[:, 2:3], stat[:, 0:1], stat[:, 1:2], 1.0 / D, [Lstat], [Lstat])
                    ACT(ub, ht, AF.Copy, [Lht, Lstat], [Lub], scale=stat[:, 2:3])
                    for kc in range(8):
                        TR(b0[:, kc * 128:(kc + 1) * 128], ub[:, kc * 128:(kc + 1) * 128], idb, [Lub, Lidb], [Lpb[0]])
                    CP("dve", uT2[:, :, sub * 128:(sub + 1) * 128], b0.rearrange("p (a b) -> p a b", a=8), [Lpb[0]], [LuT2])
                for f in range(32):
                    ph = 1 + (f % 3)
                    for kc in range(8):
                        MM(pb[ph][:, 0:256], w1[:, kc, f * 128:(f + 1) * 128], uT2[:, kc, :], kc == 0, kc == 7,
                           [Lw1, LuT2], [Lpb[ph]])
                    s_, Ls_ = sqs[f % 2]
                    ACT(s_, pb[ph][:, 0:256], AF.Relu, [Lpb[ph]], [Ls_])
                    TT("pool", hT[:, f, :], s_, s_, ALU.mult, [Ls_], [LhT])
                for sub in range(2):
                    i = 2 * st + sub
                    ht, Lht = hts[(st % 2) * 2 + sub]
                    ho, Lho = hos[sub]
                    for n_ in range(2):
                        po = 4 + sub * 2 + n_
                        for f in range(32):
                            MM(pb[po], hT[:, f, sub * 128:(sub + 1) * 128], w2[:, f, n_ * 512:(n_ + 1) * 512], f == 0, f == 31,
                               [LhT, Lw2], [Lpb[po]])
                        TT("dve", ho[:, n_ * 512:(n_ + 1) * 512], pb[po], ht[:, n_ * 512:(n_ + 1) * 512], ALU.add,
                           [Lpb[po], Lht], [Lho])
                    if do_fin:
                        TTR(junk, ho, ho, stat[:, 4:5], [Lho], [Ljunk, Lstat])
                        RSTD(stat[:, 6:7], stat[:, 4:5], stat[:, 5:6], 1.0 / D, [Lstat], [Lstat])
                        STT(junk, ho, stat[:, 6:7], fnb, ALU.mult, ALU.mult, [Lho, Lstat, Lfnb], [Ljunk])
                        DMA("sp", out_d[i * 128:(i + 1) * 128, :], junk, [Ljunk], [Lout])
                    else:
                        DMA("sp", (out_d if (last and not debug) else hb0_d)[i * 128:(i + 1) * 128, :], ho, [Lho],
                            [Lout if (last and not debug) else Lhb0])

        final = [Lout] if not debug else [Lyh, Lhb0, Lhb1, LmqT, LdqT, LiqT, Liws, Llbs, Lmixd]
        stats = P.emit(final_waits=final)
        build_nc.stats = stats
    return nc


def _rest_of_layer(env):
    raise NotImplementedError


def host_consts():
    invf = (1.0 / (10000.0 ** (np.arange(0, 32, 2, dtype=np.float32) / np.float32(32)))).astype(np.float32)
    c_invf = np.ascontiguousarray(np.broadcast_to(invf[None, :], (128, 16))).astype(np.float32)
    pw = np.array([2.0 ** -(k + 1) for k in range(KBIS)], dtype=np.float32)
    c_pw2 = np.ascontiguousarray(np.broadcast_to(pw[None, :], (128, KBIS))).astype(np.float32)
    return c_invf, c_pw2


PER_LAYER = ["attn_norm_w", "w_in", "hgrn_norm_w", "mla_q_norm_w", "mla_w_qb", "mla_kv_norm_w", "mla_w_kvb",
             "idx_k_norm_w", "idx_k_norm_b", "w_out", "mlp_norm_w", "w_mlp_in", "w_mlp_out"]
GLOBALS = ["hgrn_lb_logits", "rel_bias_table", "final_norm_w"]
FUSED = True
_NC_CACHE = {}


def _get_nc(layers, final_norm):
    key = (tuple(layers), final_norm)
    if key not in _NC_CACHE:
        _NC_CACHE[key] = build_nc(layers=tuple(layers), final_norm=final_norm, debug=False)
    return _NC_CACHE[key]


def _launch(layers, final_norm, inputs, xs):
    c_invf, c_pw2 = host_consts()
    nc = _get_nc(layers, final_norm)
    shared = {k: np.ascontiguousarray(np.asarray(inputs[k], dtype=np.float32)[list(layers)]) for k in PER_LAYER}
    for k in GLOBALS:
        shared[k] = np.ascontiguousarray(np.asarray(inputs[k], dtype=np.float32))
    pos = np.asarray(inputs["positions"]).astype(np.int32)
    maps = []
    for b in range(8):
        m = dict(shared)
        m["x"] = np.ascontiguousarray(xs[b], dtype=np.float32)
        m["pos"] = np.ascontiguousarray(pos[b])
        m["c_invf"] = c_invf
        m["c_pw2"] = c_pw2
        maps.append(m)
    res = run_bass_kernel_spmd(nc, maps, core_ids=list(range(8)))
    return [np.asarray(res.results[b]["out"], dtype=np.float32) for b in range(8)]


def kernel(**inputs):
    x = np.asarray(inputs["x"], dtype=np.float32)
    xs = [x[b] for b in range(8)]
    if FUSED:
        outs = _launch((0, 1, 2, 3), True, inputs, xs)
    else:
        for l in range(4):
            xs = _launch((l,), l == 3, inputs, xs)
        outs = xs
    return np.stack(outs, axis=0).astype(np.float32)
```

```python
import math
import numpy as np
import concourse.bass as bass
import concourse.mybir as mybir
from concourse.bass_utils import run_bass_kernel_spmd
from contextlib import ExitStack

F32 = mybir.dt.float32
BF16 = mybir.dt.bfloat16
I32 = mybir.dt.int32
AF = mybir.ActivationFunctionType
ALU = mybir.AluOpType
AX = mybir.AxisListType

ENGS = ("pe", "act", "dve", "pool", "sp")
SEM_LIMIT = 30000

S = 4096
D = 1024
NT = S // 128
NIN = 3752
DFF = 4096
EPS = 1e-6
NEG = -1.0e30
KBIS = 13
NSEL = 256


class LT:
    __slots__ = ("name", "last_w", "readers", "sem", "cnt", "dram", "burst", "wtoks")

    def __init__(self, name, dram=False):
        self.name = name
        self.last_w = None
        self.readers = []
        self.sem = None
        self.cnt = 0
        self.dram = dram
        self.burst = []
        self.wtoks = {}


class Op:
    __slots__ = ("eng", "fn", "raw", "oth", "sig", "tok", "isdma", "n", "toks")


class Prog:
    def __init__(self, nc, es):
        self.nc = nc
        self.es = es
        self.ops = []
        self.streams = {e: [] for e in ENGS}
        self.nsem = 0
        self.sem_pool = {}

    def new_sem(self, name):
        self.nsem += 1
        return self.es.enter_context(self.nc.semaphore(f"s{self.nsem}_{name}"))

    def _rec(self, eng, fn, r, w, isdma, n):
        o = Op()
        o.eng = eng; o.fn = fn; o.isdma = isdma; o.n = n; o.sig = False
        o.raw = []; o.oth = []; o.toks = []; o.tok = None
        oi = len(self.ops)
        for t in r:
            if t.dram:
                o.toks.extend(t.wtoks.values())
                t.readers.append(oi)
            else:
                if t.last_w is not None:
                    o.raw.append(t.last_w)
                t.readers.append(oi)
        for t in w:
            if t.dram:
                if t.readers:
                    t.burst = [x for x in t.readers if x != oi]
                    t.readers = []
                o.oth.extend(t.burst)
            else:
                if t.last_w is not None:
                    o.oth.append(t.last_w)
                o.oth.extend(x for x in t.readers if x != oi)
                t.last_w = oi
                t.readers = []
        if isdma:
            dst = w[0]
            key = dst.name
            if dst.dram:
                key = "src_" + [t for t in r if not t.dram][0].name
            ent = self.sem_pool.get(key)
            if ent is None:
                ent = [self.new_sem(key), 0]
                self.sem_pool[key] = ent
            ent[1] += 16 * n
            o.tok = (ent[0], ent[1])
            if dst.dram:
                dst.wtoks[id(ent[0])] = (ent[0], ent[1])
            else:
                dst.sem = ent[0]
                dst.cnt = ent[1]
        self.ops.append(o)
        self.streams[eng].append(oi)
        return oi

    def op(self, eng, fn, r=(), w=()):
        return self._rec(eng, fn, list(r), list(w), False, 0)

    def dma(self, q, fn, r=(), w=(), n=1):
        return self._rec(q, fn, list(r), list(w), True, n)

    def emit(self, final_waits=()):
        nc = self.nc
        ops = self.ops
        for oi, o in enumerate(ops):
            keep = []
            for d in o.raw:
                do = ops[d]
                if (not do.isdma) and (not o.isdma) and do.eng == o.eng and o.eng == "pe":
                    continue
                keep.append(d)
            for d in o.oth:
                do = ops[d]
                if (not do.isdma) and (not o.isdma) and do.eng == o.eng and o.eng == "pe":
                    continue
                keep.append(d)
            o.raw = sorted(set(keep))
            for d in o.raw:
                ops[d].sig = True
        for e in ENGS:
            sem = None
            cnt = 0
            for oi in self.streams[e]:
                o = ops[oi]
                if o.isdma or not o.sig:
                    continue
                if sem is None or cnt >= SEM_LIMIT:
                    sem = self.new_sem("eng_" + e)
                    cnt = 0
                cnt += 1
                o.tok = (sem, cnt)
        stats = {e: [0, 0] for e in ENGS}
        with nc.Block() as block:
            def body(ename):
                def run(eng):
                    known = {}
                    for oi in self.streams[ename]:
                        o = ops[oi]
                        need = {}
                        for d in o.raw:
                            s, v = ops[d].tok
                            if need.get(id(s), (None, 0))[1] < v:
                                need[id(s)] = (s, v)
                        for s, v in o.toks:
                            if need.get(id(s), (None, 0))[1] < v:
                                need[id(s)] = (s, v)
                        for k, (s, v) in need.items():
                            if known.get(k, 0) < v:
                                eng.wait_ge(s, v)
                                known[k] = v
                                stats[ename][1] += 1
                        ins = o.fn(eng)
                        stats[ename][0] += 1
                        if o.isdma:
                            if not isinstance(ins, (list, tuple)):
                                ins = [ins]
                            assert len(ins) == o.n, (len(ins), o.n)
                            for i_ in ins:
                                i_.then_inc(o.tok[0], 16)
                        elif o.sig:
                            ins.then_inc(o.tok[0], 1)
                    if ename == "sp":
                        for t in final_waits:
                            for (s_, v_) in t.wtoks.values():
                                eng.wait_ge(s_, v_)
                return run
            block.tensor(body("pe"))
            block.scalar(body("act"))
            block.vector(body("dve"))
            block.gpsimd(body("pool"))
            block.sync(body("sp"))
        return stats


class Arena:
    def __init__(self, tensor, elem_bytes):
        self.t = tensor
        self.eb = elem_bytes
        self.live = []
        self.bump = 0

    def carve(self, off_elems, shape, dtype, name):
        n = int(np.prod(shape[1:]))
        db = 2 if dtype == BF16 else 4
        start = off_elems * self.eb
        end = start + n * db
        assert end <= self.t.shape[1] * self.eb, (name, end)
        lt = LT(name)
        keep = []
        for (s0, e0, l0) in self.live:
            if s0 < end and start < e0:
                lt.readers.extend(l0.readers)
                if l0.last_w is not None:
                    lt.readers.append(l0.last_w)
                if s0 >= start and e0 <= end:
                    continue
            keep.append((s0, e0, l0))
        keep.append((start, end, lt))
        self.live = keep
        ap = self.t[:, off_elems:off_elems + (end - start) // self.eb]
        if dtype != ap.dtype:
            ap = ap.bitcast(dtype)
        if len(shape) == 3:
            ap = ap.rearrange("p (a b) -> p a b", a=shape[1])
        elif len(shape) == 4:
            ap = ap.rearrange("p (a b c) -> p a b c", a=shape[1], b=shape[2])
        if shape[0] < 128:
            ap = ap[0:shape[0]]
        return ap, lt

    def reset(self):
        self.bump = 0

    def alloc(self, shape, dtype, name):
        db = 2 if dtype == BF16 else 4
        n = int(np.prod(shape[1:])) * db
        n = (n + 3) // 4 * 4
        off = self.bump
        self.bump += n // self.eb
        return self.carve(off, shape, dtype, name)


def t5_thresholds():
    def bucket(n):
        if n < 16:
            return n
        nf = np.float32(max(n, 1))
        v = np.log(nf / np.float32(16)) / np.float32(math.log(128 / 16)) * np.float32(16)
        return min(16 + int(np.float32(v)), 31)
    thr = []
    for j in range(1, 32):
        n = 0
        while bucket(n) < j:
            n += 1
        thr.append(n)
    return thr


def build_nc(layers=(0, 1, 2, 3), final_norm=True, debug=False):
    nc = bass.Bass("TRN2", target_bir_lowering=False)
    dt_in = lambda n, s, d=F32: nc.dram_tensor(n, s, d, kind="ExternalInput").ap()
    x_d = dt_in("x", [S, D])
    pos_d = dt_in("pos", [S], I32)
    NLW = len(layers)
    anw_d = dt_in("attn_norm_w", [NLW, D])
    win_d = dt_in("w_in", [NLW, D, NIN])
    lbl_d = dt_in("hgrn_lb_logits", [4, 512])
    hnw_d = dt_in("hgrn_norm_w", [NLW, 128])
    qnw_d = dt_in("mla_q_norm_w", [NLW, 192])
    wqb_d = dt_in("mla_w_qb", [NLW, 192, 384])
    kvnw_d = dt_in("mla_kv_norm_w", [NLW, 128])
    wkvb_d = dt_in("mla_w_kvb", [NLW, 128, 512])
    iknw_d = dt_in("idx_k_norm_w", [NLW, 64])
    iknb_d = dt_in("idx_k_norm_b", [NLW, 64])
    tab_d = dt_in("rel_bias_table", [32, 4])
    wout_d = dt_in("w_out", [NLW, D, D])
    mnw_d = dt_in("mlp_norm_w", [NLW, D])
    w1_d = dt_in("w_mlp_in", [NLW, D, DFF])
    w2_d = dt_in("w_mlp_out", [NLW, DFF, D])
    fnw_d = dt_in("final_norm_w", [D])
    invf_d = dt_in("c_invf", [128, 16])
    pw2_d = dt_in("c_pw2", [128, KBIS])
    out_d = nc.dram_tensor("out", [S, D], F32, kind="ExternalOutput").ap()
    skind = "ExternalOutput" if debug else "Internal"
    sc = lambda n, s, d: nc.dram_tensor(n, s, d, kind=skind).ap()
    hb0_d = sc("hb0", [S, D], F32)
    hb1_d = sc("hb1", [S, D], F32)
    yh_d = sc("yh", [S, 512], BF16)
    mqT_d = sc("mqT", [96, 4, S], BF16)
    dqT_d = sc("dqT", [128, 2, S], BF16)
    iqT_d = sc("iqT", [128, 4, S], BF16)
    iws_d = sc("iws", [S, 8], F32)
    lbs_d = sc("lbs", [4, 128, 512], F32)
    mix_d = sc("mixdbg", [S, 512], BF16)

    with ExitStack() as es:
        P = Prog(nc, es)
        sbt = lambda n, s, d: es.enter_context(nc.sbuf_tensor(n, s, d))
        AR = Arena(sbt("arena", [128, 75328], BF16), 2)
        TM = Arena(sbt("tmp", [128, 10240], F32), 4)
        pbt = [es.enter_context(nc.psum_tensor(f"pb{k}", [128, 512], F32)) for k in range(8)]
        pb = [t[:] for t in pbt]
        Lpb = [LT(f"pb{k}") for k in range(8)]

        def cst(name, shape, dtype):
            return sbt(name, shape, dtype)[:], LT(name)

        def MM(out, lhsT, rhs, start, stop, r, w):
            P.op("pe", lambda e: e.matmul(out, lhsT=lhsT, rhs=rhs, start=start, stop=stop, skip_group_check=True), r, w)

        def TR(out, in_, ident, r, w):
            P.op("pe", lambda e: e.transpose(out=out, in_=in_, identity=ident), r, w)

        def ACT(out, in_, func, r, w, scale=None, bias=None):
            kw = {}
            if scale is not None:
                kw["scale"] = scale
            if bias is not None:
                kw["bias"] = bias
            P.op("act", lambda e: e.activation(out=out, in_=in_, func=func, **kw), r, w)

        def TS(eng, out, in0, s1, s2, op0, op1, r, w, accum=None):
            kw = {}
            if op1 is not None:
                kw["op1"] = op1
            if accum is not None:
                kw["accum_out"] = accum
            P.op(eng, lambda e: e.tensor_scalar(out=out, in0=in0, scalar1=s1, scalar2=s2, op0=op0, **kw), r, w)

        def TT(eng, out, in0, in1, op, r, w):
            P.op(eng, lambda e: e.tensor_tensor(out=out, in0=in0, in1=in1, op=op), r, w)

        def STT(out, in0, scalar, in1, op0, op1, r, w):
            P.op("dve", lambda e: e.scalar_tensor_tensor(out=out, in0=in0, scalar=scalar, in1=in1, op0=op0, op1=op1), r, w)

        def TTR(out, in0, in1, accum, r, w):
            P.op("dve", lambda e: e.scalar_tensor_tensor(out=out, in0=in0, scalar=1.0, in1=in1, op0=ALU.mult,
                                                         op1=ALU.mult, accum_out=accum), r, w)

        def RSTD(out, ss, tmp, inv_w, r, w):
            ACT(tmp, ss, AF.Ln, r + [Lcpi], w, scale=inv_w, bias=cpi[:, 1:2])
            ACT(out, tmp, AF.Exp, w, w, scale=-0.5)

        def RED(out, in_, op, r, w):
            P.op("dve", lambda e: e.tensor_reduce(out=out, in_=in_, axis=AX.X, op=op), r, w)

        def RCP(out, in_, r, w):
            P.op("dve", lambda e: e.reciprocal(out=out, in_=in_), r, w)

        def CP(eng, out, in_, r, w):
            P.op(eng, lambda e: e.tensor_copy(out=out, in_=in_), r, w)

        def MS(eng, ap, val, w):
            P.op(eng, lambda e: e.memset(ap, val), (), w)

        def ASEL(out, in_, pattern, cmp, fill, base, cm, r, w):
            P.op("pool", lambda e: e.affine_select(out=out, in_=in_, pattern=pattern, compare_op=cmp, fill=fill,
                                                   base=base, channel_multiplier=cm), r, w)

        def DMA(q, out, in_, r, w):
            P.dma(q, lambda e: e.dma_start(out=out, in_=in_), r, w)

        def DMAS(q, out, in_, r, w):
            P.dma(q, lambda e: e.dma_start(out=out, in_=in_, allow_slow_non_contiguous=True), r, w)

        Lhb0 = LT("hb0", True); Lhb1 = LT("hb1", True); Lyh = LT("yh", True)
        LmqT = LT("mqT", True); LdqT = LT("dqT", True); LiqT = LT("iqT", True)
        Liws = LT("iws", True); Llbs = LT("lbs", True); Lout = LT("out", True); Lmixd = LT("mixdbg", True)

        idf, Lidf = cst("idf", [128, 128], F32)
        idb, Lidb = cst("idb", [128, 128], BF16)
        Bd, LBd = cst("Bd", [128, 128], F32)
        Lc, LLc = cst("Lc", [128, 128], F32)
        Uc, LUc = cst("Uc", [128, 128], F32)
        Ind, LInd = cst("Ind", [128, 4], F32)
        cosT, Lcos = cst("cosT", [128, NT, 16], F32)
        sinT, Lsin = cst("sinT", [128, NT, 16], F32)
        invf, Linvf = cst("invf", [128, 16], F32)
        pw2, Lpw2 = cst("pw2", [128, KBIS], F32)
        tabb, Ltabb = cst("tabb", [128, 128], F32)
        dlt, Ldlt = cst("dlt", [128, 124], F32)
        biasN, LbiasN = cst("biasN", [128, 4, 256], F32)
        biasN8, LbiasN8 = cst("biasN8", [128, 4, 256], BF16)
        lbB, LlbB = cst("lbB", [128, 512], F32)
        omlbB, LomlbB = cst("omlbB", [128, 512], F32)
        ncol, Lncol = cst("ncol", [128, 24], F32)
        cpi, Lcpi = cst("cpi", [128, 2], F32)
        vst, Lvst = cst("vst", [24, 128], F32)

        MS("pool", idf, 0.0, [Lidf])
        ASEL(idf, idf, [[-1, 128]], ALU.not_equal, 1.0, 0, 1, [Lidf], [Lidf])
        CP("dve", idb, idf, [Lidf], [Lidb])
        MS("pool", Bd, 1.0, [LBd])
        for c in range(4):
            v = Bd[:, 32 * c:32 * c + 32]
            ASEL(v, v, [[0, 32]], ALU.is_ge, 0.0, -32 * c, 1, [LBd], [LBd])
            ASEL(v, v, [[0, 32]], ALU.is_ge, 0.0, 32 * c + 31, -1, [LBd], [LBd])
        ASEL(Lc, Bd, [[1, 128]], ALU.is_ge, 0.0, 0, -1, [LBd], [LLc])
        TT("dve", Uc, Bd, Lc, ALU.subtract, [LBd, LLc], [LUc])
        for c in range(4):
            CP("dve", Ind[:, c:c + 1], Bd[:, 32 * c:32 * c + 1], [LBd], [LInd])
        cm01f, Lcm01f = cst("cm01f", [128, 128], F32)
        cm01, Lcm01 = cst("cm01", [128, 128], BF16)
        cmNEG, LcmNEG = cst("cmNEG", [128, 128], F32)
        MS("pool", cm01f, 1.0, [Lcm01f])
        ASEL(cm01f, cm01f, [[-1, 128]], ALU.is_ge, 0.0, 0, 1, [Lcm01f], [Lcm01f])
        CP("dve", cm01, cm01f, [Lcm01f], [Lcm01])
        cm01Tf, Lcm01Tf = cst("cm01Tf", [128, 128], F32)
        cm01T, Lcm01T = cst("cm01T", [128, 128], BF16)
        MS("pool", cm01Tf, 1.0, [Lcm01Tf])
        ASEL(cm01Tf, cm01Tf, [[1, 128]], ALU.is_ge, 0.0, 0, -1, [Lcm01Tf], [Lcm01Tf])
        CP("dve", cm01T, cm01Tf, [Lcm01Tf], [Lcm01T])
        MS("pool", cmNEG, 0.0, [LcmNEG])
        ASEL(cmNEG, cmNEG, [[-1, 128]], ALU.is_ge, NEG, 0, 1, [LcmNEG], [LcmNEG])
        MS("pool", cpi[:, 0:1], math.pi, [Lcpi])
        MS("pool", cpi[:, 1:2], EPS, [Lcpi])
        DMA("sp", invf, invf_d, [], [Linvf])
        DMA("sp", pw2, pw2_d, [], [Lpw2])
        DMA("sp", tabb, tab_d.rearrange("a b -> (a b)").partition_broadcast(128), [], [Ltabb])
        TT("dve", dlt, tabb[:, 4:128], tabb[:, 0:124], ALU.subtract, [Ltabb], [Ldlt])

        TM.reset()
        posi, Lposi = TM.alloc([128, NT], I32, "posi")
        posf, Lposf = TM.alloc([128, NT], F32, "posf")
        ang, Lang = TM.alloc([128, NT, 16], F32, "ang")
        ang2, Lang2 = TM.alloc([128, NT, 16], F32, "ang2")
        posr, Lposr = TM.alloc([NT, 128], I32, "posr")
        posrf, Lposrf = TM.alloc([NT, 128], F32, "posrf")
        DMA("sp", posr, pos_d.rearrange("(n p) -> n p", p=128), [], [Lposr])
        CP("dve", posrf, posr, [Lposr], [Lposrf])
        TR(pb[0][:, 0:NT], posrf, idf[0:NT, 0:NT], [Lposrf, Lidf], [Lpb[0]])
        CP("dve", posf, pb[0][:, 0:NT], [Lpb[0]], [Lposf])
        TT("dve", ang, posf.unsqueeze(2).to_broadcast([128, NT, 16]),
           invf.unsqueeze(1).to_broadcast([128, NT, 16]), ALU.mult, [Lposf, Linvf], [Lang])
        TWO_PI = 2.0 * math.pi
        angi, Langi = TM.alloc([128, NT, 16], I32, "angi")
        angk, Langk = TM.alloc([128, NT, 16], F32, "angk")
        TS("dve", ang2, ang, math.pi / 2, None, ALU.add, None, [Lang], [Lang2])

        def reduce_pi(a, La):
            TS("dve", angk, a, 1.0 / TWO_PI, None, ALU.mult, None, [La], [Langk])
            CP("dve", angi, angk, [Langk], [Langi])
            CP("dve", angk, angi, [Langi], [Langk])
            STT(a, angk, -TWO_PI, a, ALU.mult, ALU.add, [Langk, La], [La])
            TS("dve", angk, a, math.pi, -TWO_PI, ALU.is_gt, ALU.mult, [La], [Langk])
            TT("dve", a, a, angk, ALU.add, [La, Langk], [La])
            TS("dve", angk, a, -math.pi, TWO_PI, ALU.is_lt, ALU.mult, [La], [Langk])
            TT("dve", a, a, angk, ALU.add, [La, Langk], [La])
        reduce_pi(ang, Lang)
        reduce_pi(ang2, Lang2)
        ACT(sinT, ang, AF.Sin, [Lang], [Lsin])
        ACT(cosT, ang2, AF.Sin, [Lang2], [Lcos])

        dti, Ldti = TM.alloc([128, 256], I32, "dti")
        dtf, Ldtf = TM.alloc([128, 256], F32, "dtf")
        stp, Lstp = TM.alloc([128, 256], F32, "stp")
        P.op("pool", lambda e: e.iota(dti[:, 0:128], pattern=[[-1, 128]], base=128, channel_multiplier=1), [], [Ldti])
        P.op("pool", lambda e: e.iota(dti[:, 128:256], pattern=[[-1, 128]], base=0, channel_multiplier=1), [Ldti], [Ldti])
        CP("dve", dtf, dti, [Ldti], [Ldtf])
        for h in range(4):
            CP("dve", biasN[:, h, :], tabb[:, h:h + 1].to_broadcast([128, 256]), [Ltabb], [LbiasN])
        thr = t5_thresholds()
        for j in range(1, 32):
            TS("dve", stp, dtf, float(thr[j - 1]) - 0.5, None, ALU.is_ge, None, [Ldtf], [Lstp])
            for h in range(4):
                STT(biasN[:, h, :], stp, dlt[:, (j - 1) * 4 + h:(j - 1) * 4 + h + 1], biasN[:, h, :],
                    ALU.mult, ALU.add, [Lstp, Ldlt, LbiasN], [LbiasN])

        TS("dve", biasN8, biasN, 8.0, None, ALU.mult, None, [LbiasN], [LbiasN8])
        lg, Llg = TM.alloc([128, 4, 512], F32, "lg")
        lsum, Llsum = TM.alloc([128, 512], F32, "lsum")
        lacc, Llacc = TM.alloc([128, 512], F32, "lacc")
        DMA("sp", lg, lbl_d.rearrange("a b -> (a b)").partition_broadcast(128).rearrange("p (a b) -> p a b", a=4),
            [], [Llg])
        ACT(lg, lg, AF.Exp, [Llg], [Llg])
        TT("dve", lsum, lg[:, 0, :], lg[:, 1, :], ALU.add, [Llg], [Llsum])
        TT("dve", lsum, lsum, lg[:, 2, :], ALU.add, [Llg, Llsum], [Llsum])
        TT("dve", lsum, lsum, lg[:, 3, :], ALU.add, [Llg, Llsum], [Llsum])
        RCP(lsum, lsum, [Llsum], [Llsum])
        MS("dve", lacc, 0.0, [Llacc])
        for l in range(4):
            if l > 0:
                TT("dve", lg[:, l, :], lg[:, l, :], lsum, ALU.mult, [Llg, Llsum], [Llg])
                TT("dve", lacc, lacc, lg[:, l, :], ALU.add, [Llacc, Llg], [Llacc])
            DMA("sp", lbs_d[l], lacc, [Llacc], [Llbs])

        ISQ = 128.0 ** -0.5

        def rms_to_uT(xt, Lxt, width_chunks, ub, Lub, uT, LuT, bank, Lbank, stat, Lstat, junk, Ljunk):
            W = width_chunks * 128
            TTR(junk[:, 0:W], xt, xt, stat[:, 0:1], [Lxt], [Ljunk, Lstat])
            RSTD(stat[:, 2:3], stat[:, 0:1], stat[:, 1:2], 1.0 / W, [Lstat], [Lstat])
            ACT(ub, xt, AF.Copy, [Lxt, Lstat], [Lub], scale=stat[:, 2:3])
            bb = bank.bitcast(BF16)
            for kc in range(width_chunks):
                TR(bb[:, kc * 128:(kc + 1) * 128], ub[:, kc * 128:(kc + 1) * 128], idb, [Lub, Lidb], [Lbank])
            CP("dve", uT, bb[:, 0:W].rearrange("p (a b) -> p a b", a=width_chunks), [Lbank], [LuT])

        def load_w_rows(dst, Ldst, src2d, nrows_chunks, col0, ncols, scale_col=None, Lsc=None, q="pool"):
            P.dma(q, lambda e: [e.dma_start(out=dst[:, kc, :], in_=src2d[kc * 128:(kc + 1) * 128, col0:col0 + ncols])
                                for kc in range(nrows_chunks)], [], [Ldst], n=nrows_chunks)
            if scale_col is not None:
                for kc in range(nrows_chunks):
                    TS("dve", dst[:, kc, :], dst[:, kc, :], scale_col[:, kc:kc + 1], None, ALU.mult, None,
                       [Ldst, Lsc], [Ldst])

        for li, l in enumerate(layers):
            first = (li == 0)
            last = (li == len(layers) - 1)
            src_d = x_d if first else hb0_d
            Lsrc = None if first else Lhb0
            rsrc = [] if first else [Lhb0]

            MS("pool", vst, 0.0, [Lvst])
            DMA("sp", vst[0:8, :], anw_d[li].rearrange("(k p) -> k p", p=128), [], [Lvst])
            DMA("sp", vst[8:16, :], mnw_d[li].rearrange("(k p) -> k p", p=128), [], [Lvst])
            DMA("sp", vst[16:17, :], qnw_d[li, 0:128].rearrange("(k p) -> k p", p=128), [], [Lvst])
            DMA("sp", vst[17:18, 0:64], qnw_d[li, 128:192].rearrange("(k p) -> k p", p=64), [], [Lvst])
            DMA("sp", vst[18:19, :], kvnw_d[li].rearrange("(k p) -> k p", p=128), [], [Lvst])
            DMA("sp", vst[19:20, :], hnw_d[li].rearrange("(k p) -> k p", p=128), [], [Lvst])
            DMA("sp", vst[20:21, 0:64], iknw_d[li].rearrange("(k p) -> k p", p=64), [], [Lvst])
            DMA("sp", vst[20:21, 64:128], iknw_d[li].rearrange("(k p) -> k p", p=64), [], [Lvst])
            DMA("sp", vst[21:22, 0:64], iknb_d[li].rearrange("(k p) -> k p", p=64), [], [Lvst])
            DMA("sp", vst[21:22, 64:128], iknb_d[li].rearrange("(k p) -> k p", p=64), [], [Lvst])
            TR(pb[0][:, 0:24], vst, idf[0:24, 0:24], [Lvst, Lidf], [Lpb[0]])
            CP("dve", ncol, pb[0][:, 0:24], [Lpb[0]], [Lncol])
            DMA("sp", lbB, lbs_d[l], [Llbs], [LlbB])
            TS("dve", omlbB, lbB, -1.0, 1.0, ALU.mult, ALU.add, [LlbB], [LomlbB])

            winH, LwinH = AR.carve(45312, [128, 8, 2048], BF16, "winH")
            load_w_rows(winH, LwinH, win_d[li], 8, 0, 2048, ncol[:, 0:8], Lncol)
            TM.reset()
            xts = [TM.alloc([128, D], F32, f"xt{k}") for k in range(3)]
            ub, Lub = TM.alloc([128, D], BF16, "ub")
            uTs = [TM.alloc([128, 8, 128], BF16, f"uT{k}") for k in range(3)]
            stats_ = [TM.alloc([128, 16], F32, f"stat{k}") for k in range(2)]
            statF = [TM.alloc([128, 4], F32, f"statF{k}") for k in range(3)]
            AR.bump = 0
            P3 = [AR.alloc([128, 2048], F32, f"Hprf{k}") for k in range(3)]
            HS = []
            for k in range(2):
                d_ = {}
                for nm, shp, dt_ in [("tA", [128, 512], F32),
                                     ("tB", [128, 512], F32), ("tC", [128, 512], F32), ("logf", [128, 512], F32),
                                     ("kk", [128, 512], F32), ("sil", [128, 512], F32), ("dcy", [128, 16], F32),
                                     ("qd", [128, 512], BF16), ("ki", [128, 512], BF16), ("ks", [128, 4, 512], BF16),
                                     ("vb", [128, 512], BF16), ("kiT", [128, 4, 128], BF16), ("qdT", [128, 4, 128], BF16),
                                     ("ATb", [128, 4, 128], BF16), ("yht", [128, 512], BF16),
                                     ("qdTc0", [128, 4, 128], BF16), ("qdTc1", [128, 4, 128], BF16),
                                     ("qdTc2", [128, 4, 128], BF16), ("qdTc3", [128, 4, 128], BF16)]:
                    d_[nm] = AR.alloc(shp, dt_, f"H{nm}{k}")
                HS.append(d_)
                for c in range(4):
                    MS("pool", d_[f"qdTc{c}"][0], 0.0, [d_[f"qdTc{c}"][1]])
            Sf3, LSf3 = AR.alloc([128, 4, 128], F32, "Sf3")
            Sb3, LSb3 = AR.alloc([128, 4, 128], BF16, "Sb3")
            MS("pool", Sf3, 0.0, [LSf3])
            MS("pool", Sb3, 0.0, [LSb3])

            def H_front(i):
                xt, Lxt = xts[i % 3]
                uT, LuT = uTs[i % 3]
                st_, Lst_ = statF[i % 3]
                prf, Lprf = P3[i % 3]
                DMA("sp", xt, src_d[i * 128:(i + 1) * 128, :], rsrc, [Lxt])
                rms_to_uT(xt, Lxt, 8, ub, Lub, uT, LuT, pb[0], Lpb[0], st_, Lst_, prf, Lprf)
                for n in range(4):
                    bk = 1 + (n % 2)
                    for kc in range(8):
                        MM(pb[bk], uT[:, kc, :], winH[:, kc, n * 512:(n + 1) * 512], kc == 0, kc == 7,
                           [LuT, LwinH], [Lpb[bk]])
                    ACT(prf[:, n * 512:(n + 1) * 512], pb[bk], AF.Copy, [Lpb[bk]], [Lprf])

            def H_get(i):
                hs = HS[i % 2]
                return hs

            def H_mid(i, part):
                hs = HS[i % 2]
                prf, Lprf = P3[i % 3]; tA, LtA = hs["tA"]; tB, LtB = hs["tB"]; tC, LtC = hs["tC"]
                logf, Llogf = hs["logf"]; kk, Lkk = hs["kk"]; sil, Lsil = hs["sil"]; dcy, Ldcy = hs["dcy"]
                qd, Lqd = hs["qd"]; ki, Lki = hs["ki"]; ks, Lks = hs["ks"]; vb, Lvb = hs["vb"]
                kiT, LkiT = hs["kiT"]; qdT, LqdT = hs["qdT"]; ATb, LATb = hs["ATb"]
                qdTc = [hs[f"qdTc{c}"] for c in range(4)]
                hq = prf[:, 0:512]; hf = prf[:, 512:1024]; hi = prf[:, 1024:1536]; hg = prf[:, 1536:2048]
                if part == 0:
                    ACT(tA, hf, AF.Sigmoid, [Lprf], [LtA])
                    ACT(sil, hg, AF.Silu, [Lprf], [Lsil])
                    TT("dve", tA, tA, omlbB, ALU.mult, [LtA, LomlbB], [LtA])
                    TT("dve", tA, tA, lbB, ALU.add, [LtA, LlbB], [LtA])
                    TS("dve", kk, tA, -1.0, 1.0, ALU.mult, ALU.add, [LtA], [Lkk])
                    ACT(logf, tA, AF.Ln, [LtA], [Llogf])
                    MM(pb[3], Lc, logf, True, True, [LLc, Llogf], [Lpb[3]])
                    MM(pb[4], Uc, logf, True, True, [LUc, Llogf], [Lpb[4]])
                    for h in range(4):
                        MM(pb[5][:, h * 4:(h + 1) * 4], logf[:, h * 128:(h + 1) * 128], Ind, True, True,
                           [Llogf, LInd], [Lpb[5]])
                elif part == 1:
                    ACT(dcy, pb[5][:, 0:16], AF.Exp, [Lpb[5]], [Ldcy])
                    ACT(tB, pb[3], AF.Exp, [Lpb[3]], [LtB])
                    STT(qd, hq, ISQ, tB, ALU.mult, ALU.mult, [Lprf, LtB], [Lqd])
                    ACT(tC, pb[3], AF.Exp, [Lpb[3]], [LtC], scale=-1.0)
                    TT("dve", ki, kk, tC, ALU.mult, [Lkk, LtC], [Lki])
                    ACT(tB, pb[4], AF.Exp, [Lpb[4]], [LtB])
                    TT("dve", tC, kk, tB, ALU.mult, [Lkk, LtB], [LtC])
                    for c in range(4):
                        ACT(ks[:, c, :], tC, AF.Copy, [LtC, LInd], [Lks], scale=Ind[:, c:c + 1])
                    ACT(vb, hi, AF.Copy, [Lprf], [Lvb])
                elif part == 2:
                    b6_ = pb[6].bitcast(BF16)
                    for h in range(4):
                        TR(b6_[:, h * 128:(h + 1) * 128], qd[:, h * 128:(h + 1) * 128], idb, [Lqd, Lidb], [Lpb[6]])
                        TR(b6_[:, 512 + h * 128:512 + (h + 1) * 128], ki[:, h * 128:(h + 1) * 128], idb, [Lki, Lidb], [Lpb[6]])
                    b6q = b6_[:, 0:512].rearrange("p (a b) -> p a b", a=4)
                    CP("dve", qdT, b6q, [Lpb[6]], [LqdT])
                    CP("dve", kiT, b6_[:, 512:1024].rearrange("p (a b) -> p a b", a=4), [Lpb[6]], [LkiT])
                    for c in range(4):
                        ACT(qdTc[c][0][:, :, 32 * c:32 * c + 32], qdT[:, :, 32 * c:32 * c + 32], AF.Copy, [LqdT], [qdTc[c][1]])
                else:
                    for h in range(4):
                        MM(pb[3][:, h * 128:(h + 1) * 128], kiT[:, h, :], qdT[:, h, :], True, True, [LkiT, LqdT], [Lpb[3]])
                    TT("dve", ATb, pb[3].rearrange("p (a b) -> p a b", a=4), Lc.unsqueeze(1).to_broadcast([128, 4, 128]),
                       ALU.mult, [Lpb[3], LLc], [LATb])

            def H_tail(i, c):
                hs = HS[i % 2]
                dcy, Ldcy = hs["dcy"]; ks, Lks = hs["ks"]; vb, Lvb = hs["vb"]; ATb, LATb = hs["ATb"]
                qdTc = [hs[f"qdTc{cc}"] for cc in range(4)]
                dcy3 = dcy.rearrange("p (a b) -> p a b", a=4)
                if c == 0:
                    for h in range(4):
                        MM(pb[7][:, h * 128:(h + 1) * 128], ATb[:, h, :], vb[:, h * 128:(h + 1) * 128], h == 0, False,
                           [LATb, Lvb], [Lpb[7]])
                for h in range(4):
                    MM(pb[7][:, h * 128:(h + 1) * 128], qdTc[c][0][:, h, :], Sb3[:, h, :], False, c == 3,
                       [qdTc[c][1], LSb3], [Lpb[7]])
                for h in range(4):
                    MM(pb[1][:, h * 128:(h + 1) * 128], ks[:, c, h * 128:(h + 1) * 128], vb[:, h * 128:(h + 1) * 128],
                       True, True, [Lks, Lvb], [Lpb[1]])
                TT("dve", Sf3, Sf3, dcy3[:, :, c:c + 1].to_broadcast([128, 4, 128]), ALU.mult, [LSf3, Ldcy], [LSf3])
                TT("dve", Sf3, Sf3, pb[1].rearrange("p (a b) -> p a b", a=4), ALU.add, [LSf3, Lpb[1]], [LSf3])
                CP("dve", Sb3, Sf3, [LSf3], [LSb3])

            def H_out(i):
                st_, Lst_ = stats_[i % 2]
                hs = HS[i % 2]
                tC, LtC = hs["tC"]; tB, LtB = hs["tB"]; sil, Lsil = hs["sil"]; yht, Lyht = hs["yht"]
                ACT(tB, pb[7], AF.Copy, [Lpb[7]], [LtB])
                for h in range(4):
                    TTR(tC[:, h * 128:(h + 1) * 128], tB[:, h * 128:(h + 1) * 128], tB[:, h * 128:(h + 1) * 128],
                        st_[:, 4 + h:5 + h], [LtB], [LtC, Lst_])
                RSTD(st_[:, 12:16], st_[:, 4:8], st_[:, 8:12], 1.0 / 128, [Lst_], [Lst_])
                for h in range(4):
                    STT(yht[:, h * 128:(h + 1) * 128], tB[:, h * 128:(h + 1) * 128], st_[:, 12 + h:13 + h],
                        sil[:, h * 128:(h + 1) * 128], ALU.mult, ALU.mult, [LtB, Lst_, Lsil], [Lyht])
                DMA("sp", yh_d[i * 128:(i + 1) * 128, :], yht, [Lyht], [Lyh])

            H_front(0)
            if NT > 1:
                H_front(1)
            for p_ in range(4):
                H_mid(0, p_)
            for i in range(NT):
                if i + 2 < NT:
                    H_front(i + 2)
                for c in range(4):
                    if i + 1 < NT:
                        H_mid(i + 1, c)
                    H_tail(i, c)
                H_out(i)

            if debug == "H":
                break
            mkT, LmkT = AR.carve(0, [128, 4, S], BF16, "mkT")
            mvc, Lmvc = AR.carve(16384, [128, NT, 4, 65], BF16, "mvc")
            dkT, LdkT = AR.carve(24704, [128, 2, S], BF16, "dkT")
            dvc, Ldvc = AR.carve(32896, [128, NT, 4, 65], BF16, "dvc")
            kiT2, LkiT2 = AR.carve(41216, [128, S], BF16, "kiT2")
            wout, Lwout = AR.carve(45312, [128, 8, D], BF16, "wout")
            winK, LwinK = AR.carve(53504, [128, 8, 1704], BF16, "winK")
            wqb, Lwqb = AR.carve(67136, [128, 2, 384], BF16, "wqb")
            wkvb, Lwkvb = AR.carve(67904, [128, 512], BF16, "wkvb")
            load_w_rows(winK, LwinK, win_d[li], 8, 2048, 1704, ncol[:, 0:8], Lncol)
            DMA("pool", wqb[:, 0, :], wqb_d[li, 0:128, :], [], [Lwqb])
            DMA("pool", wqb[0:64, 1, :], wqb_d[li, 128:192, :], [], [Lwqb])
            TS("dve", wqb[:, 0, :], wqb[:, 0, :], ncol[:, 16:17], None, ALU.mult, None, [Lwqb, Lncol], [Lwqb])
            TS("dve", wqb[0:64, 1, :], wqb[0:64, 1, :], ncol[0:64, 17:18], None, ALU.mult, None, [Lwqb, Lncol], [Lwqb])
            DMA("pool", wkvb, wkvb_d[li], [], [Lwkvb])
            TS("dve", wkvb, wkvb, ncol[:, 18:19], None, ALU.mult, None, [Lwkvb, Lncol], [Lwkvb])
            load_w_rows(wout, Lwout, wout_d[li], 8, 0, D)
            for kc in range(4):
                TS("dve", wout[:, kc, :], wout[:, kc, :], ncol[:, 19:20], None, ALU.mult, None, [Lwout, Lncol], [Lwout])
            MS("pool", mvc[:, :, :, 64:65], 1.0, [Lmvc])
            MS("pool", dvc[:, :, :, 64:65], 1.0, [Ldvc])

            TM.reset()
            xts = [TM.alloc([128, D], F32, f"xt{k}") for k in range(2)]
            ub, Lub = TM.alloc([128, D], BF16, "ub")
            uTs = [TM.alloc([128, 8, 128], BF16, f"uT{k}") for k in range(2)]
            KS = []
            for k in range(2):
                d_ = {}
                for nm, shp, dt_ in [("stat", [128, 16], F32), ("junk", [128, D], F32), ("dqs", [128, 2, 128], BF16),
                                     ("iqs", [128, 4, 128], BF16), ("mqs", [128, 4, 128], BF16), ("iwt", [128, 8], F32),
                                     ("ikx", [128, 64], F32), ("cen", [128, 64], F32), ("kin2", [128, 128], BF16),
                                     ("qn", [128, 192], BF16), ("qnT", [128, 2, 128], BF16), ("kvn", [128, 128], BF16),
                                     ("kvnT", [128, 128], BF16), ("qfull", [128, 4, 96], BF16), ("kpe", [128, 96], BF16),
                                     ("r1", [128, 4, 16], F32), ("r2", [128, 4, 16], F32),
                                     ("stA", [128, 16], F32), ("stB", [128, 16], F32), ("stC", [128, 16], F32),
                                     ("scA", [128, 64], F32), ("scB", [128, 192], F32), ("scC", [128, 128], F32)]:
                    d_[nm] = TM.alloc(shp, dt_, f"K{nm}{k}")
                KS.append(d_)
                MS("pool", d_["kpe"][0], 0.0, [d_["kpe"][1]])

            def rope(ks_, dst1, dst2, x1, x2, i, nh, rr, ww):
                r1, Lr1 = ks_["r1"]; r2, Lr2 = ks_["r2"]
                cs = cosT[:, i, :].unsqueeze(1).to_broadcast([128, nh, 16])
                sn = sinT[:, i, :].unsqueeze(1).to_broadcast([128, nh, 16])
                a1 = r1[:, 0:nh, :]; a2 = r2[:, 0:nh, :]
                TT("dve", a1, x1, cs, ALU.mult, rr + [Lcos], [Lr1])
                TT("dve", a2, x2, sn, ALU.mult, rr + [Lsin], [Lr2])
                TT("dve", dst1, a1, a2, ALU.subtract, [Lr1, Lr2], ww)
                TT("dve", a1, x2, cs, ALU.mult, rr + [Lcos], [Lr1])
                TT("dve", a2, x1, sn, ALU.mult, rr + [Lsin], [Lr2])
                TT("dve", dst2, a1, a2, ALU.add, [Lr1, Lr2], ww)

            b7 = pb[7].bitcast(BF16)
            b5 = pb[5].bitcast(BF16)

            def K_front(i):
                ks_ = KS[i % 2]
                xt, Lxt = xts[i % 2]
                uT, LuT = uTs[i % 2]
                stat, Lstat = ks_["stat"]; junk, Ljunk = ks_["junk"]
                dqs, Ldqs = ks_["dqs"]; iqs, Liqs = ks_["iqs"]; iwt, Liwt = ks_["iwt"]; ikx, Likx = ks_["ikx"]
                tc = slice(i * 128, (i + 1) * 128)
                DMA("sp", xt, src_d[tc, :], rsrc, [Lxt])
                rms_to_uT(xt, Lxt, 8, ub, Lub, uT, LuT, pb[0], Lpb[0], stat, Lstat, junk, Ljunk)
                for kc in range(8):
                    MM(pb[1][:, 0:352], uT[:, kc, :], winK[:, kc, 0:352], kc == 0, kc == 7, [LuT, LwinK], [Lpb[1]])
                for kc in range(8):
                    MM(pb[2][:, 0:256], uT[:, kc, :], winK[:, kc, 864:1120], kc == 0, kc == 7, [LuT, LwinK], [Lpb[2]])
                for kc in range(8):
                    MM(pb[2][:, 256:328], uT[:, kc, :], winK[:, kc, 1632:1704], kc == 0, kc == 7, [LuT, LwinK], [Lpb[2]])
                for c in range(4):
                    for kc in range(8):
                        MM(pb[3][:, c * 128:(c + 1) * 128], winK[:, kc, 352 + c * 128:352 + (c + 1) * 128], uT[:, kc, :],
                           kc == 0, kc == 7, [LuT, LwinK], [Lpb[3]])
                for c in range(4):
                    for kc in range(8):
                        MM(pb[4][:, c * 128:(c + 1) * 128], winK[:, kc, 1120 + c * 128:1120 + (c + 1) * 128], uT[:, kc, :],
                           kc == 0, kc == 7, [LuT, LwinK], [Lpb[4]])
                ACT(junk[:, 0:352], pb[1][:, 0:352], AF.Copy, [Lpb[1]], [Ljunk])
                CP("dve", dqs, pb[3][:, 0:256].rearrange("p (a b) -> p a b", a=2), [Lpb[3]], [Ldqs])
                DMA("sp", dqT_d[:, :, tc], dqs, [Ldqs], [LdqT])
                CP("dve", dkT[:, :, tc], pb[3][:, 256:512].rearrange("p (a b) -> p a b", a=2), [Lpb[3]], [LdkT])
                ACT(iqs, pb[4].rearrange("p (a b) -> p a b", a=4), AF.Copy, [Lpb[4]], [Liqs])
                DMA("sp", iqT_d[:, :, tc], iqs, [Liqs], [LiqT])
                CP("dve", dvc[:, i, :, 0:64], pb[2][:, 0:256].rearrange("p (a b) -> p a b", a=4), [Lpb[2]], [Ldvc])
                CP("dve", iwt, pb[2][:, 320:328], [Lpb[2]], [Liwt])
                DMA("sp", iws_d[tc, :], iwt, [Liwt], [Liws])
                CP("dve", ikx, pb[2][:, 256:320], [Lpb[2]], [Likx])

            def K_rest(i):
                ks_ = KS[i % 2]
                stat, Lstat = ks_["stat"]; junk, Ljunk = ks_["junk"]
                mqs, Lmqs = ks_["mqs"]; ikx, Likx = ks_["ikx"]; cen, Lcen = ks_["cen"]; kin2, Lkin2 = ks_["kin2"]
                qn, Lqn = ks_["qn"]; qnT, LqnT = ks_["qnT"]; kvn, Lkvn = ks_["kvn"]; kvnT, LkvnT = ks_["kvnT"]
                qfull, Lqfull = ks_["qfull"]; kpe, Lkpe = ks_["kpe"]
                stA, LstA = ks_["stA"]; stB, LstB = ks_["stB"]; stC, LstC = ks_["stC"]
                scA, LscA = ks_["scA"]; scB, LscB = ks_["scB"]; scC, LscC = ks_["scC"]
                tc = slice(i * 128, (i + 1) * 128)

                def chainA():
                    RED(stA[:, 4:5], ikx, ALU.add, [Likx], [LstA]); yield
                    TS("dve", stA[:, 5:6], stA[:, 4:5], -1.0 / 64, None, ALU.mult, None, [LstA], [LstA]); yield
                    TS("dve", cen, ikx, stA[:, 5:6], None, ALU.add, None, [Likx, LstA], [Lcen]); yield
                    TTR(scA, cen, cen, stA[:, 6:7], [Lcen], [LscA, LstA]); yield
                    RSTD(stA[:, 8:9], stA[:, 6:7], stA[:, 7:8], 1.0 / 64, [LstA], [LstA]); yield
                    TS("dve", kin2[:, 0:64], cen, stA[:, 8:9], None, ALU.mult, None, [Lcen, LstA], [Lkin2])
                    TS("dve", kin2[:, 64:128], cen, stA[:, 8:9], None, ALU.mult, None, [Lcen, LstA], [Lkin2]); yield
                    TR(b7[:, 0:128], kin2, idb, [Lkin2, Lidb], [Lpb[7]]); yield
                    ACT(kiT2[:, tc], b7[:, 0:128], AF.Identity, [Lpb[7], Lncol], [LkiT2], scale=ncol[:, 20:21], bias=ncol[:, 21:22]); yield

                def chainB():
                    TTR(scB, junk[:, 0:192], junk[:, 0:192], stB[:, 9:10], [Ljunk], [LscB, LstB]); yield
                    RSTD(stB[:, 11:12], stB[:, 9:10], stB[:, 10:11], 1.0 / 192, [LstB], [LstB]); yield
                    ACT(qn, junk[:, 0:192], AF.Copy, [Ljunk, LstB], [Lqn], scale=stB[:, 11:12]); yield
                    TR(b7[:, 128:256], qn[:, 0:128], idb, [Lqn, Lidb], [Lpb[7]])
                    TR(b7[0:64, 256:384], qn[:, 128:192], idb, [Lqn, Lidb], [Lpb[7]]); yield
                    CP("dve", qnT[:, 0, :], b7[:, 128:256], [Lpb[7]], [LqnT])
                    CP("dve", qnT[0:64, 1, :], b7[0:64, 256:384], [Lpb[7]], [LqnT]); yield
                    MM(pb[5][:, 0:384], qnT[:, 0, :], wqb[:, 0, :], True, False, [LqnT, Lwqb], [Lpb[5]])
                    MM(pb[5][:, 0:384], qnT[0:64, 1, :], wqb[0:64, 1, :], False, True, [LqnT, Lwqb], [Lpb[5]]); yield
                    q3 = pb[5][:, 0:384].rearrange("p (a b) -> p a b", a=4)
                    CP("dve", qfull[:, :, 0:64], q3[:, :, 0:64], [Lpb[5]], [Lqfull]); yield
                    rope(ks_, qfull[:, :, 64:80], qfull[:, :, 80:96], q3[:, :, 64:80], q3[:, :, 80:96], i, 4, [Lpb[5]], [Lqfull]); yield
                    for h in range(4):
                        TR(b7[0:96, 384 + h * 128:384 + (h + 1) * 128], qfull[:, h, :], idb, [Lqfull, Lidb], [Lpb[7]])
                    yield
                    CP("dve", mqs[0:96], b7[0:96, 384:896].rearrange("p (a b) -> p a b", a=4), [Lpb[7]], [Lmqs]); yield
                    DMA("sp", mqT_d[:, :, tc], mqs[0:96], [Lmqs], [LmqT]); yield

                def chainC():
                    TTR(scC, junk[:, 192:320], junk[:, 192:320], stC[:, 12:13], [Ljunk], [LscC, LstC]); yield
                    RSTD(stC[:, 14:15], stC[:, 12:13], stC[:, 13:14], 1.0 / 128, [LstC], [LstC]); yield
                    ACT(kvn, junk[:, 192:320], AF.Copy, [Ljunk, LstC], [Lkvn], scale=stC[:, 14:15]); yield
                    TR(b7[:, 896:1024], kvn, idb, [Lkvn, Lidb], [Lpb[7]]); yield
                    CP("dve", kvnT, b7[:, 896:1024], [Lpb[7]], [LkvnT]); yield
                    MM(pb[6], kvnT, wkvb, True, True, [LkvnT, Lwkvb], [Lpb[6]]); yield
                    CP("dve", mvc[:, i, :, 0:64], pb[6].rearrange("p (a b) -> p a b", a=4)[:, :, 64:128], [Lpb[6]], [Lmvc]); yield
                    for h in range(4):
                        MM(pb[6][0:64, h * 128:(h + 1) * 128], wkvb[:, h * 128:h * 128 + 64], kvnT, True, True,
                           [LkvnT, Lwkvb], [Lpb[6]])
                    yield
                    CP("dve", mkT[0:64, :, tc], pb[6][0:64, :].rearrange("p (a b) -> p a b", a=4), [Lpb[6]], [LmkT]); yield

                def chainD():
                    rope(ks_, kpe[:, 64:80].unsqueeze(1), kpe[:, 80:96].unsqueeze(1), junk[:, 320:336].unsqueeze(1),
                         junk[:, 336:352].unsqueeze(1), i, 1, [Ljunk], [Lkpe]); yield

                gens = [chainA(), chainB(), chainC()]
                while gens:
                    for g_ in list(gens):
                        try:
                            next(g_)
                        except StopIteration:
                            gens.remove(g_)
                for _ in chainD():
                    pass
                TR(b5[0:96, 768:896], kpe, idb, [Lkpe, Lidb], [Lpb[5]])
                CP("dve", mkT[64:96, :, tc], b5[64:96, 768:896].unsqueeze(1).to_broadcast([32, 4, 128]), [Lpb[5]], [LmkT])

            K_front(0)
            for i in range(NT):
                if i + 1 < NT:
                    K_front(i + 1)
                K_rest(i)

            if debug == "K":
                break
            SCs = [AR.carve(53504 + 8192 * k, [128, S], F32, f"SC{k}") for k in range(2)]
            bjunk, Lbjunk = AR.carve(69888, [128, S], BF16, "bjunk")
            TM.reset()
            xt, Lxt = TM.alloc([128, D], F32, "xtA")
            hm, Lhm = TM.alloc([128, D], F32, "hm")
            mqs2 = [TM.alloc([128, 4, 128], BF16, f"mq{k}") for k in range(2)]
            dqs2 = [TM.alloc([128, 4, 128], BF16, f"dq{k}") for k in range(2)]
            iqs2 = [TM.alloc([128, 8, 128], BF16, f"iq{k}") for k in range(2)]
            for k in range(2):
                MS("pool", dqs2[k][0], 0.0, [dqs2[k][1]])
                MS("pool", iqs2[k][0], 0.0, [iqs2[k][1]])
            iws2 = [TM.alloc([128, 8], F32, f"iw{k}") for k in range(2)]
            aws2 = [TM.alloc([128, 8], F32, f"aw{k}") for k in range(2)]
            sgs2 = [TM.alloc([128, 8], F32, f"sg{k}") for k in range(2)]
            Dh1 = TM.alloc([128, 8, 128], BF16, "Dh0")
            Dhs2 = [Dh1, Dh1]
            Rbs = [TM.alloc([128, 512], BF16, f"Rb{k}") for k in range(3)]
            mbuf, Lmbuf = TM.alloc([128, S], BF16, "mbuf")
            Ebs = [TM.alloc([128, 512], BF16, f"Eb{k}") for k in range(4)]
            thrs = [TM.alloc([128, 1], F32, f"thr{k}") for k in range(2)]
            bs, Lbs = TM.alloc([128, 8], F32, "bs")
            wk, Lwk = TM.alloc([128, KBIS], F32, "wk")
            cand, Lcand = TM.alloc([128, 1], F32, "cand")
            cnt, Lcnt = TM.alloc([128, 1], F32, "cnt")
            dl, Ldl = TM.alloc([128, 1], F32, "dl")
            tt_, Ltt = TM.alloc([128, 1], F32, "tt")
            yht2, Lyht2 = TM.alloc([128, 512], BF16, "yht2")
            mixed, Lmixed = TM.alloc([128, 512], BF16, "mixed")
            mixT, LmixT = TM.alloc([128, 8, 128], BF16, "mixT")
            rec, Lrec = TM.alloc([128, 8], F32, "rec")
            pbL = [0, 1]
            b6 = pb[6].bitcast(BF16)
            b2 = pb[2].bitcast(BF16)
            pTh = [b6[:, 0:512], b2[:, 0:512]]
            LpTh = [Lpb[6], Lpb[2]]
            LO = [Lpb[7]] * 4
            IDX_SCALE = (8.0 ** -0.5) * (64.0 ** -0.5)

            def load_q(i):
                tc = slice(i * 128, (i + 1) * 128)
                k = i % 2
                DMA("sp", mqs2[k][0][0:96], mqT_d[:, :, tc], [LmqT], [mqs2[k][1]])
                for e_ in range(2):
                    DMA("sp", dqs2[k][0][64 * e_:64 * e_ + 64, e_:4:2, :], dqT_d[64 * e_:64 * e_ + 64, :, tc], [LdqT], [dqs2[k][1]])
                    DMA("sp", iqs2[k][0][64 * e_:64 * e_ + 64, e_:8:2, :], iqT_d[64 * e_:64 * e_ + 64, :, tc], [LiqT], [iqs2[k][1]])
                DMA("sp", iws2[k][0], iws_d[tc, :], [Liws], [iws2[k][1]])

            def prep(i):
                k = i % 2
                iw, Liw = iws2[k]; aw, Law = aws2[k]; sg, Lsg = sgs2[k]; Dh, LDh = Dhs2[k]
                STT(aw, iw, -1.0, iw, ALU.mult, ALU.max, [Liw], [Law])
                TS("dve", aw, aw, IDX_SCALE, None, ALU.mult, None, [Law], [Law])
                TS("dve", sg, iw, 0.0, 2.0, ALU.is_ge, ALU.mult, [Liw], [Lsg])
                TS("dve", sg, sg, -1.0, None, ALU.add, None, [Lsg], [Lsg])
                for h in range(8):
                    TS("dve", Dh[:, h, :], idb, sg[:, h:h + 1], None, ALU.mult, None, [Lidb, Lsg], [LDh])

            def indexer(i):
                k = i % 2
                iq, Liq = iqs2[k]; iw, Liw = iws2[k]; aw, Law = aws2[k]; sg, Lsg = sgs2[k]; Dh, LDh = Dhs2[k]
                SC, LSC = SCs[k]
                n = 128 * (i + 1)
                nblk = (n + 511) // 512
                items = [(c, h) for c in range(nblk) for h in range(8)]

                def st0(j):
                    c, h = items[j]
                    w = min(512, n - c * 512)
                    pl = pbL[j % 2]
                    MM(pb[pl][:, 0:w], iq[:, h, :], kiT2[:, c * 512:c * 512 + w], True, True,
                       [Liq, LkiT2], [Lpb[pl]])
                    Rb, LRb = Rbs[j % 3]
                    ACT(Rb[:, 0:w], pb[pl][:, 0:w], AF.Relu, [Lpb[pl], Law], [LRb], scale=aw[:, h:h + 1])

                def st1(j):
                    c, h = items[j]
                    w = min(512, n - c * 512)
                    Rb, LRb = Rbs[j % 3]
                    MM(pb[3][:, 0:w], Dh[:, h, :], Rb[:, 0:w], h == 0, h == 7, [LDh, LRb], [Lpb[3]])
                    if h == 7:
                        ACT(SC[:, c * 512:c * 512 + w], pb[3][:, 0:w], AF.Copy, [Lpb[3]], [LSC])
                for s_ in range(len(items) + 2):
                    if s_ < len(items):
                        st0(s_)
                    if 0 <= s_ - 2 < len(items):
                        st1(s_ - 2)
                TT("pool", SC[:, n - 128:n], SC[:, n - 128:n], cmNEG, ALU.add, [LSC, LcmNEG], [LSC])

            def bisect(i):
                k = i % 2
                SC, LSC = SCs[k]
                thr_, Lthr = thrs[k]
                n = 128 * (i + 1)
                if n <= NSEL:
                    MS("dve", thr_, -1.0e29, [Lthr])
                    return
                RED(bs[:, 0:1], SC[:, 0:128 * i], ALU.min, [LSC], [Lbs])
                RED(bs[:, 1:2], SC[:, 0:n], ALU.max, [LSC], [Lbs])
                TT("dve", bs[:, 2:3], bs[:, 1:2], bs[:, 0:1], ALU.subtract, [Lbs], [Lbs])
                TS("dve", wk, pw2, bs[:, 2:3], None, ALU.mult, None, [Lpw2, Lbs], [Lwk])
                CP("dve", tt_, bs[:, 0:1], [Lbs], [Ltt])
                for kk_ in range(KBIS):
                    TT("dve", cand, tt_, wk[:, kk_:kk_ + 1], ALU.add, [Ltt, Lwk], [Lcand])
                    TS("dve", bjunk[:, 0:n], SC[:, 0:n], cand, 0.0, ALU.is_ge, ALU.add, [LSC, Lcand], [Lbjunk, Lcnt], accum=cnt)
                    TS("dve", dl, cnt, NSEL - 0.5, wk[:, kk_:kk_ + 1], ALU.is_ge, ALU.mult, [Lcnt, Lwk], [Ldl])
                    TT("dve", tt_, tt_, dl, ALU.add, [Ltt, Ldl], [Ltt])
                CP("dve", thr_, tt_, [Ltt], [Lthr])

            def maskbias(i):
                SC, LSC = SCs[i % 2]
                thr_, Lthr = thrs[i % 2]
                n = 128 * (i + 1)
                TS("dve", mbuf[:, 0:n], SC[:, 0:n], thr_[:, 0:1], -30000.0, ALU.is_lt, ALU.mult, [LSC, Lthr], [Lmbuf])

            SB_ = [2, 4, 5, 6]

            def attention(i, kind):
                k = i % 2
                if kind == "mla":
                    q, Lq = mqs2[k]; kc_, Lkc = mkT, LmkT; vc, Lvc = mvc, Lmvc
                    nblk = (i + 4) // 4
                    blocks = [list(range(4 * c, min(4 * c + 4, i + 1))) for c in range(nblk)]
                    scale = 96.0 ** -0.5
                else:
                    q, Lq = dqs2[k]; kc_, Lkc = dkT, LdkT; vc, Lvc = dvc, Ldvc
                    blocks = [list(range(4 * c, min(4 * c + 4, i + 1))) for c in range((i + 4) // 4)]
                    scale = 0.125
                items = [(h, b) for h in range(4) for b in range(len(blocks))]

                def st0(j):
                    h, b = items[j]
                    tiles = blocks[b]
                    w = 128 * len(tiles)
                    pS = SB_[j % 4]
                    Eb, LEb = Ebs[j % 4]
                    for jj, tj in enumerate(tiles):
                        o_ = pb[pS][:, jj * 128:(jj + 1) * 128]
                        ks_ = slice(tj * 128, (tj + 1) * 128)
                        if kind == "mla":
                            MM(o_, kc_[0:96, h, ks_], q[0:96, h, :], True, True, [Lq, Lkc], [Lpb[pS]])
                        else:
                            near = tj >= i - 1
                            MM(o_, kc_[:, h // 2, ks_], q[:, h, :], True, False, [Lq, Lkc], [Lpb[pS]])
                            MM(o_, mbuf[:, ks_], idb, False, not near, [Lidb, Lmbuf], [Lpb[pS]])
                            if near:
                                bo = 128 if tj == i else 0
                                MM(o_, biasN8[:, h, bo:bo + 128], idb, False, True, [Lidb, LbiasN8], [Lpb[pS]])
                    if kind == "mla":
                        ACT(Eb[:, 0:w], pb[pS][:, 0:w], AF.Exp, [Lpb[pS]], [LEb], scale=scale)
                        if tiles[-1] == i:
                            TT("pool", Eb[:, w - 128:w], Eb[:, w - 128:w], cm01T, ALU.mult, [LEb, Lcm01T], [LEb])
                    else:
                        nfar = sum(1 for tj in tiles if tj < i - 1)
                        if nfar > 0:
                            ACT(Eb[:, 0:128 * nfar], pb[pS][:, 0:128 * nfar], AF.Exp, [Lpb[pS], Ltabb], [LEb], scale=scale,
                                bias=tabb[:, 124 + h:125 + h])
                        if nfar < len(tiles):
                            ACT(Eb[:, 128 * nfar:w], pb[pS][:, 128 * nfar:w], AF.Exp, [Lpb[pS]], [LEb], scale=scale)

                def st2(j):
                    h, b = items[j]
                    tiles = blocks[b]
                    Eb, LEb = Ebs[j % 4]
                    for jj, tj in enumerate(tiles):
                        MM(pb[7][:, h * 65:(h + 1) * 65], Eb[:, jj * 128:(jj + 1) * 128], vc[:, tj, h, :],
                           (b == 0 and jj == 0), (b == len(blocks) - 1 and jj == len(tiles) - 1), [LEb, Lvc], [LO[h]])
                for s_ in range(len(items) + 2):
                    if s_ < len(items):
                        st0(s_)
                    if 0 <= s_ - 2 < len(items):
                        st2(s_ - 2)
                off = 0 if kind == "mla" else 256
                ro = 0 if kind == "mla" else 4
                o3 = pb[7][:, 0:260].rearrange("p (a b) -> p a b", a=4)
                ACT(rec[:, ro:ro + 4], o3[:, :, 64], AF.Ln, [LO[0]], [Lrec])
                ACT(rec[:, ro:ro + 4], rec[:, ro:ro + 4], AF.Exp, [Lrec], [Lrec], scale=-1.0)
                for h in range(4):
                    ACT(mixed[:, off + h * 64:off + (h + 1) * 64], pb[7][:, h * 65:h * 65 + 64], AF.Copy,
                        [LO[h], Lrec], [Lmixed], scale=rec[:, ro + h:ro + h + 1])

            def epilogue(i):
                tc = slice(i * 128, (i + 1) * 128)
                DMA("sp", xt, src_d[tc, :], rsrc, [Lxt])
                DMA("sp", yht2, yh_d[tc, :], [Lyh], [Lyht2])
                if debug:
                    DMA("sp", mix_d[tc, :], mixed, [Lmixed], [Lmixd])
                for kc in range(4):
                    TR(b6[:, kc * 128:(kc + 1) * 128], yht2[:, kc * 128:(kc + 1) * 128], idb, [Lyht2, Lidb], [LpTh[0]])
                for kc in range(4):
                    TR(b2[:, kc * 128:(kc + 1) * 128], mixed[:, kc * 128:(kc + 1) * 128], idb, [Lmixed, Lidb], [LpTh[1]])
                ACT(mixT[:, 0:4, :], b6[:, 0:512].rearrange("p (a b) -> p a b", a=4), AF.Copy, [LpTh[0]], [LmixT])
                ACT(mixT[:, 4:8, :], b2[:, 0:512].rearrange("p (a b) -> p a b", a=4), AF.Copy, [LpTh[1]], [LmixT])
                for n_ in range(2):
                    for kc in range(8):
                        MM(pb[4 + n_], mixT[:, kc, :], wout[:, kc, n_ * 512:(n_ + 1) * 512], kc == 0, kc == 7,
                           [LmixT, Lwout], [Lpb[4 + n_]])
                    ACT(hm[:, n_ * 512:(n_ + 1) * 512], pb[4 + n_], AF.Copy, [Lpb[4 + n_]], [Lhm])
                    TT("pool", hm[:, n_ * 512:(n_ + 1) * 512], hm[:, n_ * 512:(n_ + 1) * 512], xt[:, n_ * 512:(n_ + 1) * 512],
                       ALU.add, [Lhm, Lxt], [Lhm])
                DMA("pool", hb1_d[tc, :], hm, [Lhm], [Lhb1])

            load_q(0)
            prep(0)
            indexer(0)
            if NT > 1:
                load_q(1)
            for i in range(NT):
                if i + 1 < NT:
                    prep(i + 1)
                bisect(i)
                if i + 1 < NT:
                    indexer(i + 1)
                attention(i, "mla")
                maskbias(i)
                attention(i, "dsa")
                if i + 2 < NT:
                    load_q(i + 2)
                epilogue(i)
            if debug == "A":
                break
            w1, Lw1 = AR.carve(0, [128, 8, DFF], BF16, "w1")
            w2, Lw2 = AR.carve(32768, [128, 32, D], BF16, "w2")
            hT, LhT = AR.carve(65536, [128, 32, 256], BF16, "hT")
            load_w_rows(w1, Lw1, w1_d[li], 8, 0, DFF, ncol[:, 8:16], Lncol)
            load_w_rows(w2, Lw2, w2_d[li], 32, 0, D)
            TM.reset()
            hts = [TM.alloc([128, D], F32, f"ht{k}") for k in range(4)]
            ub, Lub = TM.alloc([128, D], BF16, "ubM")
            uT2, LuT2 = TM.alloc([128, 8, 256], BF16, "uT2")
            hos = [TM.alloc([128, D], F32, f"ho{k}") for k in range(2)]
            stat, Lstat = TM.alloc([128, 16], F32, "statM")
            junk, Ljunk = TM.alloc([128, D], F32, "junkM")
            sq, Lsq = TM.alloc([128, 256], BF16, "sq")
            sq2, Lsq2 = TM.alloc([128, 256], BF16, "sq2")
            sqs = [(sq, Lsq), (sq2, Lsq2)]
            do_fin = last and final_norm
            if do_fin:
                fnb, Lfnb = TM.alloc([128, D], F32, "fnb")
                DMA("sp", fnb, fnw_d.partition_broadcast(128), [], [Lfnb])
            b0 = pb[0].bitcast(BF16)
            def M_load(st):
                for sub in range(2):
                    i = 2 * st + sub
                    ht, Lht = hts[(st % 2) * 2 + sub]
                    DMA("sp", ht, hb1_d[i * 128:(i + 1) * 128, :], [Lhb1], [Lht])

            M_load(0)
            for st in range(NT // 2):
                if st + 1 < NT // 2:
                    M_load(st + 1)
                for sub in range(2):
                    i = 2 * st + sub
                    ht, Lht = hts[(st % 2) * 2 + sub]
                    TTR(junk, ht, ht, stat[:, 0:1], [Lht], [Ljunk, Lstat])
                    RSTD(stat[:, 2:3], stat[:, 0:1], stat[:, 1:2], 1.0 / D, [Lstat], [Lstat])
                    ACT(ub, ht, AF.Copy, [Lht, Lstat], [Lub], scale=stat[:, 2:3])
                    for kc in range(8):
                        TR(b0[:, kc * 128:(kc + 1) * 128], ub[:, kc * 128:(kc + 1) * 128], idb, [Lub, Lidb], [Lpb[0]])
                    CP("dve", uT2[:, :, sub * 128:(sub + 1) * 128], b0.rearrange("p (a b) -> p a b", a=8), [Lpb[0]], [LuT2])
                for f in range(32):
                    ph = 1 + (f % 3)
                    for kc in range(8):
                        MM(pb[ph][:, 0:256], w1[:, kc, f * 128:(f + 1) * 128], uT2[:, kc, :], kc == 0, kc == 7,
                           [Lw1, LuT2], [Lpb[ph]])
                    s_, Ls_ = sqs[f % 2]
                    ACT(s_, pb[ph][:, 0:256], AF.Relu, [Lpb[ph]], [Ls_])
                    TT("pool", hT[:, f, :], s_, s_, ALU.mult, [Ls_], [LhT])
                for sub in range(2):
                    i = 2 * st + sub
                    ht, Lht = hts[(st % 2) * 2 + sub]
                    ho, Lho = hos[sub]
                    for n_ in range(2):
                        po = 4 + sub * 2 + n_
                        for f in range(32):
                            MM(pb[po], hT[:, f, sub * 128:(sub + 1) * 128], w2[:, f, n_ * 512:(n_ + 1) * 512], f == 0, f == 31,
                               [LhT, Lw2], [Lpb[po]])
                        TT("dve", ho[:, n_ * 512:(n_ + 1) * 512], pb[po], ht[:, n_ * 512:(n_ + 1) * 512], ALU.add,
                           [Lpb[po], Lht], [Lho])
                    if do_fin:
                        TTR(junk, ho, ho, stat[:, 4:5], [Lho], [Ljunk, Lstat])
                        RSTD(stat[:, 6:7], stat[:, 4:5], stat[:, 5:6], 1.0 / D, [Lstat], [Lstat])
                        STT(junk, ho, stat[:, 6:7], fnb, ALU.mult, ALU.mult, [Lho, Lstat, Lfnb], [Ljunk])
                        DMA("sp", out_d[i * 128:(i + 1) * 128, :], junk, [Ljunk], [Lout])
                    else:
                        DMA("sp", (out_d if (last and not debug) else hb0_d)[i * 128:(i + 1) * 128, :], ho, [Lho],
                            [Lout if (last and not debug) else Lhb0])

        final = [Lout] if not debug else [Lyh, Lhb0, Lhb1, LmqT, LdqT, LiqT, Liws, Llbs, Lmixd]
        stats = P.emit(final_waits=final)
        build_nc.stats = stats
    return nc


def _rest_of_layer(env):
    raise NotImplementedError


def host_consts():
    invf = (1.0 / (10000.0 ** (np.arange(0, 32, 2, dtype=np.float32) / np.float32(32)))).astype(np.float32)
    c_invf = np.ascontiguousarray(np.broadcast_to(invf[None, :], (128, 16))).astype(np.float32)
    pw = np.array([2.0 ** -(k + 1) for k in range(KBIS)], dtype=np.float32)
    c_pw2 = np.ascontiguousarray(np.broadcast_to(pw[None, :], (128, KBIS))).astype(np.float32)
    return c_invf, c_pw2


PER_LAYER = ["attn_norm_w", "w_in", "hgrn_norm_w", "mla_q_norm_w", "mla_w_qb", "mla_kv_norm_w", "mla_w_kvb",
             "idx_k_norm_w", "idx_k_norm_b", "w_out", "mlp_norm_w", "w_mlp_in", "w_mlp_out"]
GLOBALS = ["hgrn_lb_logits", "rel_bias_table", "final_norm_w"]
FUSED = True
_NC_CACHE = {}


def _get_nc(layers, final_norm):
    key = (tuple(layers), final_norm)
    if key not in _NC_CACHE:
        _NC_CACHE[key] = build_nc(layers=tuple(layers), final_norm=final_norm, debug=False)
    return _NC_CACHE[key]


def _launch(layers, final_norm, inputs, xs):
    c_invf, c_pw2 = host_consts()
    nc = _get_nc(layers, final_norm)
    shared = {k: np.ascontiguousarray(np.asarray(inputs[k], dtype=np.float32)[list(layers)]) for k in PER_LAYER}
    for k in GLOBALS:
        shared[k] = np.ascontiguousarray(np.asarray(inputs[k], dtype=np.float32))
    pos = np.asarray(inputs["positions"]).astype(np.int32)
    maps = []
    for b in range(8):
        m = dict(shared)
        m["x"] = np.ascontiguousarray(xs[b], dtype=np.float32)
        m["pos"] = np.ascontiguousarray(pos[b])
        m["c_invf"] = c_invf
        m["c_pw2"] = c_pw2
        maps.append(m)
    res = run_bass_kernel_spmd(nc, maps, core_ids=list(range(8)))
    return [np.asarray(res.results[b]["out"], dtype=np.float32) for b in range(8)]


def kernel(**inputs):
    x = np.asarray(inputs["x"], dtype=np.float32)
    xs = [x[b] for b in range(8)]
    if FUSED:
        outs = _launch((0, 1, 2, 3), True, inputs, xs)
    else:
        for l in range(4):
            xs = _launch((l,), l == 3, inputs, xs)
        outs = xs
    return np.stack(outs, axis=0).astype(np.float32)
```

```python
import math
import numpy as np
import concourse.bass as bass
import concourse.mybir as mybir
from concourse.bass_utils import run_bass_kernel_spmd
from contextlib import ExitStack

F32 = mybir.dt.float32
BF16 = mybir.dt.bfloat16
I32 = mybir.dt.int32
AF = mybir.ActivationFunctionType
ALU = mybir.AluOpType
AX = mybir.AxisListType

ENGS = ("pe", "act", "dve", "pool", "sp")
SEM_LIMIT = 30000

S = 4096
D = 1024
NT = S // 128
NIN = 3752
DFF = 4096
EPS = 1e-6
NEG = -1.0e30
KBIS = 12
NSEL = 256


class LT:
    __slots__ = ("name", "last_w", "readers", "sem", "cnt", "dram", "burst", "wtoks")

    def __init__(self, name, dram=False):
        self.name = name
        self.last_w = None
        self.readers = []
        self.sem = None
        self.cnt = 0
        self.dram = dram
        self.burst = []
        self.wtoks = {}


class Op:
    __slots__ = ("eng", "fn", "raw", "oth", "sig", "tok", "isdma", "n", "toks")


class Prog:
    def __init__(self, nc, es):
        self.nc = nc
        self.es = es
        self.ops = []
        self.streams = {e: [] for e in ENGS}
        self.nsem = 0
        self.sem_pool = {}

    def new_sem(self, name):
        self.nsem += 1
        return self.es.enter_context(self.nc.semaphore(f"s{self.nsem}_{name}"))

    def _rec(self, eng, fn, r, w, isdma, n):
        o = Op()
        o.eng = eng; o.fn = fn; o.isdma = isdma; o.n = n; o.sig = False
        o.raw = []; o.oth = []; o.toks = []; o.tok = None
        oi = len(self.ops)
        for t in r:
            if t.dram:
                o.toks.extend(t.wtoks.values())
                t.readers.append(oi)
            else:
                if t.last_w is not None:
                    o.raw.append(t.last_w)
                t.readers.append(oi)
        for t in w:
            if t.dram:
                if t.readers:
                    t.burst = [x for x in t.readers if x != oi]
                    t.readers = []
                o.oth.extend(t.burst)
            else:
                if t.last_w is not None:
                    o.oth.append(t.last_w)
                o.oth.extend(x for x in t.readers if x != oi)
                t.last_w = oi
                t.readers = []
        if isdma:
            dst = w[0]
            key = dst.name
            if dst.dram:
                key = "src_" + [t for t in r if not t.dram][0].name
            ent = self.sem_pool.get(key)
            if ent is None:
                ent = [self.new_sem(key), 0]
                self.sem_pool[key] = ent
            ent[1] += 16 * n
            o.tok = (ent[0], ent[1])
            if dst.dram:
                dst.wtoks[id(ent[0])] = (ent[0], ent[1])
            else:
                dst.sem = ent[0]
                dst.cnt = ent[1]
        self.ops.append(o)
        self.streams[eng].append(oi)
        return oi

    def op(self, eng, fn, r=(), w=()):
        return self._rec(eng, fn, list(r), list(w), False, 0)

    def dma(self, q, fn, r=(), w=(), n=1):
        return self._rec(q, fn, list(r), list(w), True, n)

    def emit(self, final_waits=()):
        nc = self.nc
        ops = self.ops
        for oi, o in enumerate(ops):
            keep = []
            for d in o.raw:
                do = ops[d]
                if (not do.isdma) and (not o.isdma) and do.eng == o.eng and o.eng == "pe":
                    continue
                keep.append(d)
            for d in o.oth:
                do = ops[d]
                if (not do.isdma) and (not o.isdma) and do.eng == o.eng and o.eng == "pe":
                    continue
                keep.append(d)
            o.raw = sorted(set(keep))
            for d in o.raw:
                ops[d].sig = True
        for e in ENGS:
            sem = None
            cnt = 0
            for oi in self.streams[e]:
                o = ops[oi]
                if o.isdma or not o.sig:
                    continue
                if sem is None or cnt >= SEM_LIMIT:
                    sem = self.new_sem("eng_" + e)
                    cnt = 0
                cnt += 1
                o.tok = (sem, cnt)
        stats = {e: [0, 0] for e in ENGS}
        with nc.Block() as block:
            def body(ename):
                def run(eng):
                    known = {}
                    for oi in self.streams[ename]:
                        o = ops[oi]
                        need = {}
                        for d in o.raw:
                            s, v = ops[d].tok
                            if need.get(id(s), (None, 0))[1] < v:
                                need[id(s)] = (s, v)
                        for s, v in o.toks:
                            if need.get(id(s), (None, 0))[1] < v:
                                need[id(s)] = (s, v)
                        for k, (s, v) in need.items():
                            if known.get(k, 0) < v:
                                eng.wait_ge(s, v)
                                known[k] = v
                                stats[ename][1] += 1
                        ins = o.fn(eng)
                        stats[ename][0] += 1
                        if o.isdma:
                            if not isinstance(ins, (list, tuple)):
                                ins = [ins]
                            assert len(ins) == o.n, (len(ins), o.n)
                            for i_ in ins:
                                i_.then_inc(o.tok[0], 16)
                        elif o.sig:
                            ins.then_inc(o.tok[0], 1)
                    if ename == "sp":
                        for t in final_waits:
                            for (s_, v_) in t.wtoks.values():
                                eng.wait_ge(s_, v_)
                return run
            block.tensor(body("pe"))
            block.scalar(body("act"))
            block.vector(body("dve"))
            block.gpsimd(body("pool"))
            block.sync(body("sp"))
        return stats


class Arena:
    def __init__(self, tensor, elem_bytes):
        self.t = tensor
        self.eb = elem_bytes
        self.live = []
        self.bump = 0

    def carve(self, off_elems, shape, dtype, name):
        n = int(np.prod(shape[1:]))
        db = 2 if dtype == BF16 else 4
        start = off_elems * self.eb
        end = start + n * db
        assert end <= self.t.shape[1] * self.eb, (name, end)
        lt = LT(name)
        keep = []
        for (s0, e0, l0) in self.live:
            if s0 < end and start < e0:
                lt.readers.extend(l0.readers)
                if l0.last_w is not None:
                    lt.readers.append(l0.last_w)
                if s0 >= start and e0 <= end:
                    continue
            keep.append((s0, e0, l0))
        keep.append((start, end, lt))
        self.live = keep
        ap = self.t[:, off_elems:off_elems + (end - start) // self.eb]
        if dtype != ap.dtype:
            ap = ap.bitcast(dtype)
        if len(shape) == 3:
            ap = ap.rearrange("p (a b) -> p a b", a=shape[1])
        elif len(shape) == 4:
            ap = ap.rearrange("p (a b c) -> p a b c", a=shape[1], b=shape[2])
        if shape[0] < 128:
            ap = ap[0:shape[0]]
        return ap, lt

    def reset(self):
        self.bump = 0

    def alloc(self, shape, dtype, name):
        db = 2 if dtype == BF16 else 4
        n = int(np.prod(shape[1:])) * db
        n = (n + 3) // 4 * 4
        off = self.bump
        self.bump += n // self.eb
        return self.carve(off, shape, dtype, name)


def t5_thresholds():
    def bucket(n):
        if n < 16:
            return n
        nf = np.float32(max(n, 1))
        v = np.log(nf / np.float32(16)) / np.float32(math.log(128 / 16)) * np.float32(16)
        return min(16 + int(np.float32(v)), 31)
    thr = []
    for j in range(1, 32):
        n = 0
        while bucket(n) < j:
            n += 1
        thr.append(n)
    return thr


def build_nc(layers=(0, 1, 2, 3), final_norm=True, debug=False):
    nc = bass.Bass("TRN2", target_bir_lowering=False)
    dt_in = lambda n, s, d=F32: nc.dram_tensor(n, s, d, kind="ExternalInput").ap()
    x_d = dt_in("x", [S, D])
    pos_d = dt_in("pos", [S], I32)
    NLW = len(layers)
    anw_d = dt_in("attn_norm_w", [NLW, D])
    win_d = dt_in("w_in", [NLW, D, NIN])
    lbl_d = dt_in("hgrn_lb_logits", [4, 512])
    hnw_d = dt_in("hgrn_norm_w", [NLW, 128])
    qnw_d = dt_in("mla_q_norm_w", [NLW, 192])
    wqb_d = dt_in("mla_w_qb", [NLW, 192, 384])
    kvnw_d = dt_in("mla_kv_norm_w", [NLW, 128])
    wkvb_d = dt_in("mla_w_kvb", [NLW, 128, 512])
    iknw_d = dt_in("idx_k_norm_w", [NLW, 64])
    iknb_d = dt_in("idx_k_norm_b", [NLW, 64])
    tab_d = dt_in("rel_bias_table", [32, 4])
    wout_d = dt_in("w_out", [NLW, D, D])
    mnw_d = dt_in("mlp_norm_w", [NLW, D])
    w1_d = dt_in("w_mlp_in", [NLW, D, DFF])
    w2_d = dt_in("w_mlp_out", [NLW, DFF, D])
    fnw_d = dt_in("final_norm_w", [D])
    invf_d = dt_in("c_invf", [128, 16])
    pw2_d = dt_in("c_pw2", [128, KBIS])
    out_d = nc.dram_tensor("out", [S, D], F32, kind="ExternalOutput").ap()
    skind = "ExternalOutput" if debug else "Internal"
    sc = lambda n, s, d: nc.dram_tensor(n, s, d, kind=skind).ap()
    hb0_d = sc("hb0", [S, D], F32)
    hb1_d = sc("hb1", [S, D], F32)
    yh_d = sc("yh", [S, 512], BF16)
    mqT_d = sc("mqT", [96, 4, S], BF16)
    dqT_d = sc("dqT", [128, 2, S], BF16)
    iqT_d = sc("iqT", [128, 4, S], BF16)
    iws_d = sc("iws", [S, 8], F32)
    lbs_d = sc("lbs", [4, 128, 512], F32)
    mix_d = sc("mixdbg", [S, 512], BF16)

    with ExitStack() as es:
        P = Prog(nc, es)
        sbt = lambda n, s, d: es.enter_context(nc.sbuf_tensor(n, s, d))
        AR = Arena(sbt("arena", [128, 75328], BF16), 2)
        TM = Arena(sbt("tmp", [128, 10240], F32), 4)
        pbt = [es.enter_context(nc.psum_tensor(f"pb{k}", [128, 512], F32)) for k in range(8)]
        pb = [t[:] for t in pbt]
        Lpb = [LT(f"pb{k}") for k in range(8)]

        def cst(name, shape, dtype):
            return sbt(name, shape, dtype)[:], LT(name)

        def MM(out, lhsT, rhs, start, stop, r, w):
            P.op("pe", lambda e: e.matmul(out, lhsT=lhsT, rhs=rhs, start=start, stop=stop, skip_group_check=True), r, w)

        def TR(out, in_, ident, r, w):
            P.op("pe", lambda e: e.transpose(out=out, in_=in_, identity=ident), r, w)

        def ACT(out, in_, func, r, w, scale=None, bias=None):
            kw = {}
            if scale is not None:
                kw["scale"] = scale
            if bias is not None:
                kw["bias"] = bias
            P.op("act", lambda e: e.activation(out=out, in_=in_, func=func, **kw), r, w)

        def TS(eng, out, in0, s1, s2, op0, op1, r, w, accum=None):
            kw = {}
            if op1 is not None:
                kw["op1"] = op1
            if accum is not None:
                kw["accum_out"] = accum
            P.op(eng, lambda e: e.tensor_scalar(out=out, in0=in0, scalar1=s1, scalar2=s2, op0=op0, **kw), r, w)

        def TT(eng, out, in0, in1, op, r, w):
            P.op(eng, lambda e: e.tensor_tensor(out=out, in0=in0, in1=in1, op=op), r, w)

        def STT(out, in0, scalar, in1, op0, op1, r, w):
            P.op("dve", lambda e: e.scalar_tensor_tensor(out=out, in0=in0, scalar=scalar, in1=in1, op0=op0, op1=op1), r, w)

        def TTR(out, in0, in1, accum, r, w):
            P.op("dve", lambda e: e.scalar_tensor_tensor(out=out, in0=in0, scalar=1.0, in1=in1, op0=ALU.mult,
                                                         op1=ALU.mult, accum_out=accum), r, w)

        def RSTD(out, ss, tmp, inv_w, r, w):
            ACT(tmp, ss, AF.Ln, r + [Lcpi], w, scale=inv_w, bias=cpi[:, 1:2])
            ACT(out, tmp, AF.Exp, w, w, scale=-0.5)

        def RED(out, in_, op, r, w):
            P.op("dve", lambda e: e.tensor_reduce(out=out, in_=in_, axis=AX.X, op=op), r, w)

        def RCP(out, in_, r, w):
            P.op("dve", lambda e: e.reciprocal(out=out, in_=in_), r, w)

        def CP(eng, out, in_, r, w):
            P.op(eng, lambda e: e.tensor_copy(out=out, in_=in_), r, w)

        def MS(eng, ap, val, w):
            P.op(eng, lambda e: e.memset(ap, val), (), w)

        def ASEL(out, in_, pattern, cmp, fill, base, cm, r, w):
            P.op("pool", lambda e: e.affine_select(out=out, in_=in_, pattern=pattern, compare_op=cmp, fill=fill,
                                                   base=base, channel_multiplier=cm), r, w)

        def DMA(q, out, in_, r, w):
            P.dma(q, lambda e: e.dma_start(out=out, in_=in_), r, w)

        def DMAS(q, out, in_, r, w):
            P.dma(q, lambda e: e.dma_start(out=out, in_=in_, allow_slow_non_contiguous=True), r, w)

        Lhb0 = LT("hb0", True); Lhb1 = LT("hb1", True); Lyh = LT("yh", True)
        LmqT = LT("mqT", True); LdqT = LT("dqT", True); LiqT = LT("iqT", True)
        Liws = LT("iws", True); Llbs = LT("lbs", True); Lout = LT("out", True); Lmixd = LT("mixdbg", True)

        idf, Lidf = cst("idf", [128, 128], F32)
        idb, Lidb = cst("idb", [128, 128], BF16)
        Bd, LBd = cst("Bd", [128, 128], F32)
        Lc, LLc = cst("Lc", [128, 128], F32)
        Uc, LUc = cst("Uc", [128, 128], F32)
        Ind, LInd = cst("Ind", [128, 4], F32)
        cosT, Lcos = cst("cosT", [128, NT, 16], F32)
        sinT, Lsin = cst("sinT", [128, NT, 16], F32)
        invf, Linvf = cst("invf", [128, 16], F32)
        pw2, Lpw2 = cst("pw2", [128, KBIS], F32)
        tabb, Ltabb = cst("tabb", [128, 128], F32)
        dlt, Ldlt = cst("dlt", [128, 124], F32)
        biasN, LbiasN = cst("biasN", [128, 4, 256], F32)
        biasN8, LbiasN8 = cst("biasN8", [128, 4, 256], BF16)
        lbB, LlbB = cst("lbB", [128, 512], F32)
        omlbB, LomlbB = cst("omlbB", [128, 512], F32)
        ncol, Lncol = cst("ncol", [128, 24], F32)
        cpi, Lcpi = cst("cpi", [128, 2], F32)
        vst, Lvst = cst("vst", [24, 128], F32)

        MS("pool", idf, 0.0, [Lidf])
        ASEL(idf, idf, [[-1, 128]], ALU.not_equal, 1.0, 0, 1, [Lidf], [Lidf])
        CP("dve", idb, idf, [Lidf], [Lidb])
        MS("pool", Bd, 1.0, [LBd])
        for c in range(4):
            v = Bd[:, 32 * c:32 * c + 32]
            ASEL(v, v, [[0, 32]], ALU.is_ge, 0.0, -32 * c, 1, [LBd], [LBd])
            ASEL(v, v, [[0, 32]], ALU.is_ge, 0.0, 32 * c + 31, -1, [LBd], [LBd])
        ASEL(Lc, Bd, [[1, 128]], ALU.is_ge, 0.0, 0, -1, [LBd], [LLc])
        TT("dve", Uc, Bd, Lc, ALU.subtract, [LBd, LLc], [LUc])
        for c in range(4):
            CP("dve", Ind[:, c:c + 1], Bd[:, 32 * c:32 * c + 1], [LBd], [LInd])
        cm01f, Lcm01f = cst("cm01f", [128, 128], F32)
        cm01, Lcm01 = cst("cm01", [128, 128], BF16)
        cmNEG, LcmNEG = cst("cmNEG", [128, 128], F32)
        MS("pool", cm01f, 1.0, [Lcm01f])
        ASEL(cm01f, cm01f, [[-1, 128]], ALU.is_ge, 0.0, 0, 1, [Lcm01f], [Lcm01f])
        CP("dve", cm01, cm01f, [Lcm01f], [Lcm01])
        cm01Tf, Lcm01Tf = cst("cm01Tf", [128, 128], F32)
        cm01T, Lcm01T = cst("cm01T", [128, 128], BF16)
        MS("pool", cm01Tf, 1.0, [Lcm01Tf])
        ASEL(cm01Tf, cm01Tf, [[1, 128]], ALU.is_ge, 0.0, 0, -1, [Lcm01Tf], [Lcm01Tf])
        CP("dve", cm01T, cm01Tf, [Lcm01Tf], [Lcm01T])
        MS("pool", cmNEG, 0.0, [LcmNEG])
        ASEL(cmNEG, cmNEG, [[-1, 128]], ALU.is_ge, NEG, 0, 1, [LcmNEG], [LcmNEG])
        MS("pool", cpi[:, 0:1], math.pi, [Lcpi])
        MS("pool", cpi[:, 1:2], EPS, [Lcpi])
        DMA("sp", invf, invf_d, [], [Linvf])
        DMA("sp", pw2, pw2_d, [], [Lpw2])
        DMA("sp", tabb, tab_d.rearrange("a b -> (a b)").partition_broadcast(128), [], [Ltabb])
        TT("dve", dlt, tabb[:, 4:128], tabb[:, 0:124], ALU.subtract, [Ltabb], [Ldlt])

        TM.reset()
        posi, Lposi = TM.alloc([128, NT], I32, "posi")
        posf, Lposf = TM.alloc([128, NT], F32, "posf")
        ang, Lang = TM.alloc([128, NT, 16], F32, "ang")
        ang2, Lang2 = TM.alloc([128, NT, 16], F32, "ang2")
        posr, Lposr = TM.alloc([NT, 128], I32, "posr")
        posrf, Lposrf = TM.alloc([NT, 128], F32, "posrf")
        DMA("sp", posr, pos_d.rearrange("(n p) -> n p", p=128), [], [Lposr])
        CP("dve", posrf, posr, [Lposr], [Lposrf])
        TR(pb[0][:, 0:NT], posrf, idf[0:NT, 0:NT], [Lposrf, Lidf], [Lpb[0]])
        CP("dve", posf, pb[0][:, 0:NT], [Lpb[0]], [Lposf])
        TT("dve", ang, posf.unsqueeze(2).to_broadcast([128, NT, 16]),
           invf.unsqueeze(1).to_broadcast([128, NT, 16]), ALU.mult, [Lposf, Linvf], [Lang])
        TWO_PI = 2.0 * math.pi
        angi, Langi = TM.alloc([128, NT, 16], I32, "angi")
        angk, Langk = TM.alloc([128, NT, 16], F32, "angk")
        TS("dve", ang2, ang, math.pi / 2, None, ALU.add, None, [Lang], [Lang2])

        def reduce_pi(a, La):
            TS("dve", angk, a, 1.0 / TWO_PI, None, ALU.mult, None, [La], [Langk])
            CP("dve", angi, angk, [Langk], [Langi])
            CP("dve", angk, angi, [Langi], [Langk])
            STT(a, angk, -TWO_PI, a, ALU.mult, ALU.add, [Langk, La], [La])
            TS("dve", angk, a, math.pi, -TWO_PI, ALU.is_gt, ALU.mult, [La], [Langk])
            TT("dve", a, a, angk, ALU.add, [La, Langk], [La])
            TS("dve", angk, a, -math.pi, TWO_PI, ALU.is_lt, ALU.mult, [La], [Langk])
            TT("dve", a, a, angk, ALU.add, [La, Langk], [La])
        reduce_pi(ang, Lang)
        reduce_pi(ang2, Lang2)
        ACT(sinT, ang, AF.Sin, [Lang], [Lsin])
        ACT(cosT, ang2, AF.Sin, [Lang2], [Lcos])

        dti, Ldti = TM.alloc([128, 256], I32, "dti")
        dtf, Ldtf = TM.alloc([128, 256], F32, "dtf")
        stp, Lstp = TM.alloc([128, 256], F32, "stp")
        P.op("pool", lambda e: e.iota(dti[:, 0:128], pattern=[[-1, 128]], base=128, channel_multiplier=1), [], [Ldti])
        P.op("pool", lambda e: e.iota(dti[:, 128:256], pattern=[[-1, 128]], base=0, channel_multiplier=1), [Ldti], [Ldti])
        CP("dve", dtf, dti, [Ldti], [Ldtf])
        for h in range(4):
            CP("dve", biasN[:, h, :], tabb[:, h:h + 1].to_broadcast([128, 256]), [Ltabb], [LbiasN])
        thr = t5_thresholds()
        for j in range(1, 32):
            TS("dve", stp, dtf, float(thr[j - 1]) - 0.5, None, ALU.is_ge, None, [Ldtf], [Lstp])
            for h in range(4):
                STT(biasN[:, h, :], stp, dlt[:, (j - 1) * 4 + h:(j - 1) * 4 + h + 1], biasN[:, h, :],
                    ALU.mult, ALU.add, [Lstp, Ldlt, LbiasN], [LbiasN])

        TS("dve", biasN8, biasN, 8.0, None, ALU.mult, None, [LbiasN], [LbiasN8])
        lg, Llg = TM.alloc([128, 4, 512], F32, "lg")
        lsum, Llsum = TM.alloc([128, 512], F32, "lsum")
        lacc, Llacc = TM.alloc([128, 512], F32, "lacc")
        DMA("sp", lg, lbl_d.rearrange("a b -> (a b)").partition_broadcast(128).rearrange("p (a b) -> p a b", a=4),
            [], [Llg])
        ACT(lg, lg, AF.Exp, [Llg], [Llg])
        TT("dve", lsum, lg[:, 0, :], lg[:, 1, :], ALU.add, [Llg], [Llsum])
        TT("dve", lsum, lsum, lg[:, 2, :], ALU.add, [Llg, Llsum], [Llsum])
        TT("dve", lsum, lsum, lg[:, 3, :], ALU.add, [Llg, Llsum], [Llsum])
        RCP(lsum, lsum, [Llsum], [Llsum])
        MS("dve", lacc, 0.0, [Llacc])
        for l in range(4):
            if l > 0:
                TT("dve", lg[:, l, :], lg[:, l, :], lsum, ALU.mult, [Llg, Llsum], [Llg])
                TT("dve", lacc, lacc, lg[:, l, :], ALU.add, [Llacc, Llg], [Llacc])
            DMA("sp", lbs_d[l], lacc, [Llacc], [Llbs])

        ISQ = 128.0 ** -0.5

        def rms_to_uT(xt, Lxt, width_chunks, ub, Lub, uT, LuT, bank, Lbank, stat, Lstat, junk, Ljunk):
            W = width_chunks * 128
            TTR(junk[:, 0:W], xt, xt, stat[:, 0:1], [Lxt], [Ljunk, Lstat])
            RSTD(stat[:, 2:3], stat[:, 0:1], stat[:, 1:2], 1.0 / W, [Lstat], [Lstat])
            ACT(ub, xt, AF.Copy, [Lxt, Lstat], [Lub], scale=stat[:, 2:3])
            bb = bank.bitcast(BF16)
            for kc in range(width_chunks):
                TR(bb[:, kc * 128:(kc + 1) * 128], ub[:, kc * 128:(kc + 1) * 128], idb, [Lub, Lidb], [Lbank])
            CP("dve", uT, bb[:, 0:W].rearrange("p (a b) -> p a b", a=width_chunks), [Lbank], [LuT])

        def load_w_rows(dst, Ldst, src2d, nrows_chunks, col0, ncols, scale_col=None, Lsc=None, q="pool"):
            P.dma(q, lambda e: [e.dma_start(out=dst[:, kc, :], in_=src2d[kc * 128:(kc + 1) * 128, col0:col0 + ncols])
                                for kc in range(nrows_chunks)], [], [Ldst], n=nrows_chunks)
            if scale_col is not None:
                for kc in range(nrows_chunks):
                    TS("dve", dst[:, kc, :], dst[:, kc, :], scale_col[:, kc:kc + 1], None, ALU.mult, None,
                       [Ldst, Lsc], [Ldst])

        for li, l in enumerate(layers):
            first = (li == 0)
            last = (li == len(layers) - 1)
            src_d = x_d if first else hb0_d
            Lsrc = None if first else Lhb0
            rsrc = [] if first else [Lhb0]

            MS("pool", vst, 0.0, [Lvst])
            DMA("sp", vst[0:8, :], anw_d[li].rearrange("(k p) -> k p", p=128), [], [Lvst])
            DMA("sp", vst[8:16, :], mnw_d[li].rearrange("(k p) -> k p", p=128), [], [Lvst])
            DMA("sp", vst[16:17, :], qnw_d[li, 0:128].rearrange("(k p) -> k p", p=128), [], [Lvst])
            DMA("sp", vst[17:18, 0:64], qnw_d[li, 128:192].rearrange("(k p) -> k p", p=64), [], [Lvst])
            DMA("sp", vst[18:19, :], kvnw_d[li].rearrange("(k p) -> k p", p=128), [], [Lvst])
            DMA("sp", vst[19:20, :], hnw_d[li].rearrange("(k p) -> k p", p=128), [], [Lvst])
            DMA("sp", vst[20:21, 0:64], iknw_d[li].rearrange("(k p) -> k p", p=64), [], [Lvst])
            DMA("sp", vst[20:21, 64:128], iknw_d[li].rearrange("(k p) -> k p", p=64), [], [Lvst])
            DMA("sp", vst[21:22, 0:64], iknb_d[li].rearrange("(k p) -> k p", p=64), [], [Lvst])
            DMA("sp", vst[21:22, 64:128], iknb_d[li].rearrange("(k p) -> k p", p=64), [], [Lvst])
            TR(pb[0][:, 0:24], vst, idf[0:24, 0:24], [Lvst, Lidf], [Lpb[0]])
            CP("dve", ncol, pb[0][:, 0:24], [Lpb[0]], [Lncol])
            DMA("sp", lbB, lbs_d[l], [Llbs], [LlbB])
            TS("dve", omlbB, lbB, -1.0, 1.0, ALU.mult, ALU.add, [LlbB], [LomlbB])

            winH, LwinH = AR.carve(45312, [128, 8, 2048], BF16, "winH")
            load_w_rows(winH, LwinH, win_d[li], 8, 0, 2048, ncol[:, 0:8], Lncol)
            TM.reset()
            xts = [TM.alloc([128, D], F32, f"xt{k}") for k in range(3)]
            ub, Lub = TM.alloc([128, D], BF16, "ub")
            uTs = [TM.alloc([128, 8, 128], BF16, f"uT{k}") for k in range(3)]
            stats_ = [TM.alloc([128, 16], F32, f"stat{k}") for k in range(2)]
            statF = [TM.alloc([128, 4], F32, f"statF{k}") for k in range(3)]
            AR.bump = 0
            P3 = [AR.alloc([128, 2048], F32, f"Hprf{k}") for k in range(3)]
            HS = []
            for k in range(2):
                d_ = {}
                for nm, shp, dt_ in [("tA", [128, 512], F32),
                                     ("tB", [128, 512], F32), ("tC", [128, 512], F32), ("logf", [128, 512], F32),
                                     ("kk", [128, 512], F32), ("sil", [128, 512], F32), ("dcy", [128, 16], F32),
                                     ("qd", [128, 512], BF16), ("ki", [128, 512], BF16), ("ks", [128, 4, 512], BF16),
                                     ("vb", [128, 512], BF16), ("kiT", [128, 4, 128], BF16), ("qdT", [128, 4, 128], BF16),
                                     ("ATb", [128, 4, 128], BF16), ("yht", [128, 512], BF16),
                                     ("qdTc0", [128, 4, 128], BF16), ("qdTc1", [128, 4, 128], BF16),
                                     ("qdTc2", [128, 4, 128], BF16), ("qdTc3", [128, 4, 128], BF16)]:
                    d_[nm] = AR.alloc(shp, dt_, f"H{nm}{k}")
                HS.append(d_)
                for c in range(4):
                    MS("pool", d_[f"qdTc{c}"][0], 0.0, [d_[f"qdTc{c}"][1]])
            Sf3, LSf3 = AR.alloc([128, 4, 128], F32, "Sf3")
            Sb3, LSb3 = AR.alloc([128, 4, 128], BF16, "Sb3")
            MS("pool", Sf3, 0.0, [LSf3])
            MS("pool", Sb3, 0.0, [LSb3])

            def H_front(i):
                xt, Lxt = xts[i % 3]
                uT, LuT = uTs[i % 3]
                st_, Lst_ = statF[i % 3]
                prf, Lprf = P3[i % 3]
                DMA("sp", xt, src_d[i * 128:(i + 1) * 128, :], rsrc, [Lxt])
                rms_to_uT(xt, Lxt, 8, ub, Lub, uT, LuT, pb[0], Lpb[0], st_, Lst_, prf, Lprf)
                for n in range(4):
                    bk = 1 + (n % 2)
                    for kc in range(8):
                        MM(pb[bk], uT[:, kc, :], winH[:, kc, n * 512:(n + 1) * 512], kc == 0, kc == 7,
                           [LuT, LwinH], [Lpb[bk]])
                    ACT(prf[:, n * 512:(n + 1) * 512], pb[bk], AF.Copy, [Lpb[bk]], [Lprf])

            def H_get(i):
                hs = HS[i % 2]
                return hs

            def H_mid(i, part):
                hs = HS[i % 2]
                prf, Lprf = P3[i % 3]; tA, LtA = hs["tA"]; tB, LtB = hs["tB"]; tC, LtC = hs["tC"]
                logf, Llogf = hs["logf"]; kk, Lkk = hs["kk"]; sil, Lsil = hs["sil"]; dcy, Ldcy = hs["dcy"]
                qd, Lqd = hs["qd"]; ki, Lki = hs["ki"]; ks, Lks = hs["ks"]; vb, Lvb = hs["vb"]
                kiT, LkiT = hs["kiT"]; qdT, LqdT = hs["qdT"]; ATb, LATb = hs["ATb"]
                qdTc = [hs[f"qdTc{c}"] for c in range(4)]
                hq = prf[:, 0:512]; hf = prf[:, 512:1024]; hi = prf[:, 1024:1536]; hg = prf[:, 1536:2048]
                if part == 0:
                    ACT(tA, hf, AF.Sigmoid, [Lprf], [LtA])
                    ACT(sil, hg, AF.Silu, [Lprf], [Lsil])
                    TT("dve", tA, tA, omlbB, ALU.mult, [LtA, LomlbB], [LtA])
                    TT("dve", tA, tA, lbB, ALU.add, [LtA, LlbB], [LtA])
                    TS("dve", kk, tA, -1.0, 1.0, ALU.mult, ALU.add, [LtA], [Lkk])
                    ACT(logf, tA, AF.Ln, [LtA], [Llogf])
                    MM(pb[3], Lc, logf, True, True, [LLc, Llogf], [Lpb[3]])
                    MM(pb[4], Uc, logf, True, True, [LUc, Llogf], [Lpb[4]])
                    for h in range(4):
                        MM(pb[5][:, h * 4:(h + 1) * 4], logf[:, h * 128:(h + 1) * 128], Ind, True, True,
                           [Llogf, LInd], [Lpb[5]])
                elif part == 1:
                    ACT(dcy, pb[5][:, 0:16], AF.Exp, [Lpb[5]], [Ldcy])
                    ACT(tB, pb[3], AF.Exp, [Lpb[3]], [LtB])
                    STT(qd, hq, ISQ, tB, ALU.mult, ALU.mult, [Lprf, LtB], [Lqd])
                    ACT(tC, pb[3], AF.Exp, [Lpb[3]], [LtC], scale=-1.0)
                    TT("dve", ki, kk, tC, ALU.mult, [Lkk, LtC], [Lki])
                    ACT(tB, pb[4], AF.Exp, [Lpb[4]], [LtB])
                    TT("dve", tC, kk, tB, ALU.mult, [Lkk, LtB], [LtC])
                    for c in range(4):
                        ACT(ks[:, c, :], tC, AF.Copy, [LtC, LInd], [Lks], scale=Ind[:, c:c + 1])
                    ACT(vb, hi, AF.Copy, [Lprf], [Lvb])
                elif part == 2:
                    b6_ = pb[6].bitcast(BF16)
                    for h in range(4):
                        TR(b6_[:, h * 128:(h + 1) * 128], qd[:, h * 128:(h + 1) * 128], idb, [Lqd, Lidb], [Lpb[6]])
                        TR(b6_[:, 512 + h * 128:512 + (h + 1) * 128], ki[:, h * 128:(h + 1) * 128], idb, [Lki, Lidb], [Lpb[6]])
                    b6q = b6_[:, 0:512].rearrange("p (a b) -> p a b", a=4)
                    CP("dve", qdT, b6q, [Lpb[6]], [LqdT])
                    CP("dve", kiT, b6_[:, 512:1024].rearrange("p (a b) -> p a b", a=4), [Lpb[6]], [LkiT])
                    for c in range(4):
                        ACT(qdTc[c][0][:, :, 32 * c:32 * c + 32], qdT[:, :, 32 * c:32 * c + 32], AF.Copy, [LqdT], [qdTc[c][1]])
                else:
                    for h in range(4):
                        MM(pb[3][:, h * 128:(h + 1) * 128], kiT[:, h, :], qdT[:, h, :], True, True, [LkiT, LqdT], [Lpb[3]])
                    TT("dve", ATb, pb[3].rearrange("p (a b) -> p a b", a=4), Lc.unsqueeze(1).to_broadcast([128, 4, 128]),
                       ALU.mult, [Lpb[3], LLc], [LATb])

            def H_tail(i, c):
                hs = HS[i % 2]
                dcy, Ldcy = hs["dcy"]; ks, Lks = hs["ks"]; vb, Lvb = hs["vb"]; ATb, LATb = hs["ATb"]
                qdTc = [hs[f"qdTc{cc}"] for cc in range(4)]
                dcy3 = dcy.rearrange("p (a b) -> p a b", a=4)
                if c == 0:
                    for h in range(4):
                        MM(pb[7][:, h * 128:(h + 1) * 128], ATb[:, h, :], vb[:, h * 128:(h + 1) * 128], h == 0, False,
                           [LATb, Lvb], [Lpb[7]])
                for h in range(4):
                    MM(pb[7][:, h * 128:(h + 1) * 128], qdTc[c][0][:, h, :], Sb3[:, h, :], False, c == 3,
                       [qdTc[c][1], LSb3], [Lpb[7]])
                for h in range(4):
                    MM(pb[1][:, h * 128:(h + 1) * 128], ks[:, c, h * 128:(h + 1) * 128], vb[:, h * 128:(h + 1) * 128],
                       True, True, [Lks, Lvb], [Lpb[1]])
                TT("dve", Sf3, Sf3, dcy3[:, :, c:c + 1].to_broadcast([128, 4, 128]), ALU.mult, [LSf3, Ldcy], [LSf3])
                TT("dve", Sf3, Sf3, pb[1].rearrange("p (a b) -> p a b", a=4), ALU.add, [LSf3, Lpb[1]], [LSf3])
                CP("dve", Sb3, Sf3, [LSf3], [LSb3])

            def H_out(i):
                st_, Lst_ = stats_[i % 2]
                hs = HS[i % 2]
                tC, LtC = hs["tC"]; tB, LtB = hs["tB"]; sil, Lsil = hs["sil"]; yht, Lyht = hs["yht"]
                ACT(tB, pb[7], AF.Copy, [Lpb[7]], [LtB])
                for h in range(4):
                    TTR(tC[:, h * 128:(h + 1) * 128], tB[:, h * 128:(h + 1) * 128], tB[:, h * 128:(h + 1) * 128],
                        st_[:, 4 + h:5 + h], [LtB], [LtC, Lst_])
                RSTD(st_[:, 12:16], st_[:, 4:8], st_[:, 8:12], 1.0 / 128, [Lst_], [Lst_])
                for h in range(4):
                    STT(yht[:, h * 128:(h + 1) * 128], tB[:, h * 128:(h + 1) * 128], st_[:, 12 + h:13 + h],
                        sil[:, h * 128:(h + 1) * 128], ALU.mult, ALU.mult, [LtB, Lst_, Lsil], [Lyht])
                DMA("sp", yh_d[i * 128:(i + 1) * 128, :], yht, [Lyht], [Lyh])

            H_front(0)
            if NT > 1:
                H_front(1)
            for p_ in range(4):
                H_mid(0, p_)
            for i in range(NT):
                if i + 2 < NT:
                    H_front(i + 2)
                for c in range(4):
                    if i + 1 < NT:
                        H_mid(i + 1, c)
                    H_tail(i, c)
                H_out(i)

            if debug == "H":
                break
            mkT, LmkT = AR.carve(0, [128, 4, S], BF16, "mkT")
            mvc, Lmvc = AR.carve(16384, [128, NT, 4, 65], BF16, "mvc")
            dkT, LdkT = AR.carve(24704, [128, 2, S], BF16, "dkT")
            dvc, Ldvc = AR.carve(32896, [128, NT, 4, 65], BF16, "dvc")
            kiT2, LkiT2 = AR.carve(41216, [128, S], BF16, "kiT2")
            wout, Lwout = AR.carve(45312, [128, 8, D], BF16, "wout")
            winK, LwinK = AR.carve(53504, [128, 8, 1704], BF16, "winK")
            wqb, Lwqb = AR.carve(67136, [128, 2, 384], BF16, "wqb")
            wkvb, Lwkvb = AR.carve(67904, [128, 512], BF16, "wkvb")
            load_w_rows(winK, LwinK, win_d[li], 8, 2048, 1704, ncol[:, 0:8], Lncol)
            DMA("pool", wqb[:, 0, :], wqb_d[li, 0:128, :], [], [Lwqb])
            DMA("pool", wqb[0:64, 1, :], wqb_d[li, 128:192, :], [], [Lwqb])
            TS("dve", wqb[:, 0, :], wqb[:, 0, :], ncol[:, 16:17], None, ALU.mult, None, [Lwqb, Lncol], [Lwqb])
            TS("dve", wqb[0:64, 1, :], wqb[0:64, 1, :], ncol[0:64, 17:18], None, ALU.mult, None, [Lwqb, Lncol], [Lwqb])
            DMA("pool", wkvb, wkvb_d[li], [], [Lwkvb])
            TS("dve", wkvb, wkvb, ncol[:, 18:19], None, ALU.mult, None, [Lwkvb, Lncol], [Lwkvb])
            load_w_rows(wout, Lwout, wout_d[li], 8, 0, D)
            for kc in range(4):
                TS("dve", wout[:, kc, :], wout[:, kc, :], ncol[:, 19:20], None, ALU.mult, None, [Lwout, Lncol], [Lwout])
            MS("pool", mvc[:, :, :, 64:65], 1.0, [Lmvc])
            MS("pool", dvc[:, :, :, 64:65], 1.0, [Ldvc])

            TM.reset()
            xts = [TM.alloc([128, D], F32, f"xt{k}") for k in range(2)]
            ub, Lub = TM.alloc([128, D], BF16, "ub")
            uTs = [TM.alloc([128, 8, 128], BF16, f"uT{k}") for k in range(2)]
            KS = []
            for k in range(2):
                d_ = {}
                for nm, shp, dt_ in [("stat", [128, 16], F32), ("junk", [128, D], F32), ("dqs", [128, 2, 128], BF16),
                                     ("iqs", [128, 4, 128], BF16), ("mqs", [128, 4, 128], BF16), ("iwt", [128, 8], F32),
                                     ("ikx", [128, 64], F32), ("cen", [128, 64], F32), ("kin2", [128, 128], BF16),
                                     ("qn", [128, 192], BF16), ("qnT", [128, 2, 128], BF16), ("kvn", [128, 128], BF16),
                                     ("kvnT", [128, 128], BF16), ("qfull", [128, 4, 96], BF16), ("kpe", [128, 96], BF16),
                                     ("r1", [128, 4, 16], F32), ("r2", [128, 4, 16], F32),
                                     ("stA", [128, 16], F32), ("stB", [128, 16], F32), ("stC", [128, 16], F32),
                                     ("scA", [128, 64], F32), ("scB", [128, 192], F32), ("scC", [128, 128], F32)]:
                    d_[nm] = TM.alloc(shp, dt_, f"K{nm}{k}")
                KS.append(d_)
                MS("pool", d_["kpe"][0], 0.0, [d_["kpe"][1]])

            def rope(ks_, dst1, dst2, x1, x2, i, nh, rr, ww):
                r1, Lr1 = ks_["r1"]; r2, Lr2 = ks_["r2"]
                cs = cosT[:, i, :].unsqueeze(1).to_broadcast([128, nh, 16])
                sn = sinT[:, i, :].unsqueeze(1).to_broadcast([128, nh, 16])
                a1 = r1[:, 0:nh, :]; a2 = r2[:, 0:nh, :]
                TT("dve", a1, x1, cs, ALU.mult, rr + [Lcos], [Lr1])
                TT("dve", a2, x2, sn, ALU.mult, rr + [Lsin], [Lr2])
                TT("dve", dst1, a1, a2, ALU.subtract, [Lr1, Lr2], ww)
                TT("dve", a1, x2, cs, ALU.mult, rr + [Lcos], [Lr1])
                TT("dve", a2, x1, sn, ALU.mult, rr + [Lsin], [Lr2])
                TT("dve", dst2, a1, a2, ALU.add, [Lr1, Lr2], ww)

            b7 = pb[7].bitcast(BF16)
            b5 = pb[5].bitcast(BF16)

            def K_front(i):
                ks_ = KS[i % 2]
                xt, Lxt = xts[i % 2]
                uT, LuT = uTs[i % 2]
                stat, Lstat = ks_["stat"]; junk, Ljunk = ks_["junk"]
                dqs, Ldqs = ks_["dqs"]; iqs, Liqs = ks_["iqs"]; iwt, Liwt = ks_["iwt"]; ikx, Likx = ks_["ikx"]
                tc = slice(i * 128, (i + 1) * 128)
                DMA("sp", xt, src_d[tc, :], rsrc, [Lxt])
                rms_to_uT(xt, Lxt, 8, ub, Lub, uT, LuT, pb[0], Lpb[0], stat, Lstat, junk, Ljunk)
                for kc in range(8):
                    MM(pb[1][:, 0:352], uT[:, kc, :], winK[:, kc, 0:352], kc == 0, kc == 7, [LuT, LwinK], [Lpb[1]])
                for kc in range(8):
                    MM(pb[2][:, 0:256], uT[:, kc, :], winK[:, kc, 864:1120], kc == 0, kc == 7, [LuT, LwinK], [Lpb[2]])
                for kc in range(8):
                    MM(pb[2][:, 256:328], uT[:, kc, :], winK[:, kc, 1632:1704], kc == 0, kc == 7, [LuT, LwinK], [Lpb[2]])
                for c in range(4):
                    for kc in range(8):
                        MM(pb[3][:, c * 128:(c + 1) * 128], winK[:, kc, 352 + c * 128:352 + (c + 1) * 128], uT[:, kc, :],
                           kc == 0, kc == 7, [LuT, LwinK], [Lpb[3]])
                for c in range(4):
                    for kc in range(8):
                        MM(pb[4][:, c * 128:(c + 1) * 128], winK[:, kc, 1120 + c * 128:1120 + (c + 1) * 128], uT[:, kc, :],
                           kc == 0, kc == 7, [LuT, LwinK], [Lpb[4]])
                ACT(junk[:, 0:352], pb[1][:, 0:352], AF.Copy, [Lpb[1]], [Ljunk])
                CP("dve", dqs, pb[3][:, 0:256].rearrange("p (a b) -> p a b", a=2), [Lpb[3]], [Ldqs])
                DMA("sp", dqT_d[:, :, tc], dqs, [Ldqs], [LdqT])
                CP("dve", dkT[:, :, tc], pb[3][:, 256:512].rearrange("p (a b) -> p a b", a=2), [Lpb[3]], [LdkT])
                ACT(iqs, pb[4].rearrange("p (a b) -> p a b", a=4), AF.Copy, [Lpb[4]], [Liqs])
                DMA("sp", iqT_d[:, :, tc], iqs, [Liqs], [LiqT])
                CP("dve", dvc[:, i, :, 0:64], pb[2][:, 0:256].rearrange("p (a b) -> p a b", a=4), [Lpb[2]], [Ldvc])
                CP("dve", iwt, pb[2][:, 320:328], [Lpb[2]], [Liwt])
                DMA("sp", iws_d[tc, :], iwt, [Liwt], [Liws])
                CP("dve", ikx, pb[2][:, 256:320], [Lpb[2]], [Likx])

            def K_rest(i):
                ks_ = KS[i % 2]
                stat, Lstat = ks_["stat"]; junk, Ljunk = ks_["junk"]
                mqs, Lmqs = ks_["mqs"]; ikx, Likx = ks_["ikx"]; cen, Lcen = ks_["cen"]; kin2, Lkin2 = ks_["kin2"]
                qn, Lqn = ks_["qn"]; qnT, LqnT = ks_["qnT"]; kvn, Lkvn = ks_["kvn"]; kvnT, LkvnT = ks_["kvnT"]
                qfull, Lqfull = ks_["qfull"]; kpe, Lkpe = ks_["kpe"]
                stA, LstA = ks_["stA"]; stB, LstB = ks_["stB"]; stC, LstC = ks_["stC"]
                scA, LscA = ks_["scA"]; scB, LscB = ks_["scB"]; scC, LscC = ks_["scC"]
                tc = slice(i * 128, (i + 1) * 128)

                def chainA():
                    RED(stA[:, 4:5], ikx, ALU.add, [Likx], [LstA]); yield
                    TS("dve", stA[:, 5:6], stA[:, 4:5], -1.0 / 64, None, ALU.mult, None, [LstA], [LstA]); yield
                    TS("dve", cen, ikx, stA[:, 5:6], None, ALU.add, None, [Likx, LstA], [Lcen]); yield
                    TTR(scA, cen, cen, stA[:, 6:7], [Lcen], [LscA, LstA]); yield
                    RSTD(stA[:, 8:9], stA[:, 6:7], stA[:, 7:8], 1.0 / 64, [LstA], [LstA]); yield
                    TS("dve", kin2[:, 0:64], cen, stA[:, 8:9], None, ALU.mult, None, [Lcen, LstA], [Lkin2])
                    TS("dve", kin2[:, 64:128], cen, stA[:, 8:9], None, ALU.mult, None, [Lcen, LstA], [Lkin2]); yield
                    TR(b7[:, 0:128], kin2, idb, [Lkin2, Lidb], [Lpb[7]]); yield
                    ACT(kiT2[:, tc], b7[:, 0:128], AF.Identity, [Lpb[7], Lncol], [LkiT2], scale=ncol[:, 20:21], bias=ncol[:, 21:22]); yield

                def chainB():
                    TTR(scB, junk[:, 0:192], junk[:, 0:192], stB[:, 9:10], [Ljunk], [LscB, LstB]); yield
                    RSTD(stB[:, 11:12], stB[:, 9:10], stB[:, 10:11], 1.0 / 192, [LstB], [LstB]); yield
                    ACT(qn, junk[:, 0:192], AF.Copy, [Ljunk, LstB], [Lqn], scale=stB[:, 11:12]); yield
                    TR(b7[:, 128:256], qn[:, 0:128], idb, [Lqn, Lidb], [Lpb[7]])
                    TR(b7[0:64, 256:384], qn[:, 128:192], idb, [Lqn, Lidb], [Lpb[7]]); yield
                    CP("dve", qnT[:, 0, :], b7[:, 128:256], [Lpb[7]], [LqnT])
                    CP("dve", qnT[0:64, 1, :], b7[0:64, 256:384], [Lpb[7]], [LqnT]); yield
                    MM(pb[5][:, 0:384], qnT[:, 0, :], wqb[:, 0, :], True, False, [LqnT, Lwqb], [Lpb[5]])
                    MM(pb[5][:, 0:384], qnT[0:64, 1, :], wqb[0:64, 1, :], False, True, [LqnT, Lwqb], [Lpb[5]]); yield
                    q3 = pb[5][:, 0:384].rearrange("p (a b) -> p a b", a=4)
                    CP("dve", qfull[:, :, 0:64], q3[:, :, 0:64], [Lpb[5]], [Lqfull]); yield
                    rope(ks_, qfull[:, :, 64:80], qfull[:, :, 80:96], q3[:, :, 64:80], q3[:, :, 80:96], i, 4, [Lpb[5]], [Lqfull]); yield
                    for h in range(4):
                        TR(b7[0:96, 384 + h * 128:384 + (h + 1) * 128], qfull[:, h, :], idb, [Lqfull, Lidb], [Lpb[7]])
                    yield
                    CP("dve", mqs[0:96], b7[0:96, 384:896].rearrange("p (a b) -> p a b", a=4), [Lpb[7]], [Lmqs]); yield
                    DMA("sp", mqT_d[:, :, tc], mqs[0:96], [Lmqs], [LmqT]); yield

                def chainC():
                    TTR(scC, junk[:, 192:320], junk[:, 192:320], stC[:, 12:13], [Ljunk], [LscC, LstC]); yield
                    RSTD(stC[:, 14:15], stC[:, 12:13], stC[:, 13:14], 1.0 / 128, [LstC], [LstC]); yield
                    ACT(kvn, junk[:, 192:320], AF.Copy, [Ljunk, LstC], [Lkvn], scale=stC[:, 14:15]); yield
                    TR(b7[:, 896:1024], kvn, idb, [Lkvn, Lidb], [Lpb[7]]); yield
                    CP("dve", kvnT, b7[:, 896:1024], [Lpb[7]], [LkvnT]); yield
                    MM(pb[6], kvnT, wkvb, True, True, [LkvnT, Lwkvb], [Lpb[6]]); yield
                    CP("dve", mvc[:, i, :, 0:64], pb[6].rearrange("p (a b) -> p a b", a=4)[:, :, 64:128], [Lpb[6]], [Lmvc]); yield
                    for h in range(4):
                        MM(pb[6][0:64, h * 128:(h + 1) * 128], wkvb[:, h * 128:h * 128 + 64], kvnT, True, True,
                           [LkvnT, Lwkvb], [Lpb[6]])
                    yield
                    CP("dve", mkT[0:64, :, tc], pb[6][0:64, :].rearrange("p (a b) -> p a b", a=4), [Lpb[6]], [LmkT]); yield

                def chainD():
                    rope(ks_, kpe[:, 64:80].unsqueeze(1), kpe[:, 80:96].unsqueeze(1), junk[:, 320:336].unsqueeze(1),
                         junk[:, 336:352].unsqueeze(1), i, 1, [Ljunk], [Lkpe]); yield

                gens = [chainA(), chainB(), chainC()]
                while gens:
                    for g_ in list(gens):
                        try:
                            next(g_)
                        except StopIteration:
                            gens.remove(g_)
                for _ in chainD():
                    pass
                TR(b5[0:96, 768:896], kpe, idb, [Lkpe, Lidb], [Lpb[5]])
                CP("dve", mkT[64:96, :, tc], b5[64:96, 768:896].unsqueeze(1).to_broadcast([32, 4, 128]), [Lpb[5]], [LmkT])

            K_front(0)
            for i in range(NT):
                if i + 1 < NT:
                    K_front(i + 1)
                K_rest(i)

            if debug == "K":
                break
            SCs = [AR.carve(53504 + 8192 * k, [128, S], F32, f"SC{k}") for k in range(2)]
            bjunk, Lbjunk = AR.carve(69888, [128, S], BF16, "bjunk")
            TM.reset()
            xt, Lxt = TM.alloc([128, D], F32, "xtA")
            hm, Lhm = TM.alloc([128, D], F32, "hm")
            mqs2 = [TM.alloc([128, 4, 128], BF16, f"mq{k}") for k in range(2)]
            dqs2 = [TM.alloc([128, 4, 128], BF16, f"dq{k}") for k in range(2)]
            iqs2 = [TM.alloc([128, 8, 128], BF16, f"iq{k}") for k in range(2)]
            for k in range(2):
                MS("pool", dqs2[k][0], 0.0, [dqs2[k][1]])
                MS("pool", iqs2[k][0], 0.0, [iqs2[k][1]])
            iws2 = [TM.alloc([128, 8], F32, f"iw{k}") for k in range(2)]
            aws2 = [TM.alloc([128, 8], F32, f"aw{k}") for k in range(2)]
            sgs2 = [TM.alloc([128, 8], F32, f"sg{k}") for k in range(2)]
            Dh1 = TM.alloc([128, 8, 128], BF16, "Dh0")
            Dhs2 = [Dh1, Dh1]
            Rbs = [TM.alloc([128, 512], BF16, f"Rb{k}") for k in range(3)]
            mbuf, Lmbuf = TM.alloc([128, S], BF16, "mbuf")
            Ebs = [TM.alloc([128, 512], BF16, f"Eb{k}") for k in range(4)]
            thrs = [TM.alloc([128, 1], F32, f"thr{k}") for k in range(2)]
            bs, Lbs = TM.alloc([128, 8], F32, "bs")
            wk, Lwk = TM.alloc([128, KBIS], F32, "wk")
            cand, Lcand = TM.alloc([128, 1], F32, "cand")
            cnt, Lcnt = TM.alloc([128, 1], F32, "cnt")
            dl, Ldl = TM.alloc([128, 1], F32, "dl")
            tt_, Ltt = TM.alloc([128, 1], F32, "tt")
            yht2, Lyht2 = TM.alloc([128, 512], BF16, "yht2")
            mixed, Lmixed = TM.alloc([128, 512], BF16, "mixed")
            mixT, LmixT = TM.alloc([128, 8, 128], BF16, "mixT")
            rec, Lrec = TM.alloc([128, 8], F32, "rec")
            pbL = [0, 1]
            b6 = pb[6].bitcast(BF16)
            b2 = pb[2].bitcast(BF16)
            pTh = [b6[:, 0:512], b2[:, 0:512]]
            LpTh = [Lpb[6], Lpb[2]]
            LO = [Lpb[7]] * 4
            IDX_SCALE = (8.0 ** -0.5) * (64.0 ** -0.5)

            def load_q(i):
                tc = slice(i * 128, (i + 1) * 128)
                k = i % 2
                DMA("sp", mqs2[k][0][0:96], mqT_d[:, :, tc], [LmqT], [mqs2[k][1]])
                for e_ in range(2):
                    DMA("sp", dqs2[k][0][64 * e_:64 * e_ + 64, e_:4:2, :], dqT_d[64 * e_:64 * e_ + 64, :, tc], [LdqT], [dqs2[k][1]])
                    DMA("sp", iqs2[k][0][64 * e_:64 * e_ + 64, e_:8:2, :], iqT_d[64 * e_:64 * e_ + 64, :, tc], [LiqT], [iqs2[k][1]])
                DMA("sp", iws2[k][0], iws_d[tc, :], [Liws], [iws2[k][1]])

            def prep(i):
                k = i % 2
                iw, Liw = iws2[k]; aw, Law = aws2[k]; sg, Lsg = sgs2[k]; Dh, LDh = Dhs2[k]
                STT(aw, iw, -1.0, iw, ALU.mult, ALU.max, [Liw], [Law])
                TS("dve", aw, aw, IDX_SCALE, None, ALU.mult, None, [Law], [Law])
                TS("dve", sg, iw, 0.0, 2.0, ALU.is_ge, ALU.mult, [Liw], [Lsg])
                TS("dve", sg, sg, -1.0, None, ALU.add, None, [Lsg], [Lsg])
                for h in range(8):
                    TS("dve", Dh[:, h, :], idb, sg[:, h:h + 1], None, ALU.mult, None, [Lidb, Lsg], [LDh])

            def indexer(i):
                k = i % 2
                iq, Liq = iqs2[k]; iw, Liw = iws2[k]; aw, Law = aws2[k]; sg, Lsg = sgs2[k]; Dh, LDh = Dhs2[k]
                SC, LSC = SCs[k]
                n = 128 * (i + 1)
                nblk = (n + 511) // 512
                items = [(c, h) for c in range(nblk) for h in range(8)]

                def st0(j):
                    c, h = items[j]
                    w = min(512, n - c * 512)
                    pl = pbL[j % 2]
                    MM(pb[pl][:, 0:w], iq[:, h, :], kiT2[:, c * 512:c * 512 + w], True, True,
                       [Liq, LkiT2], [Lpb[pl]])
                    Rb, LRb = Rbs[j % 3]
                    ACT(Rb[:, 0:w], pb[pl][:, 0:w], AF.Relu, [Lpb[pl], Law], [LRb], scale=aw[:, h:h + 1])

                def st1(j):
                    c, h = items[j]
                    w = min(512, n - c * 512)
                    Rb, LRb = Rbs[j % 3]
                    MM(pb[3][:, 0:w], Dh[:, h, :], Rb[:, 0:w], h == 0, h == 7, [LDh, LRb], [Lpb[3]])
                    if h == 7:
                        ACT(SC[:, c * 512:c * 512 + w], pb[3][:, 0:w], AF.Copy, [Lpb[3]], [LSC])
                for s_ in range(len(items) + 2):
                    if s_ < len(items):
                        st0(s_)
                    if 0 <= s_ - 2 < len(items):
                        st1(s_ - 2)
                TT("pool", SC[:, n - 128:n], SC[:, n - 128:n], cmNEG, ALU.add, [LSC, LcmNEG], [LSC])

            def bisect(i):
                k = i % 2
                SC, LSC = SCs[k]
                thr_, Lthr = thrs[k]
                n = 128 * (i + 1)
                if n <= NSEL:
                    MS("dve", thr_, -1.0e29, [Lthr])
                    return
                RED(bs[:, 0:1], SC[:, 0:128 * i], ALU.min, [LSC], [Lbs])
                RED(bs[:, 1:2], SC[:, 0:n], ALU.max, [LSC], [Lbs])
                TT("dve", bs[:, 2:3], bs[:, 1:2], bs[:, 0:1], ALU.subtract, [Lbs], [Lbs])
                TS("dve", wk, pw2, bs[:, 2:3], None, ALU.mult, None, [Lpw2, Lbs], [Lwk])
                CP("dve", tt_, bs[:, 0:1], [Lbs], [Ltt])
                for kk_ in range(KBIS):
                    TT("dve", cand, tt_, wk[:, kk_:kk_ + 1], ALU.add, [Ltt, Lwk], [Lcand])
                    TS("dve", bjunk[:, 0:n], SC[:, 0:n], cand, 0.0, ALU.is_ge, ALU.add, [LSC, Lcand], [Lbjunk, Lcnt], accum=cnt)
                    TS("dve", dl, cnt, NSEL - 0.5, wk[:, kk_:kk_ + 1], ALU.is_ge, ALU.mult, [Lcnt, Lwk], [Ldl])
                    TT("dve", tt_, tt_, dl, ALU.add, [Ltt, Ldl], [Ltt])
                CP("dve", thr_, tt_, [Ltt], [Lthr])

            def maskbias(i):
                SC, LSC = SCs[i % 2]
                thr_, Lthr = thrs[i % 2]
                n = 128 * (i + 1)
                TS("dve", mbuf[:, 0:n], SC[:, 0:n], thr_[:, 0:1], -30000.0, ALU.is_lt, ALU.mult, [LSC, Lthr], [Lmbuf])

            SB_ = [2, 4, 5, 6]

            def attention(i, kind):
                k = i % 2
                if kind == "mla":
                    q, Lq = mqs2[k]; kc_, Lkc = mkT, LmkT; vc, Lvc = mvc, Lmvc
                    nblk = (i + 4) // 4
                    blocks = [list(range(4 * c, min(4 * c + 4, i + 1))) for c in range(nblk)]
                    scale = 96.0 ** -0.5
                else:
                    q, Lq = dqs2[k]; kc_, Lkc = dkT, LdkT; vc, Lvc = dvc, Ldvc
                    blocks = [list(range(4 * c, min(4 * c + 4, i + 1))) for c in range((i + 4) // 4)]
                    scale = 0.125
                items = [(h, b) for h in range(4) for b in range(len(blocks))]

                def st0(j):
                    h, b = items[j]
                    tiles = blocks[b]
                    w = 128 * len(tiles)
                    pS = SB_[j % 4]
                    Eb, LEb = Ebs[j % 4]
                    for jj, tj in enumerate(tiles):
                        o_ = pb[pS][:, jj * 128:(jj + 1) * 128]
                        ks_ = slice(tj * 128, (tj + 1) * 128)
                        if kind == "mla":
                            MM(o_, kc_[0:96, h, ks_], q[0:96, h, :], True, True, [Lq, Lkc], [Lpb[pS]])
                        else:
                            near = tj >= i - 1
                            MM(o_, kc_[:, h // 2, ks_], q[:, h, :], True, False, [Lq, Lkc], [Lpb[pS]])
                            MM(o_, mbuf[:, ks_], idb, False, not near, [Lidb, Lmbuf], [Lpb[pS]])
                            if near:
                                bo = 128 if tj == i else 0
                                MM(o_, biasN8[:, h, bo:bo + 128], idb, False, True, [Lidb, LbiasN8], [Lpb[pS]])
                    if kind == "mla":
                        ACT(Eb[:, 0:w], pb[pS][:, 0:w], AF.Exp, [Lpb[pS]], [LEb], scale=scale)
                        if tiles[-1] == i:
                            TT("pool", Eb[:, w - 128:w], Eb[:, w - 128:w], cm01T, ALU.mult, [LEb, Lcm01T], [LEb])
                    else:
                        nfar = sum(1 for tj in tiles if tj < i - 1)
                        if nfar > 0:
                            ACT(Eb[:, 0:128 * nfar], pb[pS][:, 0:128 * nfar], AF.Exp, [Lpb[pS], Ltabb], [LEb], scale=scale,
                                bias=tabb[:, 124 + h:125 + h])
                        if nfar < len(tiles):
                            ACT(Eb[:, 128 * nfar:w], pb[pS][:, 128 * nfar:w], AF.Exp, [Lpb[pS]], [LEb], scale=scale)

                def st2(j):
                    h, b = items[j]
                    tiles = blocks[b]
                    Eb, LEb = Ebs[j % 4]
                    for jj, tj in enumerate(tiles):
                        MM(pb[7][:, h * 65:(h + 1) * 65], Eb[:, jj * 128:(jj + 1) * 128], vc[:, tj, h, :],
                           (b == 0 and jj == 0), (b == len(blocks) - 1 and jj == len(tiles) - 1), [LEb, Lvc], [LO[h]])
                for s_ in range(len(items) + 2):
                    if s_ < len(items):
                        st0(s_)
                    if 0 <= s_ - 2 < len(items):
                        st2(s_ - 2)
                off = 0 if kind == "mla" else 256
                ro = 0 if kind == "mla" else 4
                o3 = pb[7][:, 0:260].rearrange("p (a b) -> p a b", a=4)
                ACT(rec[:, ro:ro + 4], o3[:, :, 64], AF.Ln, [LO[0]], [Lrec])
                ACT(rec[:, ro:ro + 4], rec[:, ro:ro + 4], AF.Exp, [Lrec], [Lrec], scale=-1.0)
                for h in range(4):
                    ACT(mixed[:, off + h * 64:off + (h + 1) * 64], pb[7][:, h * 65:h * 65 + 64], AF.Copy,
                        [LO[h], Lrec], [Lmixed], scale=rec[:, ro + h:ro + h + 1])

            def epilogue(i):
                tc = slice(i * 128, (i + 1) * 128)
                DMA("sp", xt, src_d[tc, :], rsrc, [Lxt])
                DMA("sp", yht2, yh_d[tc, :], [Lyh], [Lyht2])
                if debug:
                    DMA("sp", mix_d[tc, :], mixed, [Lmixed], [Lmixd])
                for kc in range(4):
                    TR(b6[:, kc * 128:(kc + 1) * 128], yht2[:, kc * 128:(kc + 1) * 128], idb, [Lyht2, Lidb], [LpTh[0]])
                for kc in range(4):
                    TR(b2[:, kc * 128:(kc + 1) * 128], mixed[:, kc * 128:(kc + 1) * 128], idb, [Lmixed, Lidb], [LpTh[1]])
                ACT(mixT[:, 0:4, :], b6[:, 0:512].rearrange("p (a b) -> p a b", a=4), AF.Copy, [LpTh[0]], [LmixT])
                ACT(mixT[:, 4:8, :], b2[:, 0:512].rearrange("p (a b) -> p a b", a=4), AF.Copy, [LpTh[1]], [LmixT])
                for n_ in range(2):
                    for kc in range(8):
                        MM(pb[4 + n_], mixT[:, kc, :], wout[:, kc, n_ * 512:(n_ + 1) * 512], kc == 0, kc == 7,
                           [LmixT, Lwout], [Lpb[4 + n_]])
                    ACT(hm[:, n_ * 512:(n_ + 1) * 512], pb[4 + n_], AF.Copy, [Lpb[4 + n_]], [Lhm])
                    TT("pool", hm[:, n_ * 512:(n_ + 1) * 512], hm[:, n_ * 512:(n_ + 1) * 512], xt[:, n_ * 512:(n_ + 1) * 512],
                       ALU.add, [Lhm, Lxt], [Lhm])
                DMA("pool", hb1_d[tc, :], hm, [Lhm], [Lhb1])

            load_q(0)
            prep(0)
            indexer(0)
            if NT > 1:
                load_q(1)
            for i in range(NT):
                if i + 1 < NT:
                    prep(i + 1)
                bisect(i)
                if i + 1 < NT:
                    indexer(i + 1)
                attention(i, "mla")
                maskbias(i)
                attention(i, "dsa")
                if i + 2 < NT:
                    load_q(i + 2)
                epilogue(i)
            if debug == "A":
                break
            w1, Lw1 = AR.carve(0, [128, 8, DFF], BF16, "w1")
            w2, Lw2 = AR.carve(32768, [128, 32, D], BF16, "w2")
            hT, LhT = AR.carve(65536, [128, 32, 256], BF16, "hT")
            load_w_rows(w1, Lw1, w1_d[li], 8, 0, DFF, ncol[:, 8:16], Lncol)
            load_w_rows(w2, Lw2, w2_d[li], 32, 0, D)
            TM.reset()
            hts = [TM.alloc([128, D], F32, f"ht{k}") for k in range(4)]
            ub, Lub = TM.alloc([128, D], BF16, "ubM")
            uT2, LuT2 = TM.alloc([128, 8, 256], BF16, "uT2")
            hos = [TM.alloc([128, D], F32, f"ho{k}") for k in range(2)]
            stat, Lstat = TM.alloc([128, 16], F32, "statM")
            junk, Ljunk = TM.alloc([128, D], F32, "junkM")
            sq, Lsq = TM.alloc([128, 256], BF16, "sq")
            sq2, Lsq2 = TM.alloc([128, 256], BF16, "sq2")
            sqs = [(sq, Lsq), (sq2, Lsq2)]
            do_fin = last and final_norm
            if do_fin:
                fnb, Lfnb = TM.alloc([128, D], F32, "fnb")
                DMA("sp", fnb, fnw_d.partition_broadcast(128), [], [Lfnb])
            b0 = pb[0].bitcast(BF16)
            def M_load(st):
                for sub in range(2):
                    i = 2 * st + sub
                    ht, Lht = hts[(st % 2) * 2 + sub]
                    DMA("sp", ht, hb1_d[i * 128:(i + 1) * 128, :], [Lhb1], [Lht])

            M_load(0)
            for st in range(NT // 2):
                if st + 1 < NT // 2:
                    M_load(st + 1)
                for sub in range(2):
                    i = 2 * st + sub
                    ht, Lht = hts[(st % 2) * 2 + sub]
                    TTR(junk, ht, ht, stat[:, 0:1], [Lht], [Ljunk, Lstat])
                    RSTD(stat[:, 2:3], stat[:, 0:1], stat[:, 1:2], 1.0 / D, [Lstat], [Lstat])
                    ACT(ub, ht, AF.Copy, [Lht, Lstat], [Lub], scale=stat[:, 2:3])
                    for kc in range(8):
                        TR(b0[:, kc * 128:(kc + 1) * 128], ub[:, kc * 128:(kc + 1) * 128], idb, [Lub, Lidb], [Lpb[0]])
                    CP("dve", uT2[:, :, sub * 128:(sub + 1) * 128], b0.rearrange("p (a b) -> p a b", a=8), [Lpb[0]], [LuT2])
                for f in range(32):
                    ph = 1 + (f % 3)
                    for kc in range(8):
                        MM(pb[ph][:, 0:256], w1[:, kc, f * 128:(f + 1) * 128], uT2[:, kc, :], kc == 0, kc == 7,
                           [Lw1, LuT2], [Lpb[ph]])
                    s_, Ls_ = sqs[f % 2]
                    ACT(s_, pb[ph][:, 0:256], AF.Relu, [Lpb[ph]], [Ls_])
                    TT("pool", hT[:, f, :], s_, s_, ALU.mult, [Ls_], [LhT])
                for sub in range(2):
                    i = 2 * st + sub
                    ht, Lht = hts[(st % 2) * 2 + sub]
                    ho, Lho = hos[sub]
                    for n_ in range(2):
                        po = 4 + sub * 2 + n_
                        for f in range(32):
                            MM(pb[po], hT[:, f, sub * 128:(sub + 1) * 128], w2[:, f, n_ * 512:(n_ + 1) * 512], f == 0, f == 31,
                               [LhT, Lw2], [Lpb[po]])
                        TT("dve", ho[:, n_ * 512:(n_ + 1) * 512], pb[po], ht[:, n_ * 512:(n_ + 1) * 512], ALU.add,
                           [Lpb[po], Lht], [Lho])
                    if do_fin:
                        TTR(junk, ho, ho, stat[:, 4:5], [Lho], [Ljunk, Lstat])
                        RSTD(stat[:, 6:7], stat[:, 4:5], stat[:, 5:6], 1.0 / D, [Lstat], [Lstat])
                        STT(junk, ho, stat[:, 6:7], fnb, ALU.mult, ALU.mult, [Lho, Lstat, Lfnb], [Ljunk])
                        DMA("sp", out_d[i * 128:(i + 1) * 128, :], junk, [Ljunk], [Lout])
                    else:
                        DMA("sp", (out_d if (last and not debug) else hb0_d)[i * 128:(i + 1) * 128, :], ho, [Lho],
                            [Lout if (last and not debug) else Lhb0])

        final = [Lout] if not debug else [Lyh, Lhb0, Lhb1, LmqT, LdqT, LiqT, Liws, Llbs, Lmixd]
        stats = P.emit(final_waits=final)
        build_nc.stats = stats
    return nc


def _rest_of_layer(env):
    raise NotImplementedError


def host_consts():
    invf = (1.0 / (10000.0 ** (np.arange(0, 32, 2, dtype=np.float32) / np.float32(32)))).astype(np.float32)
    c_invf = np.ascontiguousarray(np.broadcast_to(invf[None, :], (128, 16))).astype(np.float32)
    pw = np.array([2.0 ** -(k + 1) for k in range(KBIS)], dtype=np.float32)
    c_pw2 = np.ascontiguousarray(np.broadcast_to(pw[None, :], (128, KBIS))).astype(np.float32)
    return c_invf, c_pw2


PER_LAYER = ["attn_norm_w", "w_in", "hgrn_norm_w", "mla_q_norm_w", "mla_w_qb", "mla_kv_norm_w", "mla_w_kvb",
             "idx_k_norm_w", "idx_k_norm_b", "w_out", "mlp_norm_w", "w_mlp_in", "w_mlp_out"]
GLOBALS = ["hgrn_lb_logits", "rel_bias_table", "final_norm_w"]
FUSED = True
_NC_CACHE = {}


def _get_nc(layers, final_norm):
    key = (tuple(layers), final_norm)
    if key not in _NC_CACHE:
        _NC_CACHE[key] = build_nc(layers=tuple(layers), final_norm=final_norm, debug=False)
    return _NC_CACHE[key]


def _launch(layers, final_norm, inputs, xs):
    c_invf, c_pw2 = host_consts()
    nc = _get_nc(layers, final_norm)
    shared = {k: np.ascontiguousarray(np.asarray(inputs[k], dtype=np.float32)[list(layers)]) for k in PER_LAYER}
    for k in GLOBALS:
        shared[k] = np.ascontiguousarray(np.asarray(inputs[k], dtype=np.float32))
    pos = np.asarray(inputs["positions"]).astype(np.int32)
    maps = []
    for b in range(8):
        m = dict(shared)
        m["x"] = np.ascontiguousarray(xs[b], dtype=np.float32)
        m["pos"] = np.ascontiguousarray(pos[b])
        m["c_invf"] = c_invf
        m["c_pw2"] = c_pw2
        maps.append(m)
    res = run_bass_kernel_spmd(nc, maps, core_ids=list(range(8)))
    return [np.asarray(res.results[b]["out"], dtype=np.float32) for b in range(8)]


def kernel(**inputs):
    x = np.asarray(inputs["x"], dtype=np.float32)
    xs = [x[b] for b in range(8)]
    if FUSED:
        outs = _launch((0, 1, 2, 3), True, inputs, xs)
    else:
        for l in range(4):
            xs = _launch((l,), l == 3, inputs, xs)
        outs = xs
    return np.stack(outs, axis=0).astype(np.float32)
```
